# Optimizing a Trainium2 kernel written in Bass

```python
import math
import jax
import jax.numpy as jnp
from jax import lax
import numpy as np

D_MODEL = 1024
BATCH = 4
SEQ = 8192
DEPTH = 4

HEAD_DIM = 64
N_HEADS_TOTAL = D_MODEL // HEAD_DIM
N_MEM_HEADS = 4
N_SELF_HEADS = N_HEADS_TOTAL - N_MEM_HEADS
SELF_WIDTH = N_SELF_HEADS * HEAD_DIM
MEM_WIDTH = N_MEM_HEADS * HEAD_DIM
IN_COLS = 3 * SELF_WIDTH + MEM_WIDTH
N_MEM = 256
N_MIXERS = 2
N_MOBA_LAYERS = (DEPTH + 1) // 2
N_FOX_LAYERS = DEPTH // 2
MOBA_BLOCK = 256
MOBA_TOPK = 3
MOBA_Q_CHUNK = 16
FOX_Q_BLOCK = 128
FOX_GATE_BIAS_INIT = 2.0
N_EXPERTS = 32
TOP_K = 4
D_FF = D_MODEL
SWIGLU_LIMIT = 7.0
SWIGLU_ALPHA = 1.702
MOE_BLOCK = 512
DEEPNORM_ALPHA = (2 * DEPTH) ** 0.25
DEEPNORM_BETA = (8 * DEPTH) ** -0.25
LN_EPS = 1e-5

kernel_name = 'hybrid_moba_fox_memxattn_moe_deepnorm'


def _alibi_slopes(n):
    def pow2(m):
        start = 2.0 ** (-(2.0 ** -(math.log2(m) - 3)))
        return [start * start ** i for i in range(m)]
    if math.log2(n).is_integer():
        s = pow2(n)
    else:
        c = 2 ** math.floor(math.log2(n))
        s = pow2(c) + pow2(2 * c)[0::2][: n - c]
    return jnp.asarray(np.array(s, dtype=np.float32))


def layer_norm(x, g, b):
    xf = x.astype(jnp.float32)
    mu = jnp.mean(xf, axis=-1, keepdims=True)
    var = jnp.mean(jnp.square(xf - mu), axis=-1, keepdims=True)
    y = (xf - mu) * lax.rsqrt(var + LN_EPS) * g.astype(jnp.float32) + b.astype(jnp.float32)
    return y.astype(x.dtype)


def split_heads(t, n):
    b, s, _ = t.shape
    return t.reshape(b, s, n, HEAD_DIM).transpose(0, 2, 1, 3)


def moba_attention(q, k, v):
    B, H, T, dh = q.shape
    L = MOBA_BLOCK
    nb = -(-T // L)
    tp = nb * L
    pad = ((0, 0), (0, 0), (0, tp - T), (0, 0))
    q = jnp.pad(q, pad)
    k = jnp.pad(k, pad)
    v = jnp.pad(v, pad)
    kb = k.reshape(B, H, nb, L, dh)
    vb = v.reshape(B, H, nb, L, dh)
    kbar = jnp.mean(kb.astype(jnp.float32), axis=3)
    tpos = jnp.arange(tp)
    qblk = tpos // L
    gate = jnp.einsum('bhtd,bhnd->bhtn', q.astype(jnp.float32), kbar)
    past = jnp.arange(nb)[None, :] < qblk[:, None]
    gate = jnp.where(past, gate, -jnp.inf)
    ks = min(MOBA_TOPK, nb)
    _, sel = lax.top_k(gate, ks)
    sel_ok = sel < qblk[:, None]
    slopes = _alibi_slopes(H)[None, :, None, None]
    scale = dh ** -0.5
    gather = jax.vmap(jax.vmap(lambda blocks, ids: blocks[ids]))
    offs = jnp.arange(L)
    QC = MOBA_Q_CHUNK

    def chunk(i):
        t0 = i * QC
        qc = lax.dynamic_slice_in_dim(q, t0, QC, axis=2)
        tq = t0 + jnp.arange(QC)
        own = t0 // L
        k_own = lax.dynamic_index_in_dim(kb, own, axis=2, keepdims=False)
        v_own = lax.dynamic_index_in_dim(vb, own, axis=2, keepdims=False)
        ids = lax.dynamic_slice_in_dim(sel, t0, QC, axis=2)
        ok = lax.dynamic_slice_in_dim(sel_ok, t0, QC, axis=2)
        k_sel = gather(kb, ids)
        v_sel = gather(vb, ids)
        d_own = tq[:, None] - (own * L + offs)[None, :]
        s_own = jnp.einsum('bhqd,bhkd->bhqk', qc, k_own, preferred_element_type=jnp.float32) * scale
        s_own = jnp.where(d_own >= 0, s_own - slopes * d_own.astype(jnp.float32), -jnp.inf)
        d_sel = tq[None, None, :, None, None] - (ids[..., None] * L + offs)
        s_sel = jnp.einsum('bhqd,bhqjkd->bhqjk', qc, k_sel, preferred_element_type=jnp.float32) * scale
        s_sel = jnp.where(ok[..., None], s_sel - slopes[..., None] * d_sel.astype(jnp.float32), -jnp.inf)
        s = jnp.concatenate([s_own, s_sel.reshape(B, H, QC, ks * L)], axis=-1)
        p = jax.nn.softmax(s, axis=-1).astype(v.dtype)
        p_own = p[..., :L]
        p_sel = p[..., L:].reshape(B, H, QC, ks, L)
        return (jnp.einsum('bhqk,bhkd->bhqd', p_own, v_own)
                + jnp.einsum('bhqjk,bhqjkd->bhqd', p_sel, v_sel))

    out = lax.map(chunk, jnp.arange(tp // QC))
    out = out.transpose(1, 2, 0, 3, 4).reshape(B, H, tp, dh)
    return out[:, :, :T]


def fox_attention(q, k, v, fg_logit):
    B, H, T, dh = q.shape
    c = jnp.cumsum(jax.nn.log_sigmoid(fg_logit.astype(jnp.float32)), axis=-1)
    scale = dh ** -0.5
    kpos = jnp.arange(T)
    QB = FOX_Q_BLOCK

    def block(i):
        t0 = i * QB
        qb = lax.dynamic_slice_in_dim(q, t0, QB, axis=2)
        cq = lax.dynamic_slice_in_dim(c, t0, QB, axis=2)
        tq = t0 + jnp.arange(QB)
        s = jnp.einsum('bhqd,bhkd->bhqk', qb, k, preferred_element_type=jnp.float32) * scale
        s = s + (cq[..., None] - c[:, :, None, :])
        s = jnp.where(kpos[None, :] <= tq[:, None], s, -jnp.inf)
        p = jax.nn.softmax(s, axis=-1).astype(v.dtype)
        return jnp.einsum('bhqk,bhkd->bhqd', p, v)

    out = lax.map(block, jnp.arange(T // QB))
    return out.transpose(1, 2, 0, 3, 4).reshape(B, H, T, dh)


def memory_attention(q, mk, mv):
    s = jnp.einsum('bhqd,bhmd->bhqm', q, mk, preferred_element_type=jnp.float32) * (HEAD_DIM ** -0.5)
    p = jax.nn.softmax(s, axis=-1).astype(mv.dtype)
    return jnp.einsum('bhqm,bhmd->bhqd', p, mv)


def moe(h, router_w, router_b, w_gu, b_gu, w_dn, b_dn):
    B, T, D = h.shape
    N = B * T
    xt = h.reshape(N, D)
    logits = (xt @ router_w).astype(jnp.float32) + router_b.astype(jnp.float32)
    top_v, top_i = lax.top_k(logits, TOP_K)
    gates = jax.nn.softmax(top_v, axis=-1)
    A = N * TOP_K
    flat_e = top_i.reshape(A)
    flat_tok = jnp.repeat(jnp.arange(N), TOP_K)
    order = jnp.argsort(flat_e)
    nblk = A // MOE_BLOCK
    se_blk = flat_e[order].reshape(nblk, MOE_BLOCK)
    stok_blk = flat_tok[order].reshape(nblk, MOE_BLOCK)
    sg_blk = gates.reshape(A)[order].reshape(nblk, MOE_BLOCK)
    e_first = se_blk[:, 0]
    e_last = se_blk[:, -1]
    cnt = e_last - e_first + 1
    ends = jnp.cumsum(cnt)
    starts = ends - cnt
    n_items = nblk + N_EXPERTS - 1
    it = jnp.arange(n_items)
    item_ok = it < ends[-1]
    item_blk = jnp.minimum(jnp.searchsorted(ends, it, side='right'), nblk - 1)
    item_e = jnp.clip(e_first[item_blk] + it - starts[item_blk], 0, N_EXPERTS - 1)

    def run(args):
        b, e, ok = args
        xb = xt[stok_blk[b]]
        gu = xb @ w_gu[e] + b_gu[e]
        g = jnp.minimum(gu[:, :D_FF], SWIGLU_LIMIT)
        u = jnp.clip(gu[:, D_FF:], -SWIGLU_LIMIT, SWIGLU_LIMIT)
        a = g * jax.nn.sigmoid(SWIGLU_ALPHA * g) * (u + 1)
        y = a @ w_dn[e] + b_dn[e]
        wt = jnp.where((se_blk[b] == e) & ok, sg_blk[b], 0.0)
        return y * wt.astype(y.dtype)[:, None]

    ys = lax.map(run, (item_blk, item_e, item_ok))
    out = jnp.zeros((N, D), ys.dtype).at[stok_blk[item_blk].reshape(-1)].add(ys.reshape(-1, D))
    return out.reshape(B, T, D)


def setup_inputs(seed: int = 0) -> dict:
    key = jax.random.key(seed)
    ks = jax.random.split(key, 19)
    nrm = jax.random.normal
    s_in = D_MODEL ** -0.5
    return {
        'x': nrm(ks[0], (BATCH, SEQ, D_MODEL), jnp.float32),
        'mem': nrm(ks[1], (BATCH, N_MEM, D_MODEL), jnp.float32),
        'w_in_moba': nrm(ks[2], (N_MOBA_LAYERS, D_MODEL, IN_COLS), jnp.float32) * s_in,
        'w_in_fox': nrm(ks[3], (N_FOX_LAYERS, D_MODEL, IN_COLS + N_SELF_HEADS), jnp.float32) * s_in,
        'b_fgate': FOX_GATE_BIAS_INIT + 0.1 * nrm(ks[4], (N_FOX_LAYERS, N_SELF_HEADS), jnp.float32),
        'w_mem_kv': nrm(ks[5], (D_MODEL, 2 * MEM_WIDTH), jnp.float32) * s_in,
        'w_o': nrm(ks[6], (DEPTH, D_MODEL, D_MODEL), jnp.float32) * (s_in * DEEPNORM_BETA),
        'ln1_g': 1.0 + 0.02 * nrm(ks[7], (DEPTH, D_MODEL), jnp.float32),
        'ln1_b': 0.02 * nrm(ks[8], (DEPTH, D_MODEL), jnp.float32),
        'router_w': nrm(ks[9], (DEPTH, D_MODEL, N_EXPERTS), jnp.float32) * s_in,
        'router_b': 0.01 * nrm(ks[10], (DEPTH, N_EXPERTS), jnp.float32),
        'w_gate_up': nrm(ks[11], (DEPTH, N_EXPERTS, D_MODEL, 2 * D_FF), jnp.float32) * s_in,
        'b_gate_up': 0.01 * nrm(ks[12], (DEPTH, N_EXPERTS, 2 * D_FF), jnp.float32),
        'w_down': nrm(ks[13], (DEPTH, N_EXPERTS, D_FF, D_MODEL), jnp.float32) * (D_FF ** -0.5 * DEEPNORM_BETA),
        'b_down': 0.01 * nrm(ks[14], (DEPTH, N_EXPERTS, D_MODEL), jnp.float32),
        'ln2_g': 1.0 + 0.02 * nrm(ks[15], (DEPTH, D_MODEL), jnp.float32),
        'ln2_b': 0.02 * nrm(ks[16], (DEPTH, D_MODEL), jnp.float32),
    }


def reference(x, mem, w_in_moba, w_in_fox, b_fgate, w_mem_kv, w_o, ln1_g, ln1_b,
              router_w, router_b, w_gate_up, b_gate_up, w_down, b_down, ln2_g, ln2_b):
    B, T, D = x.shape
    mkv = mem @ w_mem_kv
    mk = split_heads(mkv[..., :MEM_WIDTH], N_MEM_HEADS)
    mv = split_heads(mkv[..., MEM_WIDTH:], N_MEM_HEADS)
    h = x
    for i in range(DEPTH):
        j = i // N_MIXERS
        is_moba = (i % N_MIXERS) == 0
        proj = h @ (w_in_moba[j] if is_moba else w_in_fox[j])
        q_s = split_heads(proj[..., :SELF_WIDTH], N_SELF_HEADS)
        k_s = split_heads(proj[..., SELF_WIDTH:2 * SELF_WIDTH], N_SELF_HEADS)
        v_s = split_heads(proj[..., 2 * SELF_WIDTH:3 * SELF_WIDTH], N_SELF_HEADS)
        q_m = split_heads(proj[..., 3 * SELF_WIDTH:IN_COLS], N_MEM_HEADS)
        if is_moba:
            o_self = moba_attention(q_s, k_s, v_s)
        else:
            fg = (proj[..., IN_COLS:] + b_fgate[j]).transpose(0, 2, 1)
            o_self = fox_attention(q_s, k_s, v_s, fg)
        o_mem = memory_attention(q_m, mk, mv)
        heads = jnp.concatenate([o_self, o_mem], axis=1)
        merged = heads.transpose(0, 2, 1, 3).reshape(B, T, D)
        h = layer_norm(DEEPNORM_ALPHA * h + merged @ w_o[i], ln1_g[i], ln1_b[i])
        f = moe(h, router_w[i], router_b[i], w_gate_up[i], b_gate_up[i], w_down[i], b_down[i])
        h = layer_norm(DEEPNORM_ALPHA * h + f, ln2_g[i], ln2_b[i])
    return h
```

```python
import contextlib
import math
import numpy as np
import ml_dtypes
import concourse.bass as bass
import concourse.mybir as mybir
from concourse.bass_utils import run_bass_kernel_spmd

F32 = mybir.dt.float32
BF16 = mybir.dt.bfloat16
I32 = mybir.dt.int32
U32 = mybir.dt.uint32
ALU = mybir.AluOpType
AF = mybir.ActivationFunctionType
AX = mybir.AxisListType

D_MODEL = 1024
BATCH = 4
SEQ = 8192
DEPTH = 4
HEAD_DIM = 64
N_SELF = 12
N_MEMH = 4
N_MEM = 256
N_EXPERTS = 32
TOP_K = 4
D_FF = 1024
SWIGLU_LIMIT = 7.0
SWIGLU_ALPHA = 1.702
ALPHA = (2 * DEPTH) ** 0.25
LN_EPS = 1e-5
NEG = -30000.0
KAUG = 100
NQT = SEQ // 512
NKT = SEQ // 128


class Prog:
    def __init__(self, nc, stack):
        self.nc = nc
        self.stack = stack
        self.engs = {"pe": nc.tensor, "act": nc.scalar, "dve": nc.vector, "pool": nc.gpsimd, "sp": nc.sync}
        self.sem = {}
        self.cnt = {}
        self.seen = {k: {} for k in self.engs}
        for k in self.engs:
            self.sem[k] = stack.enter_context(nc.semaphore("s_" + k))
            self.cnt[k] = 0
        self.ndsem = 0

    def dsem(self, name=None):
        if name is not None and ("dn_" + name) in self.sem:
            return "dn_" + name
        self.ndsem += 1
        key = "dn_" + name if name is not None else "d%d" % self.ndsem
        self.sem[key] = self.stack.enter_context(self.nc.semaphore(key))
        self.cnt[key] = 0
        return key

    def wait(self, eng, tok):
        if tok is None:
            return
        key, val = tok
        if eng == "pe" and key == "pe":
            return
        if self.seen[eng].get(key, 0) >= val:
            return
        self.engs[eng].wait_ge(self.sem[key], val)
        self.seen[eng][key] = val

    def op(self, eng, fn, deps=(), inc=True):
        for d in deps:
            self.wait(eng, d)
        inst = fn(self.engs[eng])
        if inc:
            self.cnt[eng] += 1
            inst.then_inc(self.sem[eng], 1)
            return (eng, self.cnt[eng])
        return None

    def new_epoch(self):
        self.epoch = getattr(self, "epoch", 0) + 1
        for k in self.engs:
            self.sem[k] = self.stack.enter_context(self.nc.semaphore("s_%s_e%d" % (k, self.epoch)))
            self.cnt[k] = 0
            for e in self.engs:
                self.seen[e].pop(k, None)

    def cc(self, dkey, fn, deps=()):
        for d in deps:
            self.wait("pool", d)
        inst = fn(self.engs["pool"])
        self.cnt[dkey] += 1
        inst.then_inc(self.sem[dkey])
        return (dkey, self.cnt[dkey])

    def dma(self, q, dkey, fn, deps=()):
        for d in deps:
            self.wait(q, d)
        inst = fn(self.engs[q])
        self.cnt[dkey] += 16
        inst.then_inc(self.sem[dkey], 16)
        return (dkey, self.cnt[dkey])


def _alibi_slopes(n):
    def pow2(m):
        start = 2.0 ** (-(2.0 ** -(math.log2(m) - 3)))
        return [start * start ** i for i in range(m)]
    if math.log2(n).is_integer():
        s = pow2(n)
    else:
        c = 2 ** math.floor(math.log2(n))
        s = pow2(c) + pow2(2 * c)[0::2][: n - c]
    return np.array(s, dtype=np.float32)


def emit_attn(nc, P, B, Bbf, kind, E, L):
    fox = kind == "fox"
    j = L // 2
    sfx = "_a%d" % L
    hTp = E["hTp"]
    wq, wk, wv = E["wq"][L], E["wk"][L], E["wv"][L]
    memT, wmk, wmv = E["memT"], E["wmk"], E["wmv"]
    ident_d, negm_d, kaug_d, qaug_d, iq_d, sel_d = E["ident"], E["negm"], E["kaug"], E["qaug"], E["iq"], E["sel"]
    if fox:
        wfgp, bfg, tri_d = E["wfgp"][j], E["bfg"][j], E["tri"]
    else:
        cdec, maskm_d = E["cdec"], E["maskm"]
    OT, dscr = E["OT_own"], E["dscr"]

    with contextlib.ExitStack() as st:

        def sb(name, shape, dt):
            return st.enter_context(nc.sbuf_tensor(name + sfx, shape, dt))

        QT = [sb("QT%d" % r, [KAUG, SEQ], BF16) for r in range(2)]
        KT = [sb("KT%d" % r, [KAUG, SEQ], BF16) for r in range(2)]
        VA = sb("VA", [128, NKT, 2, 128], BF16)
        hTr = [sb("hTr%d" % s, [128, 8, 512], BF16) for s in range(2)]
        wq_s = sb("wq_s", [128, 8, 128], BF16)
        wk_s = sb("wk_s", [128, 8, 128], BF16)
        wv_s = sb("wv_s", [128, 8, 128], BF16)
        memT_s = sb("memT_s", [128, 8, N_MEM], BF16)
        wmk_s = sb("wmk_s", [128, 8, 128], BF16)
        wmv_s = sb("wmv_s", [128, 8, 128], BF16)
        PT = [sb("PT%d" % s, [128, 512], BF16) for s in range(4)]
        TMP = [sb("TMP%d" % s, [128, 512], F32) for s in range(2)]
        Dt = [sb("Dt%d" % r, [128, NQT * NKT], F32) for r in range(2)]
        negm = sb("negm_s", [128, 512], F32)
        ident = sb("ident_s", [128, 128], BF16)
        OS = [sb("OS%d" % s, [64, 512], BF16) for s in range(2)]
        RR = [sb("RR%d" % s, [64, 512], F32) for s in range(2)]
        iq = sb("iq_s", [32, 16], F32)
        sel = sb("sel_s", [32, 256], F32)
        Cc = sb("Cc", [32, 512], F32)
        Z1 = sb("Z1", [32, 512], F32)
        RQ = sb("RQ", [32, 2, 512], BF16)
        RK = sb("RK", [32, 2, 512], BF16)
        refs = sb("refs", [32, 80], F32)
        bq_s = sb("bq_s", [128, 16], F32)
        if fox:
            wfg_s = sb("wfg_s", [128, 8, 512], BF16)
            bfg_s = sb("bfg_s", [32, 3], F32)
            tri = sb("tri_s", [32, 32], F32)
            ones32 = sb("ones32", [32, 512], F32)
            offs = sb("offs", [32, 1], F32)
        else:
            maskm = sb("maskm_s", [128, 2048], F32)
            kbar = [sb("kbar%d" % r, [64, 32], BF16) for r in range(2)]
            kbf = sb("kbf", [64, 32], F32)
            gp = [sb("gp%d" % s, [128, 128], F32) for s in range(2)]
            m8 = [sb("m8_%d" % s, [128, 4, 8], F32) for s in range(2)]
            thr = [sb("thr%d" % s, [128, 4], F32) for s in range(2)]
            TB = [sb("TB%d" % s, [128, 4, 96], BF16) for s in range(2)]
        d_c = P.dsem("const")
        toks = []
        toks.append(P.dma("pool", d_c, lambda e: e.dma_start(out=ident[:], in_=ident_d)))
        toks.append(P.dma("sp", d_c, lambda e: e.dma_start(out=negm[:], in_=negm_d)))
        toks.append(P.dma("sp", d_c, lambda e: e.dma_start(out=iq[:], in_=iq_d)))
        toks.append(P.dma("sp", d_c, lambda e: e.dma_start(out=sel[:], in_=sel_d)))
        for r in range(2):
            toks.append(P.dma("pool", d_c, lambda e, r=r: e.dma_start(out=KT[r][64:100, :], in_=kaug_d)))
            toks.append(P.dma("pool", d_c, lambda e, r=r: e.dma_start(out=QT[r][64:100, :], in_=qaug_d)))
        toks.append(P.dma("pool", d_c, lambda e: e.dma_start(
            out=memT_s[:], in_=memT.rearrange("(kc p) t -> p kc t", p=128))))
        toks.append(P.dma("pool", d_c, lambda e: e.dma_start(
            out=wmk_s[:], in_=wmk.rearrange("(kc p) t -> p kc t", p=128))))
        toks.append(P.dma("pool", d_c, lambda e: e.dma_start(
            out=wmv_s[:], in_=wmv.rearrange("(kc p) t -> p kc t", p=128))))
        if fox:
            toks.append(P.dma("sp", d_c, lambda e: e.dma_start(out=bfg_s[:], in_=bfg)))
            toks.append(P.dma("sp", d_c, lambda e: e.dma_start(out=tri[:], in_=tri_d)))
        else:
            toks.append(P.dma("sp", d_c, lambda e: e.dma_start(out=maskm[:], in_=maskm_d)))
        const_tok = toks[-1]
        t_va = P.op("pool", lambda e: e.memset(VA[:], 1.0))
        if fox:
            t_ones = P.op("pool", lambda e: e.memset(ones32[:], 1.0))
        else:
            for s in range(2):
                t_tb = P.op("pool", lambda e, s=s: e.memset(TB[s][:], 0.0))

        d_h = [P.dsem("h%d" % s) for s in range(2)]
        d_w = P.dsem("w")
        d_o = [P.dsem("o%d" % s) for s in range(2)]
        d_dec = P.dsem("dec")
        d_dec2 = P.dsem("dec2")

        hT_free = [None, None]
        w_free = None
        bank_free = {i: None for i in range(7)}
        bank_free["bf"] = None
        PT_free = [None] * 4
        TMP_free = [None] * 2
        OS_free = [None] * 2
        RR_free = [None] * 2
        qk_last_reader = [None, None]
        unit = 0
        ctrs = {"qk": 0, "ex": 0, "tx": 0}
        TB_free = [None, None]
        out_toks = []
        dt_free = [None, None]

        for p in range(4):
            mem_pass = p == 3
            wdeps = [w_free]
            tw = P.dma("pool", d_w, lambda e, p=p: e.dma_start(
                out=wq_s[:], in_=wq[:, p * 128:(p + 1) * 128].rearrange("(kc p) t -> p kc t", p=128)), deps=wdeps)
            if not mem_pass:
                tw = P.dma("pool", d_w, lambda e, p=p: e.dma_start(
                    out=wk_s[:], in_=wk[:, p * 128:(p + 1) * 128].rearrange("(kc p) t -> p kc t", p=128)))
                tw = P.dma("pool", d_w, lambda e, p=p: e.dma_start(
                    out=wv_s[:], in_=wv[:, p * 128:(p + 1) * 128].rearrange("(kc p) t -> p kc t", p=128)))
                if fox:
                    tw = P.dma("pool", d_w, lambda e, p=p: e.dma_start(
                        out=wfg_s[:], in_=wfgp[p].rearrange("(kc p) t -> p kc t", p=128)))

            last_ev = {}
            for i in range(NQT):
                s = i % 2
                for c4 in range(4):
                    th = P.dma("pool", d_h[s], lambda e, i=i, s=s, c4=c4: e.dma_start(
                        out=hTr[s][:, 2 * c4:2 * c4 + 2, :],
                        in_=hTp[c4, i // 8, :, :, (i % 8) * 512:(i % 8 + 1) * 512].rearrange("kk p t -> p kk t")),
                        deps=[hT_free[s]])
                cols = slice(i * 512, (i + 1) * 512)
                bq = B[0 + s]
                for kc in range(8):
                    tq = P.op("pe", lambda e, kc=kc, bq=bq, s=s: e.matmul(
                        bq[:], wq_s[:, kc, :], hTr[s][:, kc, :], start=(kc == 0), stop=(kc == 7)),
                        deps=[th, tw, bank_free[0 + s]] + ([qk_last_reader[0], qk_last_reader[1]] if kc == 0 else []),
                        inc=(kc == 7))
                e0 = P.op("act", lambda e, bq=bq, cols=cols: e.mul(QT[0][0:64, cols], bq[0:64, :], 0.125), deps=[tq, const_tok])
                e1 = P.op("act", lambda e, bq=bq, cols=cols: e.mul(QT[1][0:64, cols], bq[64:128, :], 0.125), deps=[tq])
                bank_free[0 + s] = e1
                last_ev["q"] = e1
                if not mem_pass:
                    bk = B[2 + s]
                    for kc in range(8):
                        tk = P.op("pe", lambda e, kc=kc, bk=bk, s=s: e.matmul(
                            bk[:], wk_s[:, kc, :], hTr[s][:, kc, :], start=(kc == 0), stop=(kc == 7)),
                            deps=[bank_free[2 + s]], inc=(kc == 7))
                    e2 = P.op("dve", lambda e, bk=bk, cols=cols: e.tensor_copy(KT[0][0:64, cols], bk[0:64, :]), deps=[tk, const_tok])
                    e3 = P.op("dve", lambda e, bk=bk, cols=cols: e.tensor_copy(KT[1][0:64, cols], bk[64:128, :]), deps=[tk])
                    bank_free[2 + s] = e3
                    last_ev["k"] = e3
                    bv = B[4 + s]
                    for sub in range(4):
                        for kc in range(8):
                            tv = P.op("pe", lambda e, kc=kc, bv=bv, s=s, sub=sub: e.matmul(
                                bv[:, sub * 128:(sub + 1) * 128], hTr[s][:, kc, sub * 128:(sub + 1) * 128], wv_s[:, kc, :],
                                start=(kc == 0), stop=(kc == 7)),
                                deps=[bank_free[4 + s]], inc=(sub == 3 and kc == 7))
                    bv3 = bv[:].rearrange("p (a c) -> p a c", a=4)
                    e4 = P.op("dve", lambda e, bv3=bv3, i=i: e.tensor_copy(VA[:, 4 * i:4 * i + 4, 0, 0:64], bv3[:, :, 0:64]), deps=[tv, t_va])
                    e5 = P.op("act", lambda e, bv3=bv3, i=i: e.copy(VA[:, 4 * i:4 * i + 4, 1, 0:64], bv3[:, :, 64:128]), deps=[tv, t_va, e4])
                    bank_free[4 + s] = e5
                    last_ev["v0"] = e4
                    last_ev["v1"] = e5
                    if fox:
                        for kc in range(8):
                            tf = P.op("pe", lambda e, kc=kc, s=s, i=i: e.matmul(
                                B[6][0:32, :], wfg_s[:, kc, i * 32:(i + 1) * 32], hTr[s][:, kc, :],
                                start=(i == 0 and kc == 0), stop=(i == NQT - 1 and kc == 7)),
                                deps=[bank_free[6]] if (i == 0 and kc == 0) else [], inc=(kc == 7))
                        hT_free[s] = tf
                    else:
                        hT_free[s] = tv
                else:
                    hT_free[s] = tq
            w_free = hT_free[(NQT - 1) % 2]
            proj_done = [last_ev[k] for k in last_ev]

            if mem_pass:
                for kc in range(8):
                    tmk = P.op("pe", lambda e, kc=kc: e.matmul(
                        B[2][:, 0:N_MEM], wmk_s[:, kc, :], memT_s[:, kc, :], start=(kc == 0), stop=(kc == 7)),
                        deps=[bank_free[2], const_tok], inc=(kc == 7))
                e2 = P.op("dve", lambda e: e.tensor_copy(KT[0][0:64, 0:N_MEM], B[2][0:64, 0:N_MEM]), deps=[tmk])
                e3 = P.op("dve", lambda e: e.tensor_copy(KT[1][0:64, 0:N_MEM], B[2][64:128, 0:N_MEM]), deps=[tmk])
                bank_free[2] = e3
                for mt in range(2):
                    for kc in range(8):
                        tmv = P.op("pe", lambda e, kc=kc, mt=mt: e.matmul(
                            B[4][:, mt * 128:(mt + 1) * 128], memT_s[:, kc, mt * 128:(mt + 1) * 128], wmv_s[:, kc, :],
                            start=(kc == 0), stop=(kc == 7)),
                            deps=[bank_free[4]], inc=(mt == 1 and kc == 7))
                bv3 = B[4][:, 0:256].rearrange("p (a c) -> p a c", a=2)
                e4 = P.op("dve", lambda e: e.tensor_copy(VA[:, 0:2, 0, 0:64], bv3[:, :, 0:64]), deps=[tmv])
                e5 = P.op("dve", lambda e: e.tensor_copy(VA[:, 0:2, 1, 0:64], bv3[:, :, 64:128]), deps=[tmv])
                bank_free[4] = e5
                proj_done += [e3, e5]

            if not mem_pass:
                if fox:
                    a1 = P.op("act", lambda e, p=p: e.activation(
                        out=Z1[:], in_=B[6][0:32, :], func=AF.Sigmoid, bias=bfg_s[:, p:p + 1], scale=1.0),
                        deps=[tf, const_tok, dt_free[0], dt_free[1]])
                    bank_free[6] = a1
                    a2 = P.op("act", lambda e: e.activation(out=Z1[:], in_=Z1[:], func=AF.Ln), deps=[a1])
                    c1 = P.op("dve", lambda e: e.tensor_tensor_scan(
                        out=Cc[:], data0=ones32[:], data1=Z1[:], initial=0.0, op0=ALU.mult, op1=ALU.add),
                        deps=[a2, t_ones])
                    m1 = P.op("pe", lambda e: e.matmul(B[5][0:32, 0:1], tri[:], Cc[:, 511:512], start=True, stop=True),
                              deps=[c1, bank_free[5], const_tok])
                    c2 = P.op("dve", lambda e: e.tensor_copy(offs[:], B[5][0:32, 0:1]), deps=[m1])
                    c3 = P.op("dve", lambda e: e.tensor_scalar(
                        out=Cc[:], in0=Cc[:], scalar1=offs[:, 0:1], scalar2=None, op0=ALU.add), deps=[c2])
                    bank_free[5] = c2
                    tc_ready = c3
                else:
                    tcd = P.dma("sp", d_dec2, lambda e, p=p: e.dma_start(out=Cc[:], in_=cdec[p]),
                                deps=[dt_free[0], dt_free[1]])
                    tc_ready = tcd
                c4 = P.op("dve", lambda e: e.tensor_scalar(
                    out=Z1[:], in0=Cc[:], scalar1=Cc[:, 255:256], scalar2=None, op0=ALU.subtract), deps=[tc_ready])
                c5 = P.op("dve", lambda e: e.tensor_copy(RQ[:, 0, :], Z1[:]), deps=[c4])
                c6 = P.op("dve", lambda e: e.tensor_tensor(out=Z1[:], in0=Z1[:], in1=RQ[:, 0, :], op=ALU.subtract), deps=[c5])
                c7 = P.op("dve", lambda e: e.tensor_copy(RQ[:, 1, :], Z1[:]), deps=[c6])
                ck = c7
                for jj in range(4):
                    ck = P.op("dve", lambda e, jj=jj: e.tensor_scalar(
                        out=Z1[:, jj * 128:(jj + 1) * 128], in0=Cc[:, jj * 128:(jj + 1) * 128],
                        scalar1=Cc[:, jj * 128 + 63:jj * 128 + 64], scalar2=-1.0, op0=ALU.subtract, op1=ALU.mult), deps=[ck])
                c8 = P.op("dve", lambda e: e.tensor_copy(RK[:, 0, :], Z1[:]), deps=[ck])
                c9 = P.op("dve", lambda e: e.tensor_tensor(out=Z1[:], in0=Z1[:], in1=RK[:, 0, :], op=ALU.subtract), deps=[c8])
                c10 = P.op("dve", lambda e: e.tensor_copy(RK[:, 1, :], Z1[:]), deps=[c9])
                c11 = P.op("dve", lambda e: e.tensor_scalar(
                    out=refs[:, 0:16], in0=iq[:], scalar1=Cc[:, 255:256], scalar2=None, op0=ALU.mult), deps=[c10, const_tok])
                rk3 = refs[:, 16:80].rearrange("p (i j) -> p i j", j=4)
                for jj in range(4):
                    c11 = P.op("dve", lambda e, jj=jj: e.tensor_scalar(
                        out=rk3[:, :, jj], in0=iq[:], scalar1=Cc[:, jj * 128 + 63:jj * 128 + 64], scalar2=None,
                        op0=ALU.mult), deps=[c11])
                dts = []
                for r in range(2):
                    t1 = P.dma("sp", d_dec, lambda e, r=r: e.dma_start(
                        out=dscr[r, 0].rearrange("h (i t) -> i h t", i=16), in_=RQ[r * 16:(r + 1) * 16, :, :]), deps=[c11])
                    t2 = P.dma("sp", d_dec, lambda e, r=r: e.dma_start(
                        out=dscr[r, 1].rearrange("h (i t) -> i h t", i=16), in_=RK[r * 16:(r + 1) * 16, :, :]))
                    dts.append(t2)
                dts2 = []
                for r in range(2):
                    t3 = P.dma("sp", d_dec, lambda e, r=r: e.dma_start(out=QT[r][96:98, :], in_=dscr[r, 0]),
                               deps=[dts[-1], qk_last_reader[r]])
                    t4 = P.dma("sp", d_dec, lambda e, r=r: e.dma_start(out=KT[r][98:100, :], in_=dscr[r, 1]))
                    dts2.append(t4)
                dec_rows_tok = dts2[-1]
                for r in range(2):
                    m2 = P.op("pe", lambda e, r=r: e.matmul(
                        B[5][:, 0:80], sel[:, r * 128:(r + 1) * 128], refs[:], start=True, stop=True),
                        deps=[c11, bank_free[5], const_tok])
                    c12 = P.op("dve", lambda e: e.tensor_copy(bq_s[:], B[5][:, 0:16]), deps=[m2])
                    cl = c12
                    for i in range(NQT):
                        cl = P.op("dve", lambda e, r=r, i=i: e.tensor_scalar(
                            out=Dt[r][:, i * NKT:(i + 1) * NKT], in0=B[5][:, 16:80], scalar1=-1.0, scalar2=bq_s[:, i:i + 1],
                            op0=ALU.mult, op1=ALU.add), deps=[cl], inc=(i == NQT - 1))
                    bank_free[5] = cl
                    dt_ready = cl
                dt_ready_all = dt_ready
            else:
                dec_rows_tok = None
                dt_ready_all = None

            gate_done = []
            if (not mem_pass) and (not fox):
                for r in range(2):
                    k1 = P.op("dve", lambda e, r=r: e.tensor_reduce(
                        out=kbf[:], in_=KT[r][0:64, :].rearrange("p (n l) -> p n l", l=256), axis=AX.X, op=ALU.add),
                        deps=proj_done)
                    k2 = P.op("dve", lambda e, r=r: e.tensor_scalar(
                        out=kbar[r][:], in0=kbf[:], scalar1=1.0 / 256.0, scalar2=None, op0=ALU.mult), deps=[k1])
                    for i in range(NQT):
                        s = i % 2
                        for sub in range(4):
                            c0 = i * 512 + sub * 128
                            g1 = P.op("pe", lambda e, r=r, c0=c0, sub=sub: e.matmul(
                                B[5][:, sub * 32:(sub + 1) * 32], QT[r][0:64, c0:c0 + 128], kbar[r][:], start=True, stop=True),
                                deps=[k2, bank_free[5]] + proj_done, inc=(sub == 3))
                        g2 = P.op("dve", lambda e, s=s, i=i: e.tensor_tensor(
                            out=gp[s][:], in0=B[5][:, 0:128], in1=maskm[:, i * 128:(i + 1) * 128], op=ALU.add),
                            deps=[g1, const_tok])
                        bank_free[5] = g2
                        g3 = g2
                        for sub in range(4):
                            g3 = P.op("dve", lambda e, s=s, sub=sub: e.max(m8[s][:, sub, :], gp[s][:, sub * 32:(sub + 1) * 32]),
                                      deps=[g3])
                        g4 = P.op("dve", lambda e, s=s: e.tensor_scalar(
                            out=thr[s][:], in0=m8[s][:, :, 3], scalar1=-1e29, scalar2=None, op0=ALU.max), deps=[g3])
                        g5 = g4
                        for sub in range(4):
                            g5 = P.op("dve", lambda e, s=s, sub=sub: e.tensor_scalar(
                                out=TB[s][:, sub, 64:96], in0=gp[s][:, sub * 32:(sub + 1) * 32],
                                scalar1=thr[s][:, sub:sub + 1], scalar2=NEG, op0=ALU.is_lt, op1=ALU.mult),
                                deps=[g5, t_tb, TB_free[s]])
                        for sub in range(4):
                            g6 = P.op("pe", lambda e, s=s, sub=sub: e.transpose(
                                Bbf[0:96, (s * 4 + sub) * 128:(s * 4 + sub + 1) * 128], TB[s][:, sub, :], ident[:]),
                                deps=[g5, bank_free["bf"], const_tok], inc=(sub == 3))
                        g7 = P.op("act", lambda e, r=r, i=i, s=s: e.copy(QT[r][64:96, i * 512:(i + 1) * 512], Bbf[64:96, s * 512:(s + 1) * 512]),
                            deps=[g6])
                        TB_free[s] = g6
                        bank_free["bf"] = g7
                        gate_done = [g7]

            att_deps = proj_done + gate_done + [dec_rows_tok, dt_ready_all, const_tok]
            nk_rows = 64 if mem_pass else KAUG
            for r in range(2):
                hl = 2 * p + r
                for i in range(NQT):
                    u = unit
                    unit += 1
                    ob = B[3 + (u % 2)]
                    ntile = 2 if mem_pass else 4 * (i + 1)
                    qk_tok = [None] * ntile
                    ex_tok = [None] * ntile
                    pv_tok = None
                    sidx = [None] * ntile

                    def emit_qk(j):
                        nonlocal att_deps
                        sl = emit_qk.ctr % 3
                        emit_qk.ctr += 1
                        sidx[j] = sl
                        jj = j - 4 * i
                        c0 = 128 * jj if ((not mem_pass) and jj >= 0) else 0
                        t = P.op("pe", lambda e: e.matmul(
                            B[sl][:, c0:512], KT[r][0:nk_rows, j * 128:(j + 1) * 128],
                            QT[r][0:nk_rows, i * 512 + c0:(i + 1) * 512], start=True, stop=True),
                            deps=[bank_free[sl]] + att_deps)
                        att_deps = []
                        qk_tok[j] = t

                    def emit_exp(j):
                        sl = sidx[j]
                        pt = emit_exp.ctr % 4
                        emit_exp.ctr += 1
                        jj = j - 4 * i
                        diag = (not mem_pass) and jj >= 0
                        c0 = 128 * jj if diag else 0
                        bias = 0.0 if mem_pass else Dt[r][:, i * NKT + j:i * NKT + j + 1]
                        if diag:
                            ts = emit_exp.tctr % 2
                            emit_exp.tctr += 1
                            t1 = P.op("dve", lambda e: e.tensor_tensor(
                                out=TMP[ts][:, c0:512], in0=B[sl][:, c0:512], in1=negm[:, 0:512 - c0], op=ALU.add),
                                deps=[qk_tok[j], TMP_free[ts]])
                            bank_free[sl] = t1
                            t2 = P.op("act", lambda e: e.activation(
                                out=PT[pt][:, c0:512], in_=TMP[ts][:, c0:512], func=AF.Exp, bias=bias, scale=1.0),
                                deps=[t1, PT_free[pt]])
                            TMP_free[ts] = t2
                        else:
                            t2 = P.op("act", lambda e: e.activation(
                                out=PT[pt][:, :], in_=B[sl][:, :], func=AF.Exp, bias=bias, scale=1.0),
                                deps=[qk_tok[j], PT_free[pt]])
                            bank_free[sl] = t2
                        ex_tok[j] = (t2, pt, c0)

                    def emit_pv(j):
                        nonlocal pv_tok
                        t2, pt, c0 = ex_tok[j]
                        t = P.op("pe", lambda e: e.matmul(
                            ob[:, c0:512], VA[:, j, r, :], PT[pt][:, c0:512], start=(j == 0), stop=(j == ntile - 1)),
                            deps=[t2] + ([bank_free[3 + (u % 2)]] if j == 0 else []))
                        PT_free[pt] = t
                        pv_tok = t

                    emit_qk.ctr = ctrs["qk"]
                    emit_exp.ctr = ctrs["ex"]
                    emit_exp.tctr = ctrs["tx"]
                    emit_qk(0)
                    for j in range(ntile):
                        if j + 1 < ntile:
                            emit_qk(j + 1)
                        emit_exp(j)
                        emit_pv(j)
                    ctrs["qk"] = emit_qk.ctr
                    ctrs["ex"] = emit_exp.ctr
                    ctrs["tx"] = emit_exp.tctr
                    qk_last_reader[r] = pv_tok
                    os_ = u % 2
                    n1 = P.op("dve", lambda e: e.reciprocal(RR[os_][:], ob[64:128, :]), deps=[pv_tok, RR_free[os_]])
                    n2 = P.op("dve", lambda e: e.tensor_tensor(out=OS[os_][:], in0=ob[0:64, :], in1=RR[os_][:], op=ALU.mult),
                              deps=[n1, OS_free[os_]])
                    bank_free[3 + (u % 2)] = n2
                    RR_free[os_] = n2
                    to = P.dma("sp", d_o[os_], lambda e: e.dma_start(
                        out=OT[hl * 64:(hl + 1) * 64, i * 512:(i + 1) * 512], in_=OS[os_][:]), deps=[n2])
                    OS_free[os_] = to
                    out_toks.append(to)
                dt_free[r] = qk_last_reader[r]
        barrier(P)


_CONST = {}


def attn_consts(kind, hh):
    key = (kind, hh)
    if key in _CONST:
        return _CONST[key]
    c = {}
    c["ident"] = np.eye(128, dtype=np.float32)
    p = np.arange(128)[:, None]
    f = np.arange(512)[None, :]
    c["negm"] = np.where(f >= p, 0.0, NEG).astype(np.float32)
    t = np.arange(SEQ)
    kaug = np.zeros((36, SEQ), np.float32)
    kaug[t // 256, t] = 1.0
    kaug[32:34] = 1.0
    c["kaug"] = kaug
    qaug = np.zeros((36, SEQ), np.float32)
    qaug[34:36] = 1.0
    c["qaug"] = qaug
    k = np.arange(32)
    c["iq"] = (k[:, None] % 16 == np.arange(16)[None, :]).astype(np.float32)
    sel = np.zeros((32, 256), np.float32)
    for r in range(2):
        sel[r * 16:(r + 1) * 16, r * 128:(r + 1) * 128] = 1.0
    c["sel"] = sel
    if kind == "fox":
        c["tri"] = ((k[:, None] // 16 == k[None, :] // 16) & (k[:, None] < k[None, :])).astype(np.float32)
    else:
        slopes = _alibi_slopes(N_SELF)
        cdec = np.zeros((3, 32, 512), np.float32)
        for pp in range(3):
            for r in range(2):
                h = 6 * hh + 2 * pp + r
                for i in range(16):
                    cdec[pp, r * 16 + i] = -slopes[h] * (i * 512 + np.arange(512, dtype=np.float32))
        c["cdec"] = cdec
        mm = np.zeros((64, 32), np.float32)
        for s in range(64):
            qb = s // 2
            mm[s, :] = np.where(np.arange(32) < qb, 0.0, np.where(np.arange(32) == qb, 1e30, -1e30))
        c["maskm"] = np.ascontiguousarray(np.broadcast_to(mm.reshape(1, 2048), (128, 2048))).astype(np.float32)
    _CONST[key] = c
    return c


def attn_inputs(kind, hh, hT_b, memT_b, w_in, b_fg, w_mem_kv):
    SW = N_SELF * HEAD_DIM
    heads = slice(6 * hh * 64, (6 * hh + 6) * 64)
    m = dict(attn_consts(kind, hh))
    wq_self = w_in[:, 0:SW][:, heads]
    wq_mem = w_in[:, 3 * SW:3 * SW + 256][:, hh * 128:(hh + 1) * 128]
    m["wq"] = np.ascontiguousarray(np.concatenate([wq_self, wq_mem], axis=1))
    m["wk"] = np.ascontiguousarray(w_in[:, SW:2 * SW][:, heads])
    m["wv"] = np.ascontiguousarray(w_in[:, 2 * SW:3 * SW][:, heads])
    m["hT"] = hT_b
    m["memT"] = memT_b
    m["wmk"] = np.ascontiguousarray(w_mem_kv[:, 0:256][:, hh * 128:(hh + 1) * 128])
    m["wmv"] = np.ascontiguousarray(w_mem_kv[:, 256:512][:, hh * 128:(hh + 1) * 128])
    if kind == "fox":
        wfg = w_in[:, 2560:2572]
        wfgp = np.zeros((3, 1024, 16, 32), np.float32)
        bfg = np.zeros((32, 3), np.float32)
        for pp in range(3):
            for r in range(2):
                h = 6 * hh + 2 * pp + r
                for i in range(16):
                    wfgp[pp, :, i, r * 16 + i] = wfg[:, h]
                bfg[r * 16:(r + 1) * 16, pp] = b_fg[h]
        m["wfgp"] = wfgp.reshape(3, 1024, 512)
        m["bfg"] = bfg
    return m


NTOK = 4096
NTT = NTOK // 128
CAP = 640
NSLOT = N_EXPERTS * CAP
PART = 320


def barrier(P):
    keys = list(P.cnt.keys())
    for e in P.engs:
        for k in keys:
            if P.cnt[k] > 0:
                P.wait(e, (k, P.cnt[k]))


def emit_ffn(nc, P, B, Bbf, E, L):
    sfx = "_f%d" % L
    h_in = E["x_own"] if L == 0 else E["hres"]
    out = E["out"] if L == NL - 1 else E["hres"]
    emit_hT = L < NL - 1
    OTv = E["OT_pair"].rearrange("c r q (h g t) -> (c r q h g) t", h=2, g=8)
    idxm_d = E["idxm"]
    wo, ln1g, ln1b, ln2g, ln2b = E["wo"][L], E["ln1g"][L], E["ln1b"][L], E["ln2g"][L], E["ln2b"][L]
    wr, rb = E["wr"][L], E["rb"][L]
    wgu, bgu, wdn, bdn = E["wgu"][L], E["bgu"][L], E["wdn"][L], E["bdn"][L]
    ident_d, tstr_d, iota_d, ecap_d = E["ident"], E["tstr"], E["iotae"], E["ecap"]
    H1, Xs, Ys, hT_own = E["H1"], E["Xs"], E["Ys"], E["hT_own"]
    bc_reg, bc_reg2 = E["bc_reg"], E["bc_reg2"]

    with contextlib.ExitStack() as st:
        def sbp(name, shape, dt):
            return st.enter_context(nc.sbuf_tensor(name + sfx, shape, dt))

        gates = sbp("gates", [128, NTT, 4], F32)
        slots = sbp("slots", [128, NTT * 4], I32)
        identb = sbp("identb", [128, 128], BF16)
        identf = sbp("identf", [128, 128], F32)
        tstr = sbp("tstr_s", [128, 128], F32)
        onesf = sbp("onesf", [128, 128], F32)
        iotae = sbp("iotae_s", [128, 32], F32)
        ecap = sbp("ecap_s", [128, 32], F32)
        onesb = sbp("onesb", [1, 128], BF16)
        idxm = sbp("idxm_s", [128, 64], I32)

        d_c = P.dsem("const")
        P.dma("pool", d_c, lambda e: e.dma_start(out=identb[:], in_=ident_d))
        P.dma("sp", d_c, lambda e: e.dma_start(out=identf[:], in_=ident_d))
        P.dma("sp", d_c, lambda e: e.dma_start(out=tstr[:], in_=tstr_d))
        P.dma("sp", d_c, lambda e: e.dma_start(out=iotae[:], in_=iota_d))
        P.dma("sp", d_c, lambda e: e.dma_start(out=idxm[:], in_=idxm_d))
        const_tok = P.dma("sp", d_c, lambda e: e.dma_start(out=ecap[:], in_=ecap_d))
        t_ones = P.op("pool", lambda e: e.memset(onesf[:], 1.0))
        t_ones = P.op("pool", lambda e: e.memset(onesb[:], 1.0))

        with contextlib.ExitStack() as s1:
            def sb(name, shape, dt):
                return s1.enter_context(nc.sbuf_tensor(name + sfx, shape, dt))
            wo_s = sb("wo_s", [128, 8, 1024], BF16)
            g1 = sb("g1", [128, 1024], F32)
            b1 = sb("b1", [128, 1024], F32)
            wr_s = sb("wr_s", [128, 8, 32], BF16)
            rb_s = sb("rb_s", [128, 32], F32)
            mTr = [sb("mTr%d" % i, [128, 8, 512], BF16) for i in range(2)]
            htr = [sb("htr%d" % i, [128, 1024], F32) for i in range(2)]
            rt = [sb("rt%d" % i, [128, 1024], F32) for i in range(2)]
            h1r = [sb("h1r%d" % i, [128, 1024], F32) for i in range(2)]
            h1b = [sb("h1b%d" % i, [128, 1024], BF16) for i in range(3)]
            h1T = [sb("h1T%d" % i, [128, 8, 128], BF16) for i in range(2)]
            stats = [sb("stats%d" % i, [128, 2, 6], F32) for i in range(2)]
            mv = [sb("mv%d" % i, [128, 2], F32) for i in range(2)]
            rstd = [sb("rstd%d" % i, [128, 1], F32) for i in range(2)]
            Lg = [sb("Lg%d" % i, [128, 32], F32) for i in range(2)]
            m8 = [sb("m8_%d" % i, [128, 8], F32) for i in range(2)]
            i8 = [sb("i8_%d" % i, [128, 8], U32) for i in range(2)]
            i8f = [sb("i8f_%d" % i, [128, 8], F32) for i in range(2)]
            nm0 = [sb("nm0_%d" % i, [128, 1], F32) for i in range(2)]
            e4 = [sb("e4_%d" % i, [128, 4], F32) for i in range(2)]
            ssum = [sb("ssum%d" % i, [128, 1], F32) for i in range(2)]
            Mk = [sb("Mk%d" % i, [128, 32], F32) for i in range(2)]
            Srun = [sb("Srun%d" % i, [128, 32], F32) for i in range(2)]
            sbase = [sb("sbase%d" % i, [128, 32], F32) for i in range(2)]
            ovf = [sb("ovf%d" % i, [128, 32], F32) for i in range(2)]
            prod = [sb("prod%d" % i, [128, 4, 32], F32) for i in range(2)]
            slotf = [sb("slotf%d" % i, [128, 4], F32) for i in range(2)]
            zt = sb("zt", [128, 5, 1024], BF16)

            d_w1 = P.dsem("w1")
            P.dma("pool", d_w1, lambda e: e.dma_start(out=wo_s[:], in_=wo.rearrange("(kc p) t -> p kc t", p=128)))
            P.dma("pool", d_w1, lambda e: e.dma_start(out=wr_s[:], in_=wr.rearrange("(kc p) t -> p kc t", p=128)))
            P.dma("sp", d_w1, lambda e: e.dma_start(out=g1[:], in_=ln1g))
            P.dma("sp", d_w1, lambda e: e.dma_start(out=b1[:], in_=ln1b))
            w1_tok = P.dma("sp", d_w1, lambda e: e.dma_start(out=rb_s[:], in_=rb))
            d_z = P.dsem("z")
            z_tok = None
            if L == 0:
                tz = P.op("pool", lambda e: e.memset(zt[:], 0.0))
                for ex in range(N_EXPERTS):
                    z_tok = P.dma("sp", d_z, lambda e, ex=ex: e.dma_start(
                        out=Xs[ex * CAP:(ex + 1) * CAP, :].rearrange("(s p) d -> p s d", p=128), in_=zt[:]), deps=[tz])
            t_s0 = P.op("pool", lambda e: e.memset(Srun[0][:], 0.0))

            d_m = [P.dsem("m%d" % i) for i in range(2)]
            d_h = [P.dsem("h%d" % i) for i in range(2)]
            d_h1 = [P.dsem("h1_%d" % i) for i in range(2)]
            d_sc = [P.dsem("sc%d" % i) for i in range(3)]
            mT_free = [None, None]
            ht_free = [None, None]
            rt_free = [None, None]
            h1r_free = [[], []]
            h1b_free = [None] * 3
            h1T_free = [None, None]
            bfree = {i: None for i in range(7)}
            bfree["bf"] = None
            st1 = {}
            srun_tok = t_s0
            last_scatter = None
            m_tok = None

            def stage1(t):
                nonlocal m_tok
                s = t % 2
                if t % 4 == 0:
                    ms = (t // 4) % 2
                    for kc in range(8):
                        m_tok = P.dma("pool", d_m[ms], lambda e, kc=kc: e.indirect_dma_start(
                            out=mTr[ms][:, kc, :], out_offset=None, in_=OTv,
                            in_offset=bass.IndirectOffsetOnAxis(ap=idxm[:, kc * 8 + t // 4:kc * 8 + t // 4 + 1], axis=0),
                            bounds_check=bc_reg2, oob_is_err=False), deps=[mT_free[ms], const_tok])
                ms = (t // 4) % 2
                tl = t % 4
                th = P.dma("sp", d_h[s], lambda e: e.dma_start(out=htr[s][:], in_=h_in[t * 128:(t + 1) * 128, :]),
                           deps=[ht_free[s]])
                for half in range(2):
                    for kc in range(8):
                        tm = P.op("pe", lambda e: e.matmul(
                            B[half][:], mTr[ms][:, kc, tl * 128:(tl + 1) * 128], wo_s[:, kc, half * 512:(half + 1) * 512],
                            start=(kc == 0), stop=(kc == 7)), deps=[m_tok, w1_tok, bfree[half]], inc=(kc == 7))
                    r1 = P.op("dve", lambda e: e.scalar_tensor_tensor(
                        out=rt[s][:, half * 512:(half + 1) * 512], in0=htr[s][:, half * 512:(half + 1) * 512], scalar=ALPHA,
                        in1=B[half][:], op0=ALU.mult, op1=ALU.add), deps=[tm, th, rt_free[s]])
                    bfree[half] = r1
                    r2 = P.op("dve", lambda e: e.bn_stats(stats[s][:, half, :], rt[s][:, half * 512:(half + 1) * 512]), deps=[r1])
                mT_free[ms] = tm
                ht_free[s] = r1
                r3 = P.op("dve", lambda e: e.bn_aggr(mv[s][:], stats[s][:].rearrange("p a b -> p (a b)")), deps=[r2])
                r4 = P.op("dve", lambda e: e.tensor_scalar(
                    out=rstd[s][:], in0=mv[s][:, 1:2], scalar1=LN_EPS, scalar2=None, op0=ALU.add), deps=[r3])
                r4 = P.op("act", lambda e: e.sqrt(rstd[s][:], rstd[s][:]), deps=[r4])
                r4 = P.op("dve", lambda e: e.reciprocal(rstd[s][:], rstd[s][:]), deps=[r4])
                r5 = P.op("dve", lambda e: e.tensor_scalar(
                    out=rt[s][:], in0=rt[s][:], scalar1=mv[s][:, 0:1], scalar2=rstd[s][:, 0:1],
                    op0=ALU.subtract, op1=ALU.mult), deps=[r4])
                r6 = P.op("pool", lambda e: e.tensor_tensor(out=h1r[s][:], in0=rt[s][:], in1=g1[:], op=ALU.mult),
                          deps=[r5, w1_tok] + h1r_free[s])
                rt_free[s] = r6
                r7 = P.op("pool", lambda e: e.tensor_tensor(out=h1r[s][:], in0=h1r[s][:], in1=b1[:], op=ALU.add), deps=[r6])
                ts_ = P.dma("sp", d_h1[s], lambda e: e.dma_start(out=H1[t * 128:(t + 1) * 128, :], in_=h1r[s][:]), deps=[r7])
                bs = t % 3
                r8 = P.op("act", lambda e: e.copy(h1b[bs][:], h1r[s][:]), deps=[r7, h1b_free[bs]])
                h1r_free[s] = [ts_, r8]
                st1[t] = r8

            def stage2(t):
                nonlocal srun_tok, last_scatter
                s = t % 2
                bs = t % 3
                for kc in range(8):
                    t1 = P.op("pe", lambda e: e.transpose(Bbf[:, kc * 128:(kc + 1) * 128], h1b[bs][:, kc * 128:(kc + 1) * 128], identb[:]),
                              deps=[st1[t], bfree["bf"], const_tok], inc=(kc == 7))
                t2 = P.op("act", lambda e: e.copy(h1T[s][:], Bbf[:].rearrange("p (a c) -> p a c", a=8)), deps=[t1, h1T_free[s]])
                bfree["bf"] = t2
                for kc in range(8):
                    t3 = P.op("pe", lambda e: e.matmul(B[2][:, 0:32], h1T[s][:, kc, :], wr_s[:, kc, :], start=(kc == 0), stop=(kc == 7)),
                              deps=[t2, bfree[2]], inc=(kc == 7))
                h1T_free[s] = t3
                a1 = P.op("dve", lambda e: e.tensor_tensor(out=Lg[s][:], in0=B[2][:, 0:32], in1=rb_s[:], op=ALU.add), deps=[t3, w1_tok])
                bfree[2] = a1
                a2 = P.op("dve", lambda e: e.max(m8[s][:], Lg[s][:]), deps=[a1])
                a3 = P.op("dve", lambda e: e.max_index(i8[s][:], m8[s][:], Lg[s][:]), deps=[a2])
                a4 = P.op("dve", lambda e: e.tensor_copy(i8f[s][:], i8[s][:]), deps=[a3])
                a5 = P.op("dve", lambda e: e.tensor_scalar(out=nm0[s][:], in0=m8[s][:, 0:1], scalar1=-1.0, scalar2=None, op0=ALU.mult), deps=[a4])
                a6 = P.op("act", lambda e: e.activation(out=e4[s][:], in_=m8[s][:, 0:4], func=AF.Exp, bias=nm0[s][:, 0:1], scale=1.0),
                          deps=[a5])
                a7 = P.op("dve", lambda e: e.tensor_reduce(out=ssum[s][:], in_=e4[s][:], axis=AX.X, op=ALU.add), deps=[a6])
                a8 = P.op("dve", lambda e: e.reciprocal(ssum[s][:], ssum[s][:]), deps=[a7])
                a9 = P.op("dve", lambda e: e.tensor_scalar(out=gates[:, t, :], in0=e4[s][:], scalar1=ssum[s][:, 0:1], scalar2=None, op0=ALU.mult), deps=[a8])
                a10 = P.op("dve", lambda e: e.tensor_scalar(out=Mk[s][:], in0=Lg[s][:], scalar1=m8[s][:, 3:4], scalar2=None, op0=ALU.is_ge), deps=[a9])
                p1 = P.op("pe", lambda e: e.matmul(B[3][:, 0:32], tstr[:], Mk[s][:], start=True, stop=False), deps=[a10, bfree[3], const_tok], inc=False)
                p2 = P.op("pe", lambda e: e.matmul(B[3][:, 0:32], onesf[:], Srun[s][:], start=False, stop=True), deps=[srun_tok, t_ones])
                srun_tok = P.op("pool", lambda e: e.tensor_tensor(out=Srun[1 - s][:], in0=Srun[s][:], in1=Mk[s][:], op=ALU.add), deps=[a10, p2, srun_tok])
                q1 = P.op("dve", lambda e: e.tensor_tensor(out=sbase[s][:], in0=B[3][:, 0:32], in1=ecap[:], op=ALU.add), deps=[p2, const_tok])
                q2 = P.op("dve", lambda e: e.tensor_scalar(out=ovf[s][:], in0=B[3][:, 0:32], scalar1=CAP - 0.5, scalar2=1.0e6, op0=ALU.is_gt, op1=ALU.mult), deps=[q1])
                bfree[3] = q2
                q3 = P.op("dve", lambda e: e.tensor_tensor(out=sbase[s][:], in0=sbase[s][:], in1=ovf[s][:], op=ALU.add), deps=[q2])
                q4 = q3
                for k in range(4):
                    q4 = P.op("dve", lambda e: e.scalar_tensor_tensor(
                        out=prod[s][:, k, :], in0=iotae[:], scalar=i8f[s][:, k:k + 1], in1=sbase[s][:], op0=ALU.is_equal, op1=ALU.mult),
                        deps=[q4, const_tok])
                q5 = P.op("dve", lambda e: e.tensor_reduce(out=slotf[s][:], in_=prod[s][:], axis=AX.X, op=ALU.add), deps=[q4])
                q6 = P.op("dve", lambda e: e.tensor_copy(slots[:, t * 4:t * 4 + 4], slotf[s][:]), deps=[q5])
                for k in range(4):
                    last_scatter = P.dma("pool", d_sc[bs], lambda e: e.indirect_dma_start(
                        out=Xs, out_offset=bass.IndirectOffsetOnAxis(ap=slots[:, t * 4 + k:t * 4 + k + 1], axis=0),
                        in_=h1b[bs][:, :], in_offset=None, bounds_check=bc_reg, oob_is_err=False),
                        deps=[q6, z_tok])
                h1b_free[bs] = last_scatter

            for t in range(NTT + 1):
                if t < NTT:
                    stage1(t)
                if t >= 1:
                    stage2(t - 1)
            barrier(P)

        with contextlib.ExitStack() as s2:
            def sb(name, shape, dt):
                return s2.enter_context(nc.sbuf_tensor(name + sfx, shape, dt))
            xin = [sb("xin%d" % i, [128, 5, 1024], BF16) for i in range(2)]
            XeT = [sb("XeT%d" % i, [128, 8, CAP], BF16) for i in range(2)]
            NW = 5
            wring = [sb("wring%d" % i, [128, 8, 512], BF16) for i in range(NW)]
            AT = sb("AT", [128, 8, CAP], BF16)
            gs = [sb("gs%d" % i, [128, PART], F32) for i in range(2)]
            sg = [sb("sg%d" % i, [128, PART], F32) for i in range(2)]
            u1 = [sb("u1_%d" % i, [128, PART], F32) for i in range(2)]
            tt = [sb("tt%d" % i, [128, PART], F32) for i in range(2)]
            Yt = [sb("Yt%d" % i, [128, 512], F32) for i in range(3)]
            bgu_s = sb("bgu_s", [N_EXPERTS, 2048], F32)
            bguT = sb("bguT", [128, 16, N_EXPERTS], F32)
            bdn_r = [sb("bdn%d" % i, [1, 1024], BF16) for i in range(2)]

            d_b = P.dsem("bias")
            tb = P.dma("sp", d_b, lambda e: e.dma_start(out=bgu_s[:], in_=bgu))
            for c in range(16):
                x1 = P.op("pe", lambda e, c=c: e.transpose(B[6][:, 0:32], bgu_s[:, c * 128:(c + 1) * 128], identf[0:32, 0:32]),
                          deps=[tb, const_tok] + ([x2] if c > 0 else []))
                x2 = P.op("dve", lambda e, c=c: e.tensor_copy(bguT[:, c, :], B[6][:, 0:32]), deps=[x1])
            x3 = P.op("dve", lambda e: e.tensor_scalar(out=bguT[:, 8:16, :], in0=bguT[:, 8:16, :], scalar1=1.0, scalar2=None, op0=ALU.add), deps=[x2])
            bias_tok = x3

            d_x = [P.dsem("x%d" % i) for i in range(2)]
            d_wr = [P.dsem("wr%d" % i) for i in range(NW)]
            d_bd = [P.dsem("bd%d" % i) for i in range(2)]
            d_y = [P.dsem("y%d" % i) for i in range(3)]
            xin_free = [None, None]
            XeT_free = [None, None]
            wr_free = [None] * NW
            bd_free = [None, None]
            Yt_free = [None] * 3
            AT_free = None
            bfree = {i: None for i in range(7)}
            bfree["bf"] = None
            act_free = [None, None]
            wctr = 0
            yctr = 0
            actr = 0
            y_toks = []

            pieces = []
            for ex in range(N_EXPERTS):
                for q in range(4):
                    pieces.append((ex, "gu", q))
                for hf in range(2):
                    pieces.append((ex, "dn", hf))
            piece_tok = {}
            issued = 0

            def issue_piece():
                nonlocal issued
                if issued >= len(pieces):
                    return
                ex, kd, q = pieces[issued]
                sl = issued % NW
                if kd == "gu":
                    P.dma("pool", d_wr[sl], lambda e: e.dma_start(
                        out=wring[sl][:, :, 0:256], in_=wgu[ex][:, q * 256:(q + 1) * 256].rearrange("(kc p) c -> p kc c", p=128)),
                        deps=[wr_free[sl]])
                    tk = P.dma("pool", d_wr[sl], lambda e: e.dma_start(
                        out=wring[sl][:, :, 256:512], in_=wgu[ex][:, 1024 + q * 256:1024 + (q + 1) * 256].rearrange("(kc p) c -> p kc c", p=128)))
                else:
                    tk = P.dma("pool", d_wr[sl], lambda e: e.dma_start(
                        out=wring[sl][:, :, :], in_=wdn[ex][:, q * 512:(q + 1) * 512].rearrange("(kc p) c -> p kc c", p=128)),
                        deps=[wr_free[sl]])
                piece_tok[(ex, kd, q)] = (tk, sl)
                issued += 1

            for _ in range(NW - 1):
                issue_piece()

            x_tok = {}

            def load_x(ex):
                xs = ex % 2
                x_tok[ex] = P.dma("sp", d_x[xs], lambda e: e.dma_start(
                    out=xin[xs][:], in_=Xs[ex * CAP:(ex + 1) * CAP, :].rearrange("(s p) d -> p s d", p=128)),
                    deps=[xin_free[xs]])

            load_x(0)
            for ex in range(N_EXPERTS):
                xs = ex % 2
                if ex + 1 < N_EXPERTS:
                    load_x(ex + 1)
                tbd = P.dma("pool", d_bd[xs], lambda e: e.dma_start(out=bdn_r[xs][:], in_=bdn[ex:ex + 1, :]), deps=[bd_free[xs]])
                for sidx in range(5):
                    for kc in range(8):
                        t1 = P.op("pe", lambda e: e.transpose(Bbf[:, kc * 128:(kc + 1) * 128], xin[xs][:, sidx, kc * 128:(kc + 1) * 128], identb[:]),
                                  deps=[x_tok[ex], bfree["bf"]], inc=(kc == 7))
                    t2 = P.op("act", lambda e: e.copy(XeT[xs][:, :, sidx * 128:(sidx + 1) * 128], Bbf[:].rearrange("p (a c) -> p a c", a=8)),
                              deps=[t1, XeT_free[xs]])
                    bfree["bf"] = t2
                xin_free[xs] = t1
                xet_tok = t2
                last_pool = None
                for q in range(4):
                    issue_piece()
                    wt, sl = piece_tok[(ex, "gu", q)]
                    for cl in range(2):
                        c = 2 * q + cl
                        for part in range(2):
                            cs = slice(part * PART, (part + 1) * PART)
                            for kc in range(8):
                                tg = P.op("pe", lambda e: e.matmul(B[part][:, 0:PART], wring[sl][:, kc, cl * 128:(cl + 1) * 128], XeT[xs][:, kc, cs],
                                                                   start=(kc == 0), stop=(kc == 7)), deps=[wt, xet_tok, bfree[part]], inc=(kc == 7))
                            for kc in range(8):
                                tu = P.op("pe", lambda e: e.matmul(B[2 + part][:, 0:PART], wring[sl][:, kc, 256 + cl * 128:256 + (cl + 1) * 128], XeT[xs][:, kc, cs],
                                                                   start=(kc == 0), stop=(kc == 7)), deps=[bfree[2 + part]], inc=(kc == 7))
                            a = actr % 2
                            actr += 1
                            v1 = P.op("dve", lambda e: e.tensor_scalar(out=gs[a][:], in0=B[part][:, 0:PART], scalar1=bguT[:, c, ex:ex + 1], scalar2=SWIGLU_LIMIT,
                                                                       op0=ALU.add, op1=ALU.min), deps=[tg, bias_tok, act_free[a]])
                            bfree[part] = v1
                            v2 = P.op("act", lambda e: e.activation(out=sg[a][:], in_=gs[a][:], func=AF.Sigmoid, scale=SWIGLU_ALPHA), deps=[v1])
                            v3 = P.op("dve", lambda e: e.tensor_scalar(out=u1[a][:], in0=B[2 + part][:, 0:PART], scalar1=bguT[:, 8 + c, ex:ex + 1], scalar2=SWIGLU_LIMIT + 1.0,
                                                                       op0=ALU.add, op1=ALU.min), deps=[tu])
                            bfree[2 + part] = v3
                            v4a = P.op("pool", lambda e: e.tensor_scalar(out=u1[a][:], in0=u1[a][:], scalar1=-SWIGLU_LIMIT + 1.0, scalar2=None, op0=ALU.max), deps=[v3])
                            v4 = P.op("pool", lambda e: e.tensor_tensor(out=tt[a][:], in0=u1[a][:], in1=gs[a][:], op=ALU.mult), deps=[v4a, v1])
                            v5 = P.op("pool", lambda e: e.tensor_tensor(out=AT[:, c, cs], in0=tt[a][:], in1=sg[a][:], op=ALU.mult), deps=[v2, v4, AT_free])
                            act_free[a] = v5
                            last_pool = v5
                    wr_free[sl] = tu
                XeT_free[xs] = tu
                for hf in range(2):
                    issue_piece()
                    wt, sl = piece_tok[(ex, "dn", hf)]
                    for sidx in range(5):
                        yb = 4 + (yctr % 2)
                        for kc in range(8):
                            td = P.op("pe", lambda e: e.matmul(B[yb][:], AT[:, kc, sidx * 128:(sidx + 1) * 128], wring[sl][:, kc, :],
                                                               start=(kc == 0), stop=False), deps=[wt, last_pool, bfree[yb]], inc=False)
                        td = P.op("pe", lambda e: e.matmul(B[yb][:], onesb[0:1, :], bdn_r[xs][0:1, hf * 512:(hf + 1) * 512], start=False, stop=True),
                                  deps=[tbd, t_ones])
                        ys = yctr % 3
                        yctr += 1
                        w1 = P.op("act", lambda e: e.copy(Yt[ys][:], B[yb][:]), deps=[td, Yt_free[ys]])
                        bfree[yb] = w1
                        ty = P.dma("sp", d_y[ys], lambda e: e.dma_start(
                            out=Ys[ex * CAP + sidx * 128:ex * CAP + (sidx + 1) * 128, hf * 512:(hf + 1) * 512], in_=Yt[ys][:]), deps=[w1])
                        Yt_free[ys] = ty
                        y_toks.append(ty)
                    wr_free[sl] = td
                AT_free = td
                bd_free[xs] = td
            barrier(P)

        with contextlib.ExitStack() as s3:
            def sb(name, shape, dt):
                return s3.enter_context(nc.sbuf_tensor(name + sfx, shape, dt))
            g2 = sb("g2", [128, 1024], F32)
            b2 = sb("b2", [128, 1024], F32)
            Yg = [sb("Yg%d" % i, [128, 4, 1024], F32) for i in range(2)]
            h1t = [sb("h1t%d" % i, [128, 1024], F32) for i in range(2)]
            acc = [sb("acc%d" % i, [128, 1024], F32) for i in range(2)]
            ot = [sb("ot%d" % i, [128, 1024], F32) for i in range(2)]
            stats = [sb("stats3_%d" % i, [128, 2, 6], F32) for i in range(2)]
            mv = [sb("mv3_%d" % i, [128, 2], F32) for i in range(2)]
            rstd = [sb("rstd3_%d" % i, [128, 1], F32) for i in range(2)]
            d_w3 = P.dsem("w3")
            P.dma("sp", d_w3, lambda e: e.dma_start(out=g2[:], in_=ln2g))
            w3_tok = P.dma("sp", d_w3, lambda e: e.dma_start(out=b2[:], in_=ln2b))
            d_g = [P.dsem("g%d" % i) for i in range(2)]
            d_h3 = [P.dsem("h3_%d" % i) for i in range(2)]
            d_out = [P.dsem("out%d" % i) for i in range(2)]
            for s in range(2):
                P.op("pool", lambda e, s=s: e.memset(Yg[s][:], 0.0))
            Yg_free = [None, None]
            h1t_free = [None, None]
            ot_free = [[], []]
            outs = []
            gt = {}
            hst = hT_state(nc, P, s3, sfx) if emit_hT else None

            def gather(t):
                s = t % 2
                for k in range(4):
                    gt[t] = P.dma("pool", d_g[s], lambda e: e.indirect_dma_start(
                        out=Yg[s][:, k, :], out_offset=None, in_=Ys,
                        in_offset=bass.IndirectOffsetOnAxis(ap=slots[:, t * 4 + k:t * 4 + k + 1], axis=0),
                        bounds_check=bc_reg, oob_is_err=False), deps=[Yg_free[s]])

            gather(0)
            for t in range(NTT):
                s = t % 2
                if t + 1 < NTT:
                    gather(t + 1)
                th = P.dma("sp", d_h3[s], lambda e: e.dma_start(out=h1t[s][:], in_=H1[t * 128:(t + 1) * 128, :]), deps=[h1t_free[s]])
                c1 = P.op("dve", lambda e: e.tensor_scalar(out=acc[s][:], in0=Yg[s][:, 0, :], scalar1=gates[:, t, 0:1], scalar2=None, op0=ALU.mult),
                          deps=[gt[t]])
                c2 = P.op("dve", lambda e: e.scalar_tensor_tensor(out=acc[s][:], in0=Yg[s][:, 1, :], scalar=gates[:, t, 1:2], in1=acc[s][:],
                                                                   op0=ALU.mult, op1=ALU.add), deps=[c1, gt[t]])
                c3 = P.op("dve", lambda e: e.scalar_tensor_tensor(out=acc[s][:], in0=Yg[s][:, 2, :], scalar=gates[:, t, 2:3], in1=acc[s][:],
                                                                  op0=ALU.mult, op1=ALU.add), deps=[c2])
                c4 = P.op("dve", lambda e: e.scalar_tensor_tensor(out=acc[s][:], in0=Yg[s][:, 3, :], scalar=gates[:, t, 3:4], in1=acc[s][:],
                                                                   op0=ALU.mult, op1=ALU.add), deps=[c3])
                Yg_free[s] = c4
                c5 = P.op("dve", lambda e: e.scalar_tensor_tensor(out=acc[s][:], in0=h1t[s][:], scalar=ALPHA, in1=acc[s][:],
                                                                  op0=ALU.mult, op1=ALU.add), deps=[c4, th])
                h1t_free[s] = c5
                for half in range(2):
                    c6 = P.op("dve", lambda e: e.bn_stats(stats[s][:, half, :], acc[s][:, half * 512:(half + 1) * 512]), deps=[c5])
                c7 = P.op("dve", lambda e: e.bn_aggr(mv[s][:], stats[s][:].rearrange("p a b -> p (a b)")), deps=[c6])
                c8 = P.op("dve", lambda e: e.tensor_scalar(out=rstd[s][:], in0=mv[s][:, 1:2], scalar1=LN_EPS, scalar2=None, op0=ALU.add), deps=[c7])
                c8 = P.op("act", lambda e: e.sqrt(rstd[s][:], rstd[s][:]), deps=[c8])
                c8 = P.op("dve", lambda e: e.reciprocal(rstd[s][:], rstd[s][:]), deps=[c8])
                c9 = P.op("dve", lambda e: e.tensor_scalar(out=acc[s][:], in0=acc[s][:], scalar1=mv[s][:, 0:1], scalar2=rstd[s][:, 0:1],
                                                           op0=ALU.subtract, op1=ALU.mult), deps=[c8])
                c10 = P.op("pool", lambda e: e.tensor_tensor(out=ot[s][:], in0=acc[s][:], in1=g2[:], op=ALU.mult), deps=[c9, w3_tok] + ot_free[s])
                c11 = P.op("pool", lambda e: e.tensor_tensor(out=ot[s][:], in0=ot[s][:], in1=b2[:], op=ALU.add), deps=[c10])
                to = P.dma("sp", d_out[s], lambda e: e.dma_start(out=out[t * 128:(t + 1) * 128, :], in_=ot[s][:]), deps=[c11])
                ot_free[s] = [to]
                outs.append(to)
                if emit_hT:
                    x3 = hT_tile(nc, P, Bbf, identb, hst, ot[s], c11, t, hT_own, [const_tok])
                    ot_free[s].append(hst["cast_tok"])
            barrier(P)


def hT_state(nc, P, stack, sfx):
    stt = {}
    stt["obf"] = [stack.enter_context(nc.sbuf_tensor("obf%d%s" % (i, sfx), [128, 1024], BF16)) for i in range(2)]
    stt["hTs"] = [stack.enter_context(nc.sbuf_tensor("hTs%d%s" % (i, sfx), [128, 8, 512], BF16)) for i in range(2)]
    stt["obf_free"] = [None, None]
    stt["hTs_free"] = [None, None]
    stt["bf_free"] = None
    stt["d"] = [P.dsem("hts0"), P.dsem("hts1")]
    stt["cast_tok"] = None
    return stt


def hT_tile(nc, P, Bbf, identb, stt, src, src_tok, t, hT_own, extra):
    s = t % 2
    g, tl = t // 4, t % 4
    hs = g % 2
    obf, hTs = stt["obf"], stt["hTs"]
    x1 = P.op("act", lambda e: e.copy(obf[s][:], src[:]), deps=[src_tok, stt["obf_free"][s]])
    stt["cast_tok"] = x1
    for kc in range(8):
        x2 = P.op("pe", lambda e, kc=kc: e.transpose(Bbf[:, kc * 128:(kc + 1) * 128], obf[s][:, kc * 128:(kc + 1) * 128], identb[:]),
                  deps=[x1, stt["bf_free"]] + extra, inc=(kc == 7))
    stt["obf_free"][s] = x2
    x3 = P.op("dve", lambda e: e.tensor_copy(hTs[hs][:, :, tl * 128:(tl + 1) * 128], Bbf[:].rearrange("p (a c) -> p a c", a=8)),
              deps=[x2] + ([stt["hTs_free"][hs]] if tl == 0 else []))
    stt["bf_free"] = x3
    if tl == 3:
        tw = P.dma("sp", stt["d"][hs], lambda e: e.dma_start(
            out=hT_own[:, g * 512:(g + 1) * 512].rearrange("(kc p) t -> p kc t", p=128), in_=hTs[hs][:]), deps=[x3])
        stt["hTs_free"][hs] = tw
    return x3


def emit_hT0(nc, P, Bbf, E):
    with contextlib.ExitStack() as st:
        identb = st.enter_context(nc.sbuf_tensor("identb_t0", [128, 128], BF16))
        xt = [st.enter_context(nc.sbuf_tensor("xt%d_t0" % i, [128, 1024], F32)) for i in range(2)]
        d_c = P.dsem("const")
        ct = P.dma("pool", d_c, lambda e: e.dma_start(out=identb[:], in_=E["ident"]))
        stt = hT_state(nc, P, st, "_t0")
        d_x = [P.dsem("h0"), P.dsem("h1")]
        xfree = [None, None]
        for t in range(NTT):
            s = t % 2
            tx = P.dma("sp", d_x[s], lambda e: e.dma_start(out=xt[s][:], in_=E["x_own"][t * 128:(t + 1) * 128, :]), deps=[xfree[s]])
            hT_tile(nc, P, Bbf, identb, stt, xt[s], tx, t, E["hT_own"], [ct])
            xfree[s] = stt["cast_tok"]
        barrier(P)


PAIRS = [[0, 1], [2, 3], [4, 5], [6, 7]]
NL = DEPTH


def allgather(nc, P, src, dst, kind):
    barrier(P)
    key = P.dsem("cc")
    for c in range(4):
        if kind == "hT":
            s_ap = src[c * 256:(c + 1) * 256, :]
            d_ap = dst[c].rearrange("r kk p t -> (r kk p) t")
        else:
            s_ap = src[c * 128:(c + 1) * 128, :]
            d_ap = dst[c].rearrange("r q t -> (r q) t")
        P.cc(key, lambda e: e.collective_compute("AllGather", ALU.bypass, replica_groups=PAIRS, ins=[s_ap.opt()], outs=[d_ap.opt()]))
    barrier(P)


def build_fused():
    nc = bass.Bass("TRN2", target_bir_lowering=False)
    E = {}

    def din(name, shape, dt=F32):
        E[name] = nc.dram_tensor(name, shape, dt, kind="ExternalInput").ap()

    def dint(name, shape, dt):
        E[name] = nc.dram_tensor(name, shape, dt, kind="Internal").ap()

    din("x_own", [NTOK, 1024])
    din("memT", [1024, N_MEM])
    din("wmk", [1024, 128])
    din("wmv", [1024, 128])
    din("wq", [NL, 1024, 512])
    din("wk", [NL, 1024, 384])
    din("wv", [NL, 1024, 384])
    din("wfgp", [2, 3, 1024, 512])
    din("bfg", [2, 32, 3])
    din("ident", [128, 128])
    din("negm", [128, 512])
    din("kaug", [36, SEQ])
    din("qaug", [36, SEQ])
    din("iq", [32, 16])
    din("sel", [32, 256])
    din("tri", [32, 32])
    din("cdec", [3, 32, 512])
    din("maskm", [128, 2048])
    din("idxm", [128, 64], I32)
    din("wo", [NL, 1024, 1024])
    for nm in ("ln1g", "ln1b", "ln2g", "ln2b"):
        din(nm, [NL, 128, 1024])
    din("wr", [NL, 1024, 32])
    din("rb", [NL, 128, 32])
    din("wgu", [NL, N_EXPERTS, 1024, 2048])
    din("bgu", [NL, N_EXPERTS, 2048])
    din("wdn", [NL, N_EXPERTS, 1024, 1024])
    din("bdn", [NL, N_EXPERTS, 1024])
    din("tstr", [128, 128])
    din("iotae", [128, 32])
    din("ecap", [128, 32])
    E["out"] = nc.dram_tensor("out", [NTOK, 1024], F32, kind="ExternalOutput").ap()
    dint("hT_own", [1024, NTOK], BF16)
    dint("hTp", [4, 2, 2, 128, NTOK], BF16)
    dint("OT_own", [512, SEQ], BF16)
    dint("OT_pair", [4, 2, 128, SEQ], BF16)
    dint("dscr", [2, 2, 2, SEQ], BF16)
    dint("H1", [NTOK, 1024], F32)
    dint("hres", [NTOK, 1024], F32)
    dint("Xs", [NSLOT, 1024], BF16)
    dint("Ys", [NSLOT, 1024], F32)

    with contextlib.ExitStack() as st:
        P = Prog(nc, st)
        B = [st.enter_context(nc.psum_tensor("B%d" % i, [128, 512], F32)) for i in range(7)]
        Bbf = st.enter_context(nc.psum_tensor("Bbf", [128, 1024], BF16))
        E["bc_reg"] = nc.gpsimd.to_reg(NSLOT - 1)
        E["bc_reg2"] = nc.gpsimd.to_reg(16383)
        emit_hT0(nc, P, Bbf, E)
        allgather(nc, P, E["hT_own"], E["hTp"], "hT")
        for L in range(NL):
            emit_attn(nc, P, B, Bbf, "moba" if L % 2 == 0 else "fox", E, L)
            allgather(nc, P, E["OT_own"], E["OT_pair"], "OT")
            emit_ffn(nc, P, B, Bbf, E, L)
            if L < NL - 1:
                allgather(nc, P, E["hT_own"], E["hTp"], "hT")
                P.new_epoch()
        barrier(P)
    return nc


def ffn_consts():
    if "ffn" in _CONST:
        return _CONST["ffn"]
    c = {}
    c["ident"] = np.eye(128, dtype=np.float32)
    k = np.arange(128)
    c["tstr"] = (k[:, None] < k[None, :]).astype(np.float32)
    c["iotae"] = np.ascontiguousarray(np.broadcast_to(np.arange(32, dtype=np.float32)[None, :], (128, 32)))
    c["ecap"] = np.ascontiguousarray(np.broadcast_to((np.arange(32, dtype=np.float32) * CAP)[None, :], (128, 32)))
    _CONST["ffn"] = c
    return c


def bc128(v):
    return np.ascontiguousarray(np.broadcast_to(np.asarray(v, np.float32).reshape(1, -1), (128, v.size)))


def ffn_inputs(h_tok, mT_tok, wo_l, ln1g, ln1b, ln2g, ln2b, wr_l, rb_l, wgu_l, bgu_l, wdn_l, bdn_l):
    m = dict(ffn_consts())
    m.update(h=h_tok, mT=mT_tok, wo=wo_l, ln1g=bc128(ln1g), ln1b=bc128(ln1b), ln2g=bc128(ln2g), ln2b=bc128(ln2b),
             wr=wr_l, rb=bc128(rb_l), wgu=wgu_l, bgu=bgu_l, wdn=wdn_l, bdn=bdn_l)
    return m


_FUSED = {}


def kernel(x, mem, w_in_moba, w_in_fox, b_fgate, w_mem_kv, w_o, ln1_g, ln1_b, router_w, router_b,
           w_gate_up, b_gate_up, w_down, b_down, ln2_g, ln2_b):
    f32 = lambda a: np.ascontiguousarray(np.asarray(a, dtype=np.float32))
    x, mem, w_mem_kv = f32(x), f32(mem), f32(w_mem_kv)
    w_in_moba, w_in_fox, b_fgate, w_o = f32(w_in_moba), f32(w_in_fox), f32(b_fgate), f32(w_o)
    SW = N_SELF * HEAD_DIM
    shared = dict(ffn_consts())
    ca = attn_consts("moba", 0)
    for k in ("ident", "negm", "kaug", "qaug", "iq", "sel", "maskm"):
        shared[k] = ca[k]
    shared["tri"] = attn_consts("fox", 0)["tri"]
    shared["wr"] = f32(router_w)[:NL]
    shared["rb"] = np.stack([bc128(f32(router_b)[l]) for l in range(NL)])
    for nm, v in (("ln1g", ln1_g), ("ln1b", ln1_b), ("ln2g", ln2_g), ("ln2b", ln2_b)):
        shared[nm] = np.stack([bc128(f32(v)[l]) for l in range(NL)])
    shared["wgu"], shared["bgu"] = f32(w_gate_up)[:NL], f32(b_gate_up)[:NL]
    shared["wdn"], shared["bdn"] = f32(w_down)[:NL], f32(b_down)[:NL]
    perm = np.zeros(1024, np.int64)
    for hp in range(4):
        for r in range(2):
            for q in range(128):
                hl, dd = 2 * hp + q // 64, q % 64
                g = (6 * r + hl) * 64 + dd if hl < 6 else 768 + (2 * r + hl - 6) * 64 + dd
                perm[(hp * 2 + r) * 128 + q] = g
    shared["wo"] = np.ascontiguousarray(w_o[:NL][:, perm, :])
    per_half = []
    for hh in range(2):
        d = {}
        heads = slice(6 * hh * 64, (6 * hh + 6) * 64)
        wq, wk, wv = [], [], []
        for l in range(NL):
            w_in = w_in_moba[l // 2] if l % 2 == 0 else w_in_fox[l // 2]
            wq.append(np.concatenate([w_in[:, 0:SW][:, heads], w_in[:, 3 * SW:3 * SW + 256][:, hh * 128:(hh + 1) * 128]], axis=1))
            wk.append(w_in[:, SW:2 * SW][:, heads])
            wv.append(w_in[:, 2 * SW:3 * SW][:, heads])
        d["wq"], d["wk"], d["wv"] = np.ascontiguousarray(np.stack(wq)), np.ascontiguousarray(np.stack(wk)), np.ascontiguousarray(np.stack(wv))
        wfgp = np.zeros((2, 3, 1024, 16, 32), np.float32)
        bfg = np.zeros((2, 32, 3), np.float32)
        for j in range(2):
            wfg = w_in_fox[j][:, 2560:2572]
            for pp in range(3):
                for r in range(2):
                    h = 6 * hh + 2 * pp + r
                    for i in range(16):
                        wfgp[j, pp, :, i, r * 16 + i] = wfg[:, h]
                    bfg[j, r * 16:(r + 1) * 16, pp] = b_fgate[j, h]
        d["wfgp"] = wfgp.reshape(2, 3, 1024, 512)
        d["bfg"] = bfg
        d["cdec"] = attn_consts("moba", hh)["cdec"]
        d["wmk"] = np.ascontiguousarray(w_mem_kv[:, 0:256][:, hh * 128:(hh + 1) * 128])
        d["wmv"] = np.ascontiguousarray(w_mem_kv[:, 256:512][:, hh * 128:(hh + 1) * 128])
        kc = np.arange(8)[None, :, None]
        p = np.arange(128)[:, None, None]
        g = np.arange(8)[None, None, :]
        d["idxm"] = np.ascontiguousarray((((kc * 128 + p) * 2 + hh) * 8 + g).reshape(128, 64).astype(np.int32))
        per_half.append(d)
    maps = []
    for c in range(8):
        b, hh = c // 2, c % 2
        m = dict(shared)
        m.update(per_half[hh])
        m["x_own"] = np.ascontiguousarray(x[b, hh * NTOK:(hh + 1) * NTOK])
        m["memT"] = np.ascontiguousarray(mem[b].T)
        maps.append(m)
    if NL not in _FUSED:
        _FUSED[NL] = build_fused()
    res = run_bass_kernel_spmd(_FUSED[NL], maps, core_ids=list(range(8))).results
    out = np.empty((BATCH, SEQ, D_MODEL), np.float32)
    for c in range(8):
        b, hh = c // 2, c % 2
        out[b, hh * NTOK:(hh + 1) * NTOK] = np.asarray(res[c]["out"])
    return out
```

```python
import contextlib
import math
import numpy as np
import ml_dtypes
import concourse.bass as bass
import concourse.mybir as mybir
from concourse.bass_utils import run_bass_kernel_spmd

F32 = mybir.dt.float32
BF16 = mybir.dt.bfloat16
I32 = mybir.dt.int32
U32 = mybir.dt.uint32
ALU = mybir.AluOpType
AF = mybir.ActivationFunctionType
AX = mybir.AxisListType

D_MODEL = 1024
BATCH = 4
SEQ = 8192
DEPTH = 4
HEAD_DIM = 64
N_SELF = 12
N_MEMH = 4
N_MEM = 256
N_EXPERTS = 32
TOP_K = 4
D_FF = 1024
SWIGLU_LIMIT = 7.0
SWIGLU_ALPHA = 1.702
ALPHA = (2 * DEPTH) ** 0.25
LN_EPS = 1e-5
NEG = -30000.0
KAUG = 100
NQT = SEQ // 512
NKT = SEQ // 128


class Prog:
    def __init__(self, nc, stack):
        self.nc = nc
        self.stack = stack
        self.engs = {"pe": nc.tensor, "act": nc.scalar, "dve": nc.vector, "pool": nc.gpsimd, "sp": nc.sync}
        self.sem = {}
        self.cnt = {}
        self.seen = {k: {} for k in self.engs}
        for k in self.engs:
            self.sem[k] = stack.enter_context(nc.semaphore("s_" + k))
            self.cnt[k] = 0
        self.ndsem = 0

    def dsem(self, name=None):
        if name is not None and ("dn_" + name) in self.sem:
            return "dn_" + name
        self.ndsem += 1
        key = "dn_" + name if name is not None else "d%d" % self.ndsem
        self.sem[key] = self.stack.enter_context(self.nc.semaphore(key))
        self.cnt[key] = 0
        return key

    def wait(self, eng, tok):
        if tok is None:
            return
        key, val = tok
        if eng == "pe" and key == "pe":
            return
        if self.seen[eng].get(key, 0) >= val:
            return
        self.engs[eng].wait_ge(self.sem[key], val)
        self.seen[eng][key] = val

    def op(self, eng, fn, deps=(), inc=True):
        for d in deps:
            self.wait(eng, d)
        inst = fn(self.engs[eng])
        if inc:
            self.cnt[eng] += 1
            inst.then_inc(self.sem[eng], 1)
            return (eng, self.cnt[eng])
        return None

    def new_epoch(self):
        self.epoch = getattr(self, "epoch", 0) + 1
        for k in self.engs:
            self.sem[k] = self.stack.enter_context(self.nc.semaphore("s_%s_e%d" % (k, self.epoch)))
            self.cnt[k] = 0
            for e in self.engs:
                self.seen[e].pop(k, None)

    def cc(self, dkey, fn, deps=()):
        for d in deps:
            self.wait("pool", d)
        inst = fn(self.engs["pool"])
        self.cnt[dkey] += 1
        inst.then_inc(self.sem[dkey])
        return (dkey, self.cnt[dkey])

    def dma(self, q, dkey, fn, deps=()):
        for d in deps:
            self.wait(q, d)
        inst = fn(self.engs[q])
        self.cnt[dkey] += 16
        inst.then_inc(self.sem[dkey], 16)
        return (dkey, self.cnt[dkey])


def _alibi_slopes(n):
    def pow2(m):
        start = 2.0 ** (-(2.0 ** -(math.log2(m) - 3)))
        return [start * start ** i for i in range(m)]
    if math.log2(n).is_integer():
        s = pow2(n)
    else:
        c = 2 ** math.floor(math.log2(n))
        s = pow2(c) + pow2(2 * c)[0::2][: n - c]
    return np.array(s, dtype=np.float32)


def emit_attn(nc, P, B, Bbf, kind, E, L):
    fox = kind == "fox"
    j = L // 2
    sfx = "_a%d" % L
    hTp = E["hTp"]
    wq, wk, wv = E["wq"][L], E["wk"][L], E["wv"][L]
    memT, wmk, wmv = E["memT"], E["wmk"], E["wmv"]
    ident_d, negm_d, kaug_d, qaug_d, iq_d, sel_d = E["ident"], E["negm"], E["kaug"], E["qaug"], E["iq"], E["sel"]
    if fox:
        wfgp, bfg, tri_d = E["wfgp"][j], E["bfg"][j], E["tri"]
    else:
        cdec, maskm_d = E["cdec"], E["maskm"]
    OT, dscr = E["OT_own"], E["dscr"]

    with contextlib.ExitStack() as st:

        def sb(name, shape, dt):
            return st.enter_context(nc.sbuf_tensor(name + sfx, shape, dt))

        QT = [sb("QT%d" % r, [KAUG, SEQ], BF16) for r in range(2)]
        KT = [sb("KT%d" % r, [KAUG, SEQ], BF16) for r in range(2)]
        VA = sb("VA", [128, NKT, 2, 128], BF16)
        hTr = [sb("hTr%d" % s, [128, 8, 512], BF16) for s in range(2)]
        wq_s = sb("wq_s", [128, 8, 128], BF16)
        wk_s = sb("wk_s", [128, 8, 128], BF16)
        wv_s = sb("wv_s", [128, 8, 128], BF16)
        memT_s = sb("memT_s", [128, 8, N_MEM], BF16)
        wmk_s = sb("wmk_s", [128, 8, 128], BF16)
        wmv_s = sb("wmv_s", [128, 8, 128], BF16)
        PT = [sb("PT%d" % s, [128, 512], BF16) for s in range(4)]
        TMP = [sb("TMP%d" % s, [128, 512], F32) for s in range(2)]
        Dt = [sb("Dt%d" % r, [128, NQT * NKT], F32) for r in range(2)]
        negm = sb("negm_s", [128, 512], F32)
        ident = sb("ident_s", [128, 128], BF16)
        OS = [sb("OS%d" % s, [64, 512], BF16) for s in range(2)]
        RR = [sb("RR%d" % s, [64, 512], F32) for s in range(2)]
        iq = sb("iq_s", [32, 16], F32)
        sel = sb("sel_s", [32, 256], F32)
        Cc = sb("Cc", [32, 512], F32)
        Z1 = sb("Z1", [32, 512], F32)
        RQ = sb("RQ", [32, 2, 512], BF16)
        RK = sb("RK", [32, 2, 512], BF16)
        refs = sb("refs", [32, 80], F32)
        bq_s = sb("bq_s", [128, 16], F32)
        if fox:
            wfg_s = sb("wfg_s", [128, 8, 512], BF16)
            bfg_s = sb("bfg_s", [32, 3], F32)
            tri = sb("tri_s", [32, 32], F32)
            ones32 = sb("ones32", [32, 512], F32)
            offs = sb("offs", [32, 1], F32)
        else:
            maskm = sb("maskm_s", [128, 2048], F32)
            kbar = [sb("kbar%d" % r, [64, 32], BF16) for r in range(2)]
            kbf = sb("kbf", [64, 32], F32)
            gp = [sb("gp%d" % s, [128, 128], F32) for s in range(2)]
            m8 = [sb("m8_%d" % s, [128, 4, 8], F32) for s in range(2)]
            thr = [sb("thr%d" % s, [128, 4], F32) for s in range(2)]
            TB = [sb("TB%d" % s, [128, 4, 96], BF16) for s in range(2)]
        d_c = P.dsem("const")
        toks = []
        toks.append(P.dma("pool", d_c, lambda e: e.dma_start(out=ident[:], in_=ident_d)))
        toks.append(P.dma("sp", d_c, lambda e: e.dma_start(out=negm[:], in_=negm_d)))
        toks.append(P.dma("sp", d_c, lambda e: e.dma_start(out=iq[:], in_=iq_d)))
        toks.append(P.dma("sp", d_c, lambda e: e.dma_start(out=sel[:], in_=sel_d)))
        for r in range(2):
            toks.append(P.dma("pool", d_c, lambda e, r=r: e.dma_start(out=KT[r][64:100, :], in_=kaug_d)))
            toks.append(P.dma("pool", d_c, lambda e, r=r: e.dma_start(out=QT[r][64:100, :], in_=qaug_d)))
        toks.append(P.dma("pool", d_c, lambda e: e.dma_start(
            out=memT_s[:], in_=memT.rearrange("(kc p) t -> p kc t", p=128))))
        toks.append(P.dma("pool", d_c, lambda e: e.dma_start(
            out=wmk_s[:], in_=wmk.rearrange("(kc p) t -> p kc t", p=128))))
        toks.append(P.dma("pool", d_c, lambda e: e.dma_start(
            out=wmv_s[:], in_=wmv.rearrange("(kc p) t -> p kc t", p=128))))
        if fox:
            toks.append(P.dma("sp", d_c, lambda e: e.dma_start(out=bfg_s[:], in_=bfg)))
            toks.append(P.dma("sp", d_c, lambda e: e.dma_start(out=tri[:], in_=tri_d)))
        else:
            toks.append(P.dma("sp", d_c, lambda e: e.dma_start(out=maskm[:], in_=maskm_d)))
        const_tok = toks[-1]
        t_va = P.op("pool", lambda e: e.memset(VA[:], 1.0))
        if fox:
            t_ones = P.op("pool", lambda e: e.memset(ones32[:], 1.0))
        else:
            for s in range(2):
                t_tb = P.op("pool", lambda e, s=s: e.memset(TB[s][:], 0.0))

        d_h = [P.dsem("h%d" % s) for s in range(2)]
        d_w = P.dsem("w")
        d_o = [P.dsem("o%d" % s) for s in range(2)]
        d_dec = P.dsem("dec")
        d_dec2 = P.dsem("dec2")

        hT_free = [None, None]
        w_free = None
        bank_free = {i: None for i in range(7)}
        bank_free["bf"] = None
        PT_free = [None] * 4
        TMP_free = [None] * 2
        OS_free = [None] * 2
        RR_free = [None] * 2
        qk_last_reader = [None, None]
        unit = 0
        ctrs = {"qk": 0, "ex": 0, "tx": 0}
        TB_free = [None, None]
        out_toks = []
        dt_free = [None, None]

        for p in range(4):
            mem_pass = p == 3
            wdeps = [w_free]
            tw = P.dma("pool", d_w, lambda e, p=p: e.dma_start(
                out=wq_s[:], in_=wq[:, p * 128:(p + 1) * 128].rearrange("(kc p) t -> p kc t", p=128)), deps=wdeps)
            if not mem_pass:
                tw = P.dma("pool", d_w, lambda e, p=p: e.dma_start(
                    out=wk_s[:], in_=wk[:, p * 128:(p + 1) * 128].rearrange("(kc p) t -> p kc t", p=128)))
                tw = P.dma("pool", d_w, lambda e, p=p: e.dma_start(
                    out=wv_s[:], in_=wv[:, p * 128:(p + 1) * 128].rearrange("(kc p) t -> p kc t", p=128)))
                if fox:
                    tw = P.dma("pool", d_w, lambda e, p=p: e.dma_start(
                        out=wfg_s[:], in_=wfgp[p].rearrange("(kc p) t -> p kc t", p=128)))

            last_ev = {}
            for i in range(NQT):
                s = i % 2
                for c4 in range(4):
                    th = P.dma("sp", d_h[s], lambda e, i=i, s=s, c4=c4: e.dma_start(
                        out=hTr[s][:, 2 * c4:2 * c4 + 2, :],
                        in_=hTp[c4, i // 8, :, :, (i % 8) * 512:(i % 8 + 1) * 512].rearrange("kk p t -> p kk t")),
                        deps=[hT_free[s]])
                cols = slice(i * 512, (i + 1) * 512)
                bq = B[0 + s]
                for kc in range(8):
                    tq = P.op("pe", lambda e, kc=kc, bq=bq, s=s: e.matmul(
                        bq[:], wq_s[:, kc, :], hTr[s][:, kc, :], start=(kc == 0), stop=(kc == 7)),
                        deps=[th, tw, bank_free[0 + s]] + ([qk_last_reader[0], qk_last_reader[1]] if kc == 0 else []),
                        inc=(kc == 7))
                e0 = P.op("act", lambda e, bq=bq, cols=cols: e.mul(QT[0][0:64, cols], bq[0:64, :], 0.125), deps=[tq, const_tok])
                e1 = P.op("act", lambda e, bq=bq, cols=cols: e.mul(QT[1][0:64, cols], bq[64:128, :], 0.125), deps=[tq])
                bank_free[0 + s] = e1
                last_ev["q"] = e1
                if not mem_pass:
                    bk = B[2 + s]
                    for kc in range(8):
                        tk = P.op("pe", lambda e, kc=kc, bk=bk, s=s: e.matmul(
                            bk[:], wk_s[:, kc, :], hTr[s][:, kc, :], start=(kc == 0), stop=(kc == 7)),
                            deps=[bank_free[2 + s]], inc=(kc == 7))
                    e2 = P.op("dve", lambda e, bk=bk, cols=cols: e.tensor_copy(KT[0][0:64, cols], bk[0:64, :]), deps=[tk, const_tok])
                    e3 = P.op("dve", lambda e, bk=bk, cols=cols: e.tensor_copy(KT[1][0:64, cols], bk[64:128, :]), deps=[tk])
                    bank_free[2 + s] = e3
                    last_ev["k"] = e3
                    bv = B[4 + s]
                    for sub in range(4):
                        for kc in range(8):
                            tv = P.op("pe", lambda e, kc=kc, bv=bv, s=s, sub=sub: e.matmul(
                                bv[:, sub * 128:(sub + 1) * 128], hTr[s][:, kc, sub * 128:(sub + 1) * 128], wv_s[:, kc, :],
                                start=(kc == 0), stop=(kc == 7)),
                                deps=[bank_free[4 + s]], inc=(sub == 3 and kc == 7))
                    bv3 = bv[:].rearrange("p (a c) -> p a c", a=4)
                    e4 = P.op("dve", lambda e, bv3=bv3, i=i: e.tensor_copy(VA[:, 4 * i:4 * i + 4, 0, 0:64], bv3[:, :, 0:64]), deps=[tv, t_va])
                    e5 = P.op("act", lambda e, bv3=bv3, i=i: e.copy(VA[:, 4 * i:4 * i + 4, 1, 0:64], bv3[:, :, 64:128]), deps=[tv, t_va, e4])
                    bank_free[4 + s] = e5
                    last_ev["v0"] = e4
                    last_ev["v1"] = e5
                    if fox:
                        for kc in range(8):
                            tf = P.op("pe", lambda e, kc=kc, s=s, i=i: e.matmul(
                                B[6][0:32, :], wfg_s[:, kc, i * 32:(i + 1) * 32], hTr[s][:, kc, :],
                                start=(i == 0 and kc == 0), stop=(i == NQT - 1 and kc == 7)),
                                deps=[bank_free[6]] if (i == 0 and kc == 0) else [], inc=(kc == 7))
                        hT_free[s] = tf
                    else:
                        hT_free[s] = tv
                else:
                    hT_free[s] = tq
            w_free = hT_free[(NQT - 1) % 2]
            proj_done = [last_ev[k] for k in last_ev]

            if mem_pass:
                for kc in range(8):
                    tmk = P.op("pe", lambda e, kc=kc: e.matmul(
                        B[2][:, 0:N_MEM], wmk_s[:, kc, :], memT_s[:, kc, :], start=(kc == 0), stop=(kc == 7)),
                        deps=[bank_free[2], const_tok], inc=(kc == 7))
                e2 = P.op("dve", lambda e: e.tensor_copy(KT[0][0:64, 0:N_MEM], B[2][0:64, 0:N_MEM]), deps=[tmk])
                e3 = P.op("dve", lambda e: e.tensor_copy(KT[1][0:64, 0:N_MEM], B[2][64:128, 0:N_MEM]), deps=[tmk])
                bank_free[2] = e3
                for mt in range(2):
                    for kc in range(8):
                        tmv = P.op("pe", lambda e, kc=kc, mt=mt: e.matmul(
                            B[4][:, mt * 128:(mt + 1) * 128], memT_s[:, kc, mt * 128:(mt + 1) * 128], wmv_s[:, kc, :],
                            start=(kc == 0), stop=(kc == 7)),
                            deps=[bank_free[4]], inc=(mt == 1 and kc == 7))
                bv3 = B[4][:, 0:256].rearrange("p (a c) -> p a c", a=2)
                e4 = P.op("dve", lambda e: e.tensor_copy(VA[:, 0:2, 0, 0:64], bv3[:, :, 0:64]), deps=[tmv])
                e5 = P.op("dve", lambda e: e.tensor_copy(VA[:, 0:2, 1, 0:64], bv3[:, :, 64:128]), deps=[tmv])
                bank_free[4] = e5
                proj_done += [e3, e5]

            if not mem_pass:
                if fox:
                    a1 = P.op("act", lambda e, p=p: e.activation(
                        out=Z1[:], in_=B[6][0:32, :], func=AF.Sigmoid, bias=bfg_s[:, p:p + 1], scale=1.0),
                        deps=[tf, const_tok, dt_free[0], dt_free[1]])
                    bank_free[6] = a1
                    a2 = P.op("act", lambda e: e.activation(out=Z1[:], in_=Z1[:], func=AF.Ln), deps=[a1])
                    c1 = P.op("dve", lambda e: e.tensor_tensor_scan(
                        out=Cc[:], data0=ones32[:], data1=Z1[:], initial=0.0, op0=ALU.mult, op1=ALU.add),
                        deps=[a2, t_ones])
                    m1 = P.op("pe", lambda e: e.matmul(B[5][0:32, 0:1], tri[:], Cc[:, 511:512], start=True, stop=True),
                              deps=[c1, bank_free[5], const_tok])
                    c2 = P.op("dve", lambda e: e.tensor_copy(offs[:], B[5][0:32, 0:1]), deps=[m1])
                    c3 = P.op("dve", lambda e: e.tensor_scalar(
                        out=Cc[:], in0=Cc[:], scalar1=offs[:, 0:1], scalar2=None, op0=ALU.add), deps=[c2])
                    bank_free[5] = c2
                    tc_ready = c3
                else:
                    tcd = P.dma("sp", d_dec2, lambda e, p=p: e.dma_start(out=Cc[:], in_=cdec[p]),
                                deps=[dt_free[0], dt_free[1]])
                    tc_ready = tcd
                c4 = P.op("dve", lambda e: e.tensor_scalar(
                    out=Z1[:], in0=Cc[:], scalar1=Cc[:, 255:256], scalar2=None, op0=ALU.subtract), deps=[tc_ready])
                c5 = P.op("dve", lambda e: e.tensor_copy(RQ[:, 0, :], Z1[:]), deps=[c4])
                c6 = P.op("dve", lambda e: e.tensor_tensor(out=Z1[:], in0=Z1[:], in1=RQ[:, 0, :], op=ALU.subtract), deps=[c5])
                c7 = P.op("dve", lambda e: e.tensor_copy(RQ[:, 1, :], Z1[:]), deps=[c6])
                ck = c7
                for jj in range(4):
                    ck = P.op("dve", lambda e, jj=jj: e.tensor_scalar(
                        out=Z1[:, jj * 128:(jj + 1) * 128], in0=Cc[:, jj * 128:(jj + 1) * 128],
                        scalar1=Cc[:, jj * 128 + 63:jj * 128 + 64], scalar2=-1.0, op0=ALU.subtract, op1=ALU.mult), deps=[ck])
                c8 = P.op("dve", lambda e: e.tensor_copy(RK[:, 0, :], Z1[:]), deps=[ck])
                c9 = P.op("dve", lambda e: e.tensor_tensor(out=Z1[:], in0=Z1[:], in1=RK[:, 0, :], op=ALU.subtract), deps=[c8])
                c10 = P.op("dve", lambda e: e.tensor_copy(RK[:, 1, :], Z1[:]), deps=[c9])
                c11 = P.op("dve", lambda e: e.tensor_scalar(
                    out=refs[:, 0:16], in0=iq[:], scalar1=Cc[:, 255:256], scalar2=None, op0=ALU.mult), deps=[c10, const_tok])
                rk3 = refs[:, 16:80].rearrange("p (i j) -> p i j", j=4)
                for jj in range(4):
                    c11 = P.op("dve", lambda e, jj=jj: e.tensor_scalar(
                        out=rk3[:, :, jj], in0=iq[:], scalar1=Cc[:, jj * 128 + 63:jj * 128 + 64], scalar2=None,
                        op0=ALU.mult), deps=[c11])
                dts = []
                for r in range(2):
                    t1 = P.dma("sp", d_dec, lambda e, r=r: e.dma_start(
                        out=dscr[r, 0].rearrange("h (i t) -> i h t", i=16), in_=RQ[r * 16:(r + 1) * 16, :, :]), deps=[c11])
                    t2 = P.dma("sp", d_dec, lambda e, r=r: e.dma_start(
                        out=dscr[r, 1].rearrange("h (i t) -> i h t", i=16), in_=RK[r * 16:(r + 1) * 16, :, :]))
                    dts.append(t2)
                dts2 = []
                for r in range(2):
                    t3 = P.dma("sp", d_dec, lambda e, r=r: e.dma_start(out=QT[r][96:98, :], in_=dscr[r, 0]),
                               deps=[dts[-1], qk_last_reader[r]])
                    t4 = P.dma("sp", d_dec, lambda e, r=r: e.dma_start(out=KT[r][98:100, :], in_=dscr[r, 1]))
                    dts2.append(t4)
                dec_rows_tok = dts2[-1]
                for r in range(2):
                    m2 = P.op("pe", lambda e, r=r: e.matmul(
                        B[5][:, 0:80], sel[:, r * 128:(r + 1) * 128], refs[:], start=True, stop=True),
                        deps=[c11, bank_free[5], const_tok])
                    c12 = P.op("dve", lambda e: e.tensor_copy(bq_s[:], B[5][:, 0:16]), deps=[m2])
                    cl = c12
                    for i in range(NQT):
                        cl = P.op("dve", lambda e, r=r, i=i: e.tensor_scalar(
                            out=Dt[r][:, i * NKT:(i + 1) * NKT], in0=B[5][:, 16:80], scalar1=-1.0, scalar2=bq_s[:, i:i + 1],
                            op0=ALU.mult, op1=ALU.add), deps=[cl], inc=(i == NQT - 1))
                    bank_free[5] = cl
                    dt_ready = cl
                dt_ready_all = dt_ready
            else:
                dec_rows_tok = None
                dt_ready_all = None

            gate_done = []
            if (not mem_pass) and (not fox):
                for r in range(2):
                    k1 = P.op("dve", lambda e, r=r: e.tensor_reduce(
                        out=kbf[:], in_=KT[r][0:64, :].rearrange("p (n l) -> p n l", l=256), axis=AX.X, op=ALU.add),
                        deps=proj_done)
                    k2 = P.op("dve", lambda e, r=r: e.tensor_scalar(
                        out=kbar[r][:], in0=kbf[:], scalar1=1.0 / 256.0, scalar2=None, op0=ALU.mult), deps=[k1])
                    for i in range(NQT):
                        s = i % 2
                        for sub in range(4):
                            c0 = i * 512 + sub * 128
                            g1 = P.op("pe", lambda e, r=r, c0=c0, sub=sub: e.matmul(
                                B[5][:, sub * 32:(sub + 1) * 32], QT[r][0:64, c0:c0 + 128], kbar[r][:], start=True, stop=True),
                                deps=[k2, bank_free[5]] + proj_done, inc=(sub == 3))
                        g2 = P.op("dve", lambda e, s=s, i=i: e.tensor_tensor(
                            out=gp[s][:], in0=B[5][:, 0:128], in1=maskm[:, i * 128:(i + 1) * 128], op=ALU.add),
                            deps=[g1, const_tok])
                        bank_free[5] = g2
                        g3 = g2
                        for sub in range(4):
                            g3 = P.op("dve", lambda e, s=s, sub=sub: e.max(m8[s][:, sub, :], gp[s][:, sub * 32:(sub + 1) * 32]),
                                      deps=[g3])
                        g4 = P.op("dve", lambda e, s=s: e.tensor_scalar(
                            out=thr[s][:], in0=m8[s][:, :, 3], scalar1=-1e29, scalar2=None, op0=ALU.max), deps=[g3])
                        g5 = g4
                        for sub in range(4):
                            g5 = P.op("dve", lambda e, s=s, sub=sub: e.tensor_scalar(
                                out=TB[s][:, sub, 64:96], in0=gp[s][:, sub * 32:(sub + 1) * 32],
                                scalar1=thr[s][:, sub:sub + 1], scalar2=NEG, op0=ALU.is_lt, op1=ALU.mult),
                                deps=[g5, t_tb, TB_free[s]])
                        for sub in range(4):
                            g6 = P.op("pe", lambda e, s=s, sub=sub: e.transpose(
                                Bbf[0:96, (s * 4 + sub) * 128:(s * 4 + sub + 1) * 128], TB[s][:, sub, :], ident[:]),
                                deps=[g5, bank_free["bf"], const_tok], inc=(sub == 3))
                        g7 = P.op("act", lambda e, r=r, i=i, s=s: e.copy(QT[r][64:96, i * 512:(i + 1) * 512], Bbf[64:96, s * 512:(s + 1) * 512]),
                            deps=[g6])
                        TB_free[s] = g6
                        bank_free["bf"] = g7
                        gate_done = [g7]

            att_deps = proj_done + gate_done + [dec_rows_tok, dt_ready_all, const_tok]
            nk_rows = 64 if mem_pass else KAUG
            for r in range(2):
                hl = 2 * p + r
                for i in range(NQT):
                    u = unit
                    unit += 1
                    ob = B[3 + (u % 2)]
                    ntile = 2 if mem_pass else 4 * (i + 1)
                    qk_tok = [None] * ntile
                    ex_tok = [None] * ntile
                    pv_tok = None
                    sidx = [None] * ntile

                    def emit_qk(j):
                        nonlocal att_deps
                        sl = emit_qk.ctr % 3
                        emit_qk.ctr += 1
                        sidx[j] = sl
                        jj = j - 4 * i
                        c0 = 128 * jj if ((not mem_pass) and jj >= 0) else 0
                        t = P.op("pe", lambda e: e.matmul(
                            B[sl][:, c0:512], KT[r][0:nk_rows, j * 128:(j + 1) * 128],
                            QT[r][0:nk_rows, i * 512 + c0:(i + 1) * 512], start=True, stop=True),
                            deps=[bank_free[sl]] + att_deps)
                        att_deps = []
                        qk_tok[j] = t

                    def emit_exp(j):
                        sl = sidx[j]
                        pt = emit_exp.ctr % 4
                        emit_exp.ctr += 1
                        jj = j - 4 * i
                        diag = (not mem_pass) and jj >= 0
                        c0 = 128 * jj if diag else 0
                        bias = 0.0 if mem_pass else Dt[r][:, i * NKT + j:i * NKT + j + 1]
                        if diag:
                            ts = emit_exp.tctr % 2
                            emit_exp.tctr += 1
                            t1 = P.op("dve", lambda e: e.tensor_tensor(
                                out=TMP[ts][:, c0:512], in0=B[sl][:, c0:512], in1=negm[:, 0:512 - c0], op=ALU.add),
                                deps=[qk_tok[j], TMP_free[ts]])
                            bank_free[sl] = t1
                            t2 = P.op("act", lambda e: e.activation(
                                out=PT[pt][:, c0:512], in_=TMP[ts][:, c0:512], func=AF.Exp, bias=bias, scale=1.0),
                                deps=[t1, PT_free[pt]])
                            TMP_free[ts] = t2
                        else:
                            t2 = P.op("act", lambda e: e.activation(
                                out=PT[pt][:, :], in_=B[sl][:, :], func=AF.Exp, bias=bias, scale=1.0),
                                deps=[qk_tok[j], PT_free[pt]])
                            bank_free[sl] = t2
                        ex_tok[j] = (t2, pt, c0)

                    def emit_pv(j):
                        nonlocal pv_tok
                        t2, pt, c0 = ex_tok[j]
                        t = P.op("pe", lambda e: e.matmul(
                            ob[:, c0:512], VA[:, j, r, :], PT[pt][:, c0:512], start=(j == 0), stop=(j == ntile - 1)),
                            deps=[t2] + ([bank_free[3 + (u % 2)]] if j == 0 else []))
                        PT_free[pt] = t
                        pv_tok = t

                    emit_qk.ctr = ctrs["qk"]
                    emit_exp.ctr = ctrs["ex"]
                    emit_exp.tctr = ctrs["tx"]
                    emit_qk(0)
                    for j in range(ntile):
                        if j + 1 < ntile:
                            emit_qk(j + 1)
                        emit_exp(j)
                        emit_pv(j)
                    ctrs["qk"] = emit_qk.ctr
                    ctrs["ex"] = emit_exp.ctr
                    ctrs["tx"] = emit_exp.tctr
                    qk_last_reader[r] = pv_tok
                    os_ = u % 2
                    n1 = P.op("dve", lambda e: e.reciprocal(RR[os_][:], ob[64:128, :]), deps=[pv_tok, RR_free[os_]])
                    n2 = P.op("dve", lambda e: e.tensor_tensor(out=OS[os_][:], in0=ob[0:64, :], in1=RR[os_][:], op=ALU.mult),
                              deps=[n1, OS_free[os_]])
                    bank_free[3 + (u % 2)] = n2
                    RR_free[os_] = n2
                    to = P.dma("sp", d_o[os_], lambda e: e.dma_start(
                        out=OT[hl * 64:(hl + 1) * 64, i * 512:(i + 1) * 512], in_=OS[os_][:]), deps=[n2])
                    OS_free[os_] = to
                    out_toks.append(to)
                dt_free[r] = qk_last_reader[r]
        barrier(P)


_CONST = {}


def attn_consts(kind, hh):
    key = (kind, hh)
    if key in _CONST:
        return _CONST[key]
    c = {}
    c["ident"] = np.eye(128, dtype=np.float32)
    p = np.arange(128)[:, None]
    f = np.arange(512)[None, :]
    c["negm"] = np.where(f >= p, 0.0, NEG).astype(np.float32)
    t = np.arange(SEQ)
    kaug = np.zeros((36, SEQ), np.float32)
    kaug[t // 256, t] = 1.0
    kaug[32:34] = 1.0
    c["kaug"] = kaug
    qaug = np.zeros((36, SEQ), np.float32)
    qaug[34:36] = 1.0
    c["qaug"] = qaug
    k = np.arange(32)
    c["iq"] = (k[:, None] % 16 == np.arange(16)[None, :]).astype(np.float32)
    sel = np.zeros((32, 256), np.float32)
    for r in range(2):
        sel[r * 16:(r + 1) * 16, r * 128:(r + 1) * 128] = 1.0
    c["sel"] = sel
    if kind == "fox":
        c["tri"] = ((k[:, None] // 16 == k[None, :] // 16) & (k[:, None] < k[None, :])).astype(np.float32)
    else:
        slopes = _alibi_slopes(N_SELF)
        cdec = np.zeros((3, 32, 512), np.float32)
        for pp in range(3):
            for r in range(2):
                h = 6 * hh + 2 * pp + r
                for i in range(16):
                    cdec[pp, r * 16 + i] = -slopes[h] * (i * 512 + np.arange(512, dtype=np.float32))
        c["cdec"] = cdec
        mm = np.zeros((64, 32), np.float32)
        for s in range(64):
            qb = s // 2
            mm[s, :] = np.where(np.arange(32) < qb, 0.0, np.where(np.arange(32) == qb, 1e30, -1e30))
        c["maskm"] = np.ascontiguousarray(np.broadcast_to(mm.reshape(1, 2048), (128, 2048))).astype(np.float32)
    _CONST[key] = c
    return c


def attn_inputs(kind, hh, hT_b, memT_b, w_in, b_fg, w_mem_kv):
    SW = N_SELF * HEAD_DIM
    heads = slice(6 * hh * 64, (6 * hh + 6) * 64)
    m = dict(attn_consts(kind, hh))
    wq_self = w_in[:, 0:SW][:, heads]
    wq_mem = w_in[:, 3 * SW:3 * SW + 256][:, hh * 128:(hh + 1) * 128]
    m["wq"] = np.ascontiguousarray(np.concatenate([wq_self, wq_mem], axis=1))
    m["wk"] = np.ascontiguousarray(w_in[:, SW:2 * SW][:, heads])
    m["wv"] = np.ascontiguousarray(w_in[:, 2 * SW:3 * SW][:, heads])
    m["hT"] = hT_b
    m["memT"] = memT_b
    m["wmk"] = np.ascontiguousarray(w_mem_kv[:, 0:256][:, hh * 128:(hh + 1) * 128])
    m["wmv"] = np.ascontiguousarray(w_mem_kv[:, 256:512][:, hh * 128:(hh + 1) * 128])
    if kind == "fox":
        wfg = w_in[:, 2560:2572]
        wfgp = np.zeros((3, 1024, 16, 32), np.float32)
        bfg = np.zeros((32, 3), np.float32)
        for pp in range(3):
            for r in range(2):
                h = 6 * hh + 2 * pp + r
                for i in range(16):
                    wfgp[pp, :, i, r * 16 + i] = wfg[:, h]
                bfg[r * 16:(r + 1) * 16, pp] = b_fg[h]
        m["wfgp"] = wfgp.reshape(3, 1024, 512)
        m["bfg"] = bfg
    return m


NTOK = 4096
NTT = NTOK // 128
CAP = 640
NSLOT = N_EXPERTS * CAP
PART = 320


def barrier(P):
    keys = list(P.cnt.keys())
    for e in P.engs:
        for k in keys:
            if P.cnt[k] > 0:
                P.wait(e, (k, P.cnt[k]))


def emit_ffn(nc, P, B, Bbf, E, L):
    sfx = "_f%d" % L
    h_in = E["x_own"] if L == 0 else E["hres"]
    out = E["out"] if L == NL - 1 else E["hres"]
    emit_hT = L < NL - 1
    OTv = E["OT_pair"].rearrange("c r q (h g t) -> (c r q h g) t", h=2, g=8)
    idxm_d = E["idxm"]
    wo, ln1g, ln1b, ln2g, ln2b = E["wo"][L], E["ln1g"][L], E["ln1b"][L], E["ln2g"][L], E["ln2b"][L]
    wr, rb = E["wr"][L], E["rb"][L]
    wgu, bgu, wdn, bdn = E["wgu"][L], E["bgu"][L], E["wdn"][L], E["bdn"][L]
    ident_d, tstr_d, iota_d, ecap_d = E["ident"], E["tstr"], E["iotae"], E["ecap"]
    H1, Xs, Ys, hT_own = E["H1"], E["Xs"], E["Ys"], E["hT_own"]
    bc_reg, bc_reg2 = E["bc_reg"], E["bc_reg2"]

    with contextlib.ExitStack() as st:
        def sbp(name, shape, dt):
            return st.enter_context(nc.sbuf_tensor(name + sfx, shape, dt))

        gates = sbp("gates", [128, NTT, 4], F32)
        slots = sbp("slots", [128, NTT * 4], I32)
        identb = sbp("identb", [128, 128], BF16)
        identf = sbp("identf", [128, 128], F32)
        tstr = sbp("tstr_s", [128, 128], F32)
        onesf = sbp("onesf", [128, 128], F32)
        iotae = sbp("iotae_s", [128, 32], F32)
        ecap = sbp("ecap_s", [128, 32], F32)
        onesb = sbp("onesb", [1, 128], BF16)
        idxm = sbp("idxm_s", [128, 64], I32)

        d_c = P.dsem("const")
        P.dma("pool", d_c, lambda e: e.dma_start(out=identb[:], in_=ident_d))
        P.dma("sp", d_c, lambda e: e.dma_start(out=identf[:], in_=ident_d))
        P.dma("sp", d_c, lambda e: e.dma_start(out=tstr[:], in_=tstr_d))
        P.dma("sp", d_c, lambda e: e.dma_start(out=iotae[:], in_=iota_d))
        P.dma("sp", d_c, lambda e: e.dma_start(out=idxm[:], in_=idxm_d))
        const_tok = P.dma("sp", d_c, lambda e: e.dma_start(out=ecap[:], in_=ecap_d))
        t_ones = P.op("pool", lambda e: e.memset(onesf[:], 1.0))
        t_ones = P.op("pool", lambda e: e.memset(onesb[:], 1.0))

        with contextlib.ExitStack() as s1:
            def sb(name, shape, dt):
                return s1.enter_context(nc.sbuf_tensor(name + sfx, shape, dt))
            wo_s = sb("wo_s", [128, 8, 1024], BF16)
            g1 = sb("g1", [128, 1024], F32)
            b1 = sb("b1", [128, 1024], F32)
            wr_s = sb("wr_s", [128, 8, 32], BF16)
            rb_s = sb("rb_s", [128, 32], F32)
            mTr = [sb("mTr%d" % i, [128, 8, 512], BF16) for i in range(2)]
            htr = [sb("htr%d" % i, [128, 1024], F32) for i in range(2)]
            rt = [sb("rt%d" % i, [128, 1024], F32) for i in range(2)]
            h1r = [sb("h1r%d" % i, [128, 1024], F32) for i in range(2)]
            h1b = [sb("h1b%d" % i, [128, 1024], BF16) for i in range(3)]
            h1T = [sb("h1T%d" % i, [128, 8, 128], BF16) for i in range(2)]
            stats = [sb("stats%d" % i, [128, 2, 6], F32) for i in range(2)]
            mv = [sb("mv%d" % i, [128, 2], F32) for i in range(2)]
            rstd = [sb("rstd%d" % i, [128, 1], F32) for i in range(2)]
            Lg = [sb("Lg%d" % i, [128, 32], F32) for i in range(2)]
            m8 = [sb("m8_%d" % i, [128, 8], F32) for i in range(2)]
            i8 = [sb("i8_%d" % i, [128, 8], U32) for i in range(2)]
            i8f = [sb("i8f_%d" % i, [128, 8], F32) for i in range(2)]
            nm0 = [sb("nm0_%d" % i, [128, 1], F32) for i in range(2)]
            e4 = [sb("e4_%d" % i, [128, 4], F32) for i in range(2)]
            ssum = [sb("ssum%d" % i, [128, 1], F32) for i in range(2)]
            Mk = [sb("Mk%d" % i, [128, 32], F32) for i in range(2)]
            Srun = [sb("Srun%d" % i, [128, 32], F32) for i in range(2)]
            sbase = [sb("sbase%d" % i, [128, 32], F32) for i in range(2)]
            ovf = [sb("ovf%d" % i, [128, 32], F32) for i in range(2)]
            prod = [sb("prod%d" % i, [128, 4, 32], F32) for i in range(2)]
            slotf = [sb("slotf%d" % i, [128, 4], F32) for i in range(2)]
            zt = sb("zt", [128, 5, 1024], BF16)

            d_w1 = P.dsem("w1")
            P.dma("pool", d_w1, lambda e: e.dma_start(out=wo_s[:], in_=wo.rearrange("(kc p) t -> p kc t", p=128)))
            P.dma("pool", d_w1, lambda e: e.dma_start(out=wr_s[:], in_=wr.rearrange("(kc p) t -> p kc t", p=128)))
            P.dma("sp", d_w1, lambda e: e.dma_start(out=g1[:], in_=ln1g))
            P.dma("sp", d_w1, lambda e: e.dma_start(out=b1[:], in_=ln1b))
            w1_tok = P.dma("sp", d_w1, lambda e: e.dma_start(out=rb_s[:], in_=rb))
            d_z = P.dsem("z")
            z_tok = None
            if L == 0:
                tz = P.op("pool", lambda e: e.memset(zt[:], 0.0))
                for ex in range(N_EXPERTS):
                    z_tok = P.dma("sp", d_z, lambda e, ex=ex: e.dma_start(
                        out=Xs[ex * CAP:(ex + 1) * CAP, :].rearrange("(s p) d -> p s d", p=128), in_=zt[:]), deps=[tz])
            t_s0 = P.op("pool", lambda e: e.memset(Srun[0][:], 0.0))

            d_m = [P.dsem("m%d" % i) for i in range(2)]
            d_h = [P.dsem("h%d" % i) for i in range(2)]
            d_h1 = [P.dsem("h1_%d" % i) for i in range(2)]
            d_sc = [P.dsem("sc%d" % i) for i in range(3)]
            mT_free = [None, None]
            ht_free = [None, None]
            rt_free = [None, None]
            h1r_free = [[], []]
            h1b_free = [None] * 3
            h1T_free = [None, None]
            bfree = {i: None for i in range(7)}
            bfree["bf"] = None
            st1 = {}
            srun_tok = t_s0
            last_scatter = None
            m_tok = None

            def stage1(t):
                nonlocal m_tok
                s = t % 2
                if t % 4 == 0:
                    ms = (t // 4) % 2
                    for kc in range(8):
                        m_tok = P.dma("pool", d_m[ms], lambda e, kc=kc: e.indirect_dma_start(
                            out=mTr[ms][:, kc, :], out_offset=None, in_=OTv,
                            in_offset=bass.IndirectOffsetOnAxis(ap=idxm[:, kc * 8 + t // 4:kc * 8 + t // 4 + 1], axis=0),
                            bounds_check=bc_reg2, oob_is_err=False), deps=[mT_free[ms], const_tok])
                ms = (t // 4) % 2
                tl = t % 4
                th = P.dma("sp", d_h[s], lambda e: e.dma_start(out=htr[s][:], in_=h_in[t * 128:(t + 1) * 128, :]),
                           deps=[ht_free[s]])
                for half in range(2):
                    for kc in range(8):
                        tm = P.op("pe", lambda e: e.matmul(
                            B[half][:], mTr[ms][:, kc, tl * 128:(tl + 1) * 128], wo_s[:, kc, half * 512:(half + 1) * 512],
                            start=(kc == 0), stop=(kc == 7)), deps=[m_tok, w1_tok, bfree[half]], inc=(kc == 7))
                    r1 = P.op("dve", lambda e: e.scalar_tensor_tensor(
                        out=rt[s][:, half * 512:(half + 1) * 512], in0=htr[s][:, half * 512:(half + 1) * 512], scalar=ALPHA,
                        in1=B[half][:], op0=ALU.mult, op1=ALU.add), deps=[tm, th, rt_free[s]])
                    bfree[half] = r1
                    r2 = P.op("dve", lambda e: e.bn_stats(stats[s][:, half, :], rt[s][:, half * 512:(half + 1) * 512]), deps=[r1])
                mT_free[ms] = tm
                ht_free[s] = r1
                r3 = P.op("dve", lambda e: e.bn_aggr(mv[s][:], stats[s][:].rearrange("p a b -> p (a b)")), deps=[r2])
                r4 = P.op("dve", lambda e: e.tensor_scalar(
                    out=rstd[s][:], in0=mv[s][:, 1:2], scalar1=LN_EPS, scalar2=None, op0=ALU.add), deps=[r3])
                r4 = P.op("act", lambda e: e.sqrt(rstd[s][:], rstd[s][:]), deps=[r4])
                r4 = P.op("dve", lambda e: e.reciprocal(rstd[s][:], rstd[s][:]), deps=[r4])
                r5 = P.op("dve", lambda e: e.tensor_scalar(
                    out=rt[s][:], in0=rt[s][:], scalar1=mv[s][:, 0:1], scalar2=rstd[s][:, 0:1],
                    op0=ALU.subtract, op1=ALU.mult), deps=[r4])
                r6 = P.op("pool", lambda e: e.tensor_tensor(out=h1r[s][:], in0=rt[s][:], in1=g1[:], op=ALU.mult),
                          deps=[r5, w1_tok] + h1r_free[s])
                rt_free[s] = r6
                r7 = P.op("pool", lambda e: e.tensor_tensor(out=h1r[s][:], in0=h1r[s][:], in1=b1[:], op=ALU.add), deps=[r6])
                ts_ = P.dma("sp", d_h1[s], lambda e: e.dma_start(out=H1[t * 128:(t + 1) * 128, :], in_=h1r[s][:]), deps=[r7])
                bs = t % 3
                r8 = P.op("act", lambda e: e.copy(h1b[bs][:], h1r[s][:]), deps=[r7, h1b_free[bs]])
                h1r_free[s] = [ts_, r8]
                st1[t] = r8

            def stage2(t):
                nonlocal srun_tok, last_scatter
                s = t % 2
                bs = t % 3
                for kc in range(8):
                    t1 = P.op("pe", lambda e: e.transpose(Bbf[:, kc * 128:(kc + 1) * 128], h1b[bs][:, kc * 128:(kc + 1) * 128], identb[:]),
                              deps=[st1[t], bfree["bf"], const_tok], inc=(kc == 7))
                t2 = P.op("act", lambda e: e.copy(h1T[s][:], Bbf[:].rearrange("p (a c) -> p a c", a=8)), deps=[t1, h1T_free[s]])
                bfree["bf"] = t2
                for kc in range(8):
                    t3 = P.op("pe", lambda e: e.matmul(B[2][:, 0:32], h1T[s][:, kc, :], wr_s[:, kc, :], start=(kc == 0), stop=(kc == 7)),
                              deps=[t2, bfree[2]], inc=(kc == 7))
                h1T_free[s] = t3
                a1 = P.op("dve", lambda e: e.tensor_tensor(out=Lg[s][:], in0=B[2][:, 0:32], in1=rb_s[:], op=ALU.add), deps=[t3, w1_tok])
                bfree[2] = a1
                a2 = P.op("dve", lambda e: e.max(m8[s][:], Lg[s][:]), deps=[a1])
                a3 = P.op("dve", lambda e: e.max_index(i8[s][:], m8[s][:], Lg[s][:]), deps=[a2])
                a4 = P.op("dve", lambda e: e.tensor_copy(i8f[s][:], i8[s][:]), deps=[a3])
                a5 = P.op("dve", lambda e: e.tensor_scalar(out=nm0[s][:], in0=m8[s][:, 0:1], scalar1=-1.0, scalar2=None, op0=ALU.mult), deps=[a4])
                a6 = P.op("act", lambda e: e.activation(out=e4[s][:], in_=m8[s][:, 0:4], func=AF.Exp, bias=nm0[s][:, 0:1], scale=1.0),
                          deps=[a5])
                a7 = P.op("dve", lambda e: e.tensor_reduce(out=ssum[s][:], in_=e4[s][:], axis=AX.X, op=ALU.add), deps=[a6])
                a8 = P.op("dve", lambda e: e.reciprocal(ssum[s][:], ssum[s][:]), deps=[a7])
                a9 = P.op("dve", lambda e: e.tensor_scalar(out=gates[:, t, :], in0=e4[s][:], scalar1=ssum[s][:, 0:1], scalar2=None, op0=ALU.mult), deps=[a8])
                a10 = P.op("dve", lambda e: e.tensor_scalar(out=Mk[s][:], in0=Lg[s][:], scalar1=m8[s][:, 3:4], scalar2=None, op0=ALU.is_ge), deps=[a9])
                p1 = P.op("pe", lambda e: e.matmul(B[3][:, 0:32], tstr[:], Mk[s][:], start=True, stop=False), deps=[a10, bfree[3], const_tok], inc=False)
                p2 = P.op("pe", lambda e: e.matmul(B[3][:, 0:32], onesf[:], Srun[s][:], start=False, stop=True), deps=[srun_tok, t_ones])
                srun_tok = P.op("pool", lambda e: e.tensor_tensor(out=Srun[1 - s][:], in0=Srun[s][:], in1=Mk[s][:], op=ALU.add), deps=[a10, p2, srun_tok])
                q1 = P.op("dve", lambda e: e.tensor_tensor(out=sbase[s][:], in0=B[3][:, 0:32], in1=ecap[:], op=ALU.add), deps=[p2, const_tok])
                q2 = P.op("dve", lambda e: e.tensor_scalar(out=ovf[s][:], in0=B[3][:, 0:32], scalar1=CAP - 0.5, scalar2=1.0e6, op0=ALU.is_gt, op1=ALU.mult), deps=[q1])
                bfree[3] = q2
                q3 = P.op("dve", lambda e: e.tensor_tensor(out=sbase[s][:], in0=sbase[s][:], in1=ovf[s][:], op=ALU.add), deps=[q2])
                q4 = q3
                for k in range(4):
                    q4 = P.op("dve", lambda e: e.scalar_tensor_tensor(
                        out=prod[s][:, k, :], in0=iotae[:], scalar=i8f[s][:, k:k + 1], in1=sbase[s][:], op0=ALU.is_equal, op1=ALU.mult),
                        deps=[q4, const_tok])
                q5 = P.op("dve", lambda e: e.tensor_reduce(out=slotf[s][:], in_=prod[s][:], axis=AX.X, op=ALU.add), deps=[q4])
                q6 = P.op("dve", lambda e: e.tensor_copy(slots[:, t * 4:t * 4 + 4], slotf[s][:]), deps=[q5])
                for k in range(4):
                    last_scatter = P.dma("pool", d_sc[bs], lambda e: e.indirect_dma_start(
                        out=Xs, out_offset=bass.IndirectOffsetOnAxis(ap=slots[:, t * 4 + k:t * 4 + k + 1], axis=0),
                        in_=h1b[bs][:, :], in_offset=None, bounds_check=bc_reg, oob_is_err=False),
                        deps=[q6, z_tok])
                h1b_free[bs] = last_scatter

            for t in range(NTT + 1):
                if t < NTT:
                    stage1(t)
                if t >= 1:
                    stage2(t - 1)
            barrier(P)

        with contextlib.ExitStack() as s2:
            def sb(name, shape, dt):
                return s2.enter_context(nc.sbuf_tensor(name + sfx, shape, dt))
            xin = [sb("xin%d" % i, [128, 5, 1024], BF16) for i in range(2)]
            XeT = [sb("XeT%d" % i, [128, 8, CAP], BF16) for i in range(2)]
            NW = 5
            wring = [sb("wring%d" % i, [128, 8, 512], BF16) for i in range(NW)]
            AT = sb("AT", [128, 8, CAP], BF16)
            gs = [sb("gs%d" % i, [128, PART], F32) for i in range(2)]
            sg = [sb("sg%d" % i, [128, PART], F32) for i in range(2)]
            u1 = [sb("u1_%d" % i, [128, PART], F32) for i in range(2)]
            tt = [sb("tt%d" % i, [128, PART], F32) for i in range(2)]
            Yt = [sb("Yt%d" % i, [128, 512], F32) for i in range(3)]
            bgu_s = sb("bgu_s", [N_EXPERTS, 2048], F32)
            bguT = sb("bguT", [128, 16, N_EXPERTS], F32)
            bdn_r = [sb("bdn%d" % i, [1, 1024], BF16) for i in range(2)]

            d_b = P.dsem("bias")
            tb = P.dma("sp", d_b, lambda e: e.dma_start(out=bgu_s[:], in_=bgu))
            for c in range(16):
                x1 = P.op("pe", lambda e, c=c: e.transpose(B[6][:, 0:32], bgu_s[:, c * 128:(c + 1) * 128], identf[0:32, 0:32]),
                          deps=[tb, const_tok] + ([x2] if c > 0 else []))
                x2 = P.op("dve", lambda e, c=c: e.tensor_copy(bguT[:, c, :], B[6][:, 0:32]), deps=[x1])
            x3 = P.op("dve", lambda e: e.tensor_scalar(out=bguT[:, 8:16, :], in0=bguT[:, 8:16, :], scalar1=1.0, scalar2=None, op0=ALU.add), deps=[x2])
            bias_tok = x3

            d_x = [P.dsem("x%d" % i) for i in range(2)]
            d_wr = [P.dsem("wr%d" % i) for i in range(NW)]
            d_bd = [P.dsem("bd%d" % i) for i in range(2)]
            d_y = [P.dsem("y%d" % i) for i in range(3)]
            xin_free = [None, None]
            XeT_free = [None, None]
            wr_free = [None] * NW
            bd_free = [None, None]
            Yt_free = [None] * 3
            AT_free = None
            bfree = {i: None for i in range(7)}
            bfree["bf"] = None
            act_free = [None, None]
            wctr = 0
            yctr = 0
            actr = 0
            y_toks = []

            pieces = []
            for ex in range(N_EXPERTS):
                for q in range(4):
                    pieces.append((ex, "gu", q))
                for hf in range(2):
                    pieces.append((ex, "dn", hf))
            piece_tok = {}
            issued = 0

            def issue_piece():
                nonlocal issued
                if issued >= len(pieces):
                    return
                ex, kd, q = pieces[issued]
                sl = issued % NW
                if kd == "gu":
                    P.dma("pool", d_wr[sl], lambda e: e.dma_start(
                        out=wring[sl][:, :, 0:256], in_=wgu[ex][:, q * 256:(q + 1) * 256].rearrange("(kc p) c -> p kc c", p=128)),
                        deps=[wr_free[sl]])
                    tk = P.dma("pool", d_wr[sl], lambda e: e.dma_start(
                        out=wring[sl][:, :, 256:512], in_=wgu[ex][:, 1024 + q * 256:1024 + (q + 1) * 256].rearrange("(kc p) c -> p kc c", p=128)))
                else:
                    tk = P.dma("pool", d_wr[sl], lambda e: e.dma_start(
                        out=wring[sl][:, :, :], in_=wdn[ex][:, q * 512:(q + 1) * 512].rearrange("(kc p) c -> p kc c", p=128)),
                        deps=[wr_free[sl]])
                piece_tok[(ex, kd, q)] = (tk, sl)
                issued += 1

            for _ in range(NW - 1):
                issue_piece()

            x_tok = {}

            def load_x(ex):
                xs = ex % 2
                x_tok[ex] = P.dma("sp", d_x[xs], lambda e: e.dma_start(
                    out=xin[xs][:], in_=Xs[ex * CAP:(ex + 1) * CAP, :].rearrange("(s p) d -> p s d", p=128)),
                    deps=[xin_free[xs]])

            load_x(0)
            for ex in range(N_EXPERTS):
                xs = ex % 2
                if ex + 1 < N_EXPERTS:
                    load_x(ex + 1)
                tbd = P.dma("pool", d_bd[xs], lambda e: e.dma_start(out=bdn_r[xs][:], in_=bdn[ex:ex + 1, :]), deps=[bd_free[xs]])
                for sidx in range(5):
                    for kc in range(8):
                        t1 = P.op("pe", lambda e: e.transpose(Bbf[:, kc * 128:(kc + 1) * 128], xin[xs][:, sidx, kc * 128:(kc + 1) * 128], identb[:]),
                                  deps=[x_tok[ex], bfree["bf"]], inc=(kc == 7))
                    t2 = P.op("act", lambda e: e.copy(XeT[xs][:, :, sidx * 128:(sidx + 1) * 128], Bbf[:].rearrange("p (a c) -> p a c", a=8)),
                              deps=[t1, XeT_free[xs]])
                    bfree["bf"] = t2
                xin_free[xs] = t1
                xet_tok = t2
                last_pool = None
                for q in range(4):
                    issue_piece()
                    wt, sl = piece_tok[(ex, "gu", q)]
                    for cl in range(2):
                        c = 2 * q + cl
                        for part in range(2):
                            cs = slice(part * PART, (part + 1) * PART)
                            for kc in range(8):
                                tg = P.op("pe", lambda e: e.matmul(B[part][:, 0:PART], wring[sl][:, kc, cl * 128:(cl + 1) * 128], XeT[xs][:, kc, cs],
                                                                   start=(kc == 0), stop=(kc == 7)), deps=[wt, xet_tok, bfree[part]], inc=(kc == 7))
                            for kc in range(8):
                                tu = P.op("pe", lambda e: e.matmul(B[2 + part][:, 0:PART], wring[sl][:, kc, 256 + cl * 128:256 + (cl + 1) * 128], XeT[xs][:, kc, cs],
                                                                   start=(kc == 0), stop=(kc == 7)), deps=[bfree[2 + part]], inc=(kc == 7))
                            a = actr % 2
                            actr += 1
                            v1 = P.op("dve", lambda e: e.tensor_scalar(out=gs[a][:], in0=B[part][:, 0:PART], scalar1=bguT[:, c, ex:ex + 1], scalar2=SWIGLU_LIMIT,
                                                                       op0=ALU.add, op1=ALU.min), deps=[tg, bias_tok, act_free[a]])
                            bfree[part] = v1
                            v2 = P.op("act", lambda e: e.activation(out=sg[a][:], in_=gs[a][:], func=AF.Sigmoid, scale=SWIGLU_ALPHA), deps=[v1])
                            v3 = P.op("dve", lambda e: e.tensor_scalar(out=u1[a][:], in0=B[2 + part][:, 0:PART], scalar1=bguT[:, 8 + c, ex:ex + 1], scalar2=SWIGLU_LIMIT + 1.0,
                                                                       op0=ALU.add, op1=ALU.min), deps=[tu])
                            bfree[2 + part] = v3
                            v4 = P.op("dve", lambda e: e.scalar_tensor_tensor(out=tt[a][:], in0=u1[a][:], scalar=-SWIGLU_LIMIT + 1.0, in1=gs[a][:],
                                                                              op0=ALU.max, op1=ALU.mult), deps=[v3, v1])
                            v5 = P.op("dve", lambda e: e.tensor_tensor(out=AT[:, c, cs], in0=tt[a][:], in1=sg[a][:], op=ALU.mult), deps=[v2, v4, AT_free])
                            act_free[a] = v5
                            last_pool = v5
                    wr_free[sl] = tu
                XeT_free[xs] = tu
                for hf in range(2):
                    issue_piece()
                    wt, sl = piece_tok[(ex, "dn", hf)]
                    for sidx in range(5):
                        yb = 4 + (yctr % 2)
                        for kc in range(8):
                            td = P.op("pe", lambda e: e.matmul(B[yb][:], AT[:, kc, sidx * 128:(sidx + 1) * 128], wring[sl][:, kc, :],
                                                               start=(kc == 0), stop=False), deps=[wt, last_pool, bfree[yb]], inc=False)
                        td = P.op("pe", lambda e: e.matmul(B[yb][:], onesb[0:1, :], bdn_r[xs][0:1, hf * 512:(hf + 1) * 512], start=False, stop=True),
                                  deps=[tbd, t_ones])
                        ys = yctr % 3
                        yctr += 1
                        w1 = P.op("act", lambda e: e.copy(Yt[ys][:], B[yb][:]), deps=[td, Yt_free[ys]])
                        bfree[yb] = w1
                        ty = P.dma("sp", d_y[ys], lambda e: e.dma_start(
                            out=Ys[ex * CAP + sidx * 128:ex * CAP + (sidx + 1) * 128, hf * 512:(hf + 1) * 512], in_=Yt[ys][:]), deps=[w1])
                        Yt_free[ys] = ty
                        y_toks.append(ty)
                    wr_free[sl] = td
                AT_free = td
                bd_free[xs] = td
            barrier(P)

        with contextlib.ExitStack() as s3:
            def sb(name, shape, dt):
                return s3.enter_context(nc.sbuf_tensor(name + sfx, shape, dt))
            g2 = sb("g2", [128, 1024], F32)
            b2 = sb("b2", [128, 1024], F32)
            Yg = [sb("Yg%d" % i, [128, 4, 1024], F32) for i in range(2)]
            h1t = [sb("h1t%d" % i, [128, 1024], F32) for i in range(2)]
            acc = [sb("acc%d" % i, [128, 1024], F32) for i in range(2)]
            ot = [sb("ot%d" % i, [128, 1024], F32) for i in range(2)]
            stats = [sb("stats3_%d" % i, [128, 2, 6], F32) for i in range(2)]
            mv = [sb("mv3_%d" % i, [128, 2], F32) for i in range(2)]
            rstd = [sb("rstd3_%d" % i, [128, 1], F32) for i in range(2)]
            d_w3 = P.dsem("w3")
            P.dma("sp", d_w3, lambda e: e.dma_start(out=g2[:], in_=ln2g))
            w3_tok = P.dma("sp", d_w3, lambda e: e.dma_start(out=b2[:], in_=ln2b))
            d_g = [P.dsem("g%d" % i) for i in range(2)]
            d_h3 = [P.dsem("h3_%d" % i) for i in range(2)]
            d_out = [P.dsem("out%d" % i) for i in range(2)]
            for s in range(2):
                P.op("pool", lambda e, s=s: e.memset(Yg[s][:], 0.0))
            Yg_free = [None, None]
            h1t_free = [None, None]
            ot_free = [[], []]
            outs = []
            gt = {}
            hst = hT_state(nc, P, s3, sfx) if emit_hT else None

            def gather(t):
                s = t % 2
                for k in range(4):
                    gt[t] = P.dma("pool", d_g[s], lambda e: e.indirect_dma_start(
                        out=Yg[s][:, k, :], out_offset=None, in_=Ys,
                        in_offset=bass.IndirectOffsetOnAxis(ap=slots[:, t * 4 + k:t * 4 + k + 1], axis=0),
                        bounds_check=bc_reg, oob_is_err=False), deps=[Yg_free[s]])

            gather(0)
            for t in range(NTT):
                s = t % 2
                if t + 1 < NTT:
                    gather(t + 1)
                th = P.dma("sp", d_h3[s], lambda e: e.dma_start(out=h1t[s][:], in_=H1[t * 128:(t + 1) * 128, :]), deps=[h1t_free[s]])
                c1 = P.op("dve", lambda e: e.tensor_scalar(out=acc[s][:], in0=Yg[s][:, 0, :], scalar1=gates[:, t, 0:1], scalar2=None, op0=ALU.mult),
                          deps=[gt[t]])
                c2 = P.op("dve", lambda e: e.scalar_tensor_tensor(out=acc[s][:], in0=Yg[s][:, 1, :], scalar=gates[:, t, 1:2], in1=acc[s][:],
                                                                   op0=ALU.mult, op1=ALU.add), deps=[c1, gt[t]])
                c3 = P.op("dve", lambda e: e.scalar_tensor_tensor(out=acc[s][:], in0=Yg[s][:, 2, :], scalar=gates[:, t, 2:3], in1=acc[s][:],
                                                                  op0=ALU.mult, op1=ALU.add), deps=[c2])
                c4 = P.op("dve", lambda e: e.scalar_tensor_tensor(out=acc[s][:], in0=Yg[s][:, 3, :], scalar=gates[:, t, 3:4], in1=acc[s][:],
                                                                   op0=ALU.mult, op1=ALU.add), deps=[c3])
                Yg_free[s] = c4
                c5 = P.op("dve", lambda e: e.scalar_tensor_tensor(out=acc[s][:], in0=h1t[s][:], scalar=ALPHA, in1=acc[s][:],
                                                                  op0=ALU.mult, op1=ALU.add), deps=[c4, th])
                h1t_free[s] = c5
                for half in range(2):
                    c6 = P.op("dve", lambda e: e.bn_stats(stats[s][:, half, :], acc[s][:, half * 512:(half + 1) * 512]), deps=[c5])
                c7 = P.op("dve", lambda e: e.bn_aggr(mv[s][:], stats[s][:].rearrange("p a b -> p (a b)")), deps=[c6])
                c8 = P.op("dve", lambda e: e.tensor_scalar(out=rstd[s][:], in0=mv[s][:, 1:2], scalar1=LN_EPS, scalar2=None, op0=ALU.add), deps=[c7])
                c8 = P.op("act", lambda e: e.sqrt(rstd[s][:], rstd[s][:]), deps=[c8])
                c8 = P.op("dve", lambda e: e.reciprocal(rstd[s][:], rstd[s][:]), deps=[c8])
                c9 = P.op("dve", lambda e: e.tensor_scalar(out=acc[s][:], in0=acc[s][:], scalar1=mv[s][:, 0:1], scalar2=rstd[s][:, 0:1],
                                                           op0=ALU.subtract, op1=ALU.mult), deps=[c8])
                c10 = P.op("pool", lambda e: e.tensor_tensor(out=ot[s][:], in0=acc[s][:], in1=g2[:], op=ALU.mult), deps=[c9, w3_tok] + ot_free[s])
                c11 = P.op("pool", lambda e: e.tensor_tensor(out=ot[s][:], in0=ot[s][:], in1=b2[:], op=ALU.add), deps=[c10])
                to = P.dma("sp", d_out[s], lambda e: e.dma_start(out=out[t * 128:(t + 1) * 128, :], in_=ot[s][:]), deps=[c11])
                ot_free[s] = [to]
                outs.append(to)
                if emit_hT:
                    x3 = hT_tile(nc, P, Bbf, identb, hst, ot[s], c11, t, hT_own, [const_tok])
                    ot_free[s].append(hst["cast_tok"])
            barrier(P)


def hT_state(nc, P, stack, sfx):
    stt = {}
    stt["obf"] = [stack.enter_context(nc.sbuf_tensor("obf%d%s" % (i, sfx), [128, 1024], BF16)) for i in range(2)]
    stt["hTs"] = [stack.enter_context(nc.sbuf_tensor("hTs%d%s" % (i, sfx), [128, 8, 512], BF16)) for i in range(2)]
    stt["obf_free"] = [None, None]
    stt["hTs_free"] = [None, None]
    stt["bf_free"] = None
    stt["d"] = [P.dsem("hts0"), P.dsem("hts1")]
    stt["cast_tok"] = None
    return stt


def hT_tile(nc, P, Bbf, identb, stt, src, src_tok, t, hT_own, extra):
    s = t % 2
    g, tl = t // 4, t % 4
    hs = g % 2
    obf, hTs = stt["obf"], stt["hTs"]
    x1 = P.op("act", lambda e: e.copy(obf[s][:], src[:]), deps=[src_tok, stt["obf_free"][s]])
    stt["cast_tok"] = x1
    for kc in range(8):
        x2 = P.op("pe", lambda e, kc=kc: e.transpose(Bbf[:, kc * 128:(kc + 1) * 128], obf[s][:, kc * 128:(kc + 1) * 128], identb[:]),
                  deps=[x1, stt["bf_free"]] + extra, inc=(kc == 7))
    stt["obf_free"][s] = x2
    x3 = P.op("dve", lambda e: e.tensor_copy(hTs[hs][:, :, tl * 128:(tl + 1) * 128], Bbf[:].rearrange("p (a c) -> p a c", a=8)),
              deps=[x2] + ([stt["hTs_free"][hs]] if tl == 0 else []))
    stt["bf_free"] = x3
    if tl == 3:
        tw = P.dma("sp", stt["d"][hs], lambda e: e.dma_start(
            out=hT_own[:, g * 512:(g + 1) * 512].rearrange("(kc p) t -> p kc t", p=128), in_=hTs[hs][:]), deps=[x3])
        stt["hTs_free"][hs] = tw
    return x3


def emit_hT0(nc, P, Bbf, E):
    with contextlib.ExitStack() as st:
        identb = st.enter_context(nc.sbuf_tensor("identb_t0", [128, 128], BF16))
        xt = [st.enter_context(nc.sbuf_tensor("xt%d_t0" % i, [128, 1024], F32)) for i in range(2)]
        d_c = P.dsem("const")
        ct = P.dma("pool", d_c, lambda e: e.dma_start(out=identb[:], in_=E["ident"]))
        stt = hT_state(nc, P, st, "_t0")
        d_x = [P.dsem("h0"), P.dsem("h1")]
        xfree = [None, None]
        for t in range(NTT):
            s = t % 2
            tx = P.dma("sp", d_x[s], lambda e: e.dma_start(out=xt[s][:], in_=E["x_own"][t * 128:(t + 1) * 128, :]), deps=[xfree[s]])
            hT_tile(nc, P, Bbf, identb, stt, xt[s], tx, t, E["hT_own"], [ct])
            xfree[s] = stt["cast_tok"]
        barrier(P)


PAIRS = [[0, 1], [2, 3], [4, 5], [6, 7]]
NL = DEPTH


def allgather(nc, P, src, dst, kind):
    barrier(P)
    key = P.dsem("cc")
    for c in range(4):
        if kind == "hT":
            s_ap = src[c * 256:(c + 1) * 256, :]
            d_ap = dst[c].rearrange("r kk p t -> (r kk p) t")
        else:
            s_ap = src[c * 128:(c + 1) * 128, :]
            d_ap = dst[c].rearrange("r q t -> (r q) t")
        P.cc(key, lambda e: e.collective_compute("AllGather", ALU.bypass, replica_groups=PAIRS, ins=[s_ap.opt()], outs=[d_ap.opt()]))
    barrier(P)


def build_fused():
    nc = bass.Bass("TRN2", target_bir_lowering=False)
    E = {}

    def din(name, shape, dt=F32):
        E[name] = nc.dram_tensor(name, shape, dt, kind="ExternalInput").ap()

    def dint(name, shape, dt):
        E[name] = nc.dram_tensor(name, shape, dt, kind="Internal").ap()

    din("x_own", [NTOK, 1024])
    din("memT", [1024, N_MEM])
    din("wmk", [1024, 128])
    din("wmv", [1024, 128])
    din("wq", [NL, 1024, 512])
    din("wk", [NL, 1024, 384])
    din("wv", [NL, 1024, 384])
    din("wfgp", [2, 3, 1024, 512])
    din("bfg", [2, 32, 3])
    din("ident", [128, 128])
    din("negm", [128, 512])
    din("kaug", [36, SEQ])
    din("qaug", [36, SEQ])
    din("iq", [32, 16])
    din("sel", [32, 256])
    din("tri", [32, 32])
    din("cdec", [3, 32, 512])
    din("maskm", [128, 2048])
    din("idxm", [128, 64], I32)
    din("wo", [NL, 1024, 1024])
    for nm in ("ln1g", "ln1b", "ln2g", "ln2b"):
        din(nm, [NL, 128, 1024])
    din("wr", [NL, 1024, 32])
    din("rb", [NL, 128, 32])
    din("wgu", [NL, N_EXPERTS, 1024, 2048])
    din("bgu", [NL, N_EXPERTS, 2048])
    din("wdn", [NL, N_EXPERTS, 1024, 1024])
    din("bdn", [NL, N_EXPERTS, 1024])
    din("tstr", [128, 128])
    din("iotae", [128, 32])
    din("ecap", [128, 32])
    E["out"] = nc.dram_tensor("out", [NTOK, 1024], F32, kind="ExternalOutput").ap()
    dint("hT_own", [1024, NTOK], BF16)
    dint("hTp", [4, 2, 2, 128, NTOK], BF16)
    dint("OT_own", [512, SEQ], BF16)
    dint("OT_pair", [4, 2, 128, SEQ], BF16)
    dint("dscr", [2, 2, 2, SEQ], BF16)
    dint("H1", [NTOK, 1024], F32)
    dint("hres", [NTOK, 1024], F32)
    dint("Xs", [NSLOT, 1024], BF16)
    dint("Ys", [NSLOT, 1024], F32)

    with contextlib.ExitStack() as st:
        P = Prog(nc, st)
        B = [st.enter_context(nc.psum_tensor("B%d" % i, [128, 512], F32)) for i in range(7)]
        Bbf = st.enter_context(nc.psum_tensor("Bbf", [128, 1024], BF16))
        E["bc_reg"] = nc.gpsimd.to_reg(NSLOT - 1)
        E["bc_reg2"] = nc.gpsimd.to_reg(16383)
        emit_hT0(nc, P, Bbf, E)
        allgather(nc, P, E["hT_own"], E["hTp"], "hT")
        for L in range(NL):
            emit_attn(nc, P, B, Bbf, "moba" if L % 2 == 0 else "fox", E, L)
            allgather(nc, P, E["OT_own"], E["OT_pair"], "OT")
            emit_ffn(nc, P, B, Bbf, E, L)
            if L < NL - 1:
                allgather(nc, P, E["hT_own"], E["hTp"], "hT")
                P.new_epoch()
        barrier(P)
    return nc


def ffn_consts():
    if "ffn" in _CONST:
        return _CONST["ffn"]
    c = {}
    c["ident"] = np.eye(128, dtype=np.float32)
    k = np.arange(128)
    c["tstr"] = (k[:, None] < k[None, :]).astype(np.float32)
    c["iotae"] = np.ascontiguousarray(np.broadcast_to(np.arange(32, dtype=np.float32)[None, :], (128, 32)))
    c["ecap"] = np.ascontiguousarray(np.broadcast_to((np.arange(32, dtype=np.float32) * CAP)[None, :], (128, 32)))
    _CONST["ffn"] = c
    return c


def bc128(v):
    return np.ascontiguousarray(np.broadcast_to(np.asarray(v, np.float32).reshape(1, -1), (128, v.size)))


def ffn_inputs(h_tok, mT_tok, wo_l, ln1g, ln1b, ln2g, ln2b, wr_l, rb_l, wgu_l, bgu_l, wdn_l, bdn_l):
    m = dict(ffn_consts())
    m.update(h=h_tok, mT=mT_tok, wo=wo_l, ln1g=bc128(ln1g), ln1b=bc128(ln1b), ln2g=bc128(ln2g), ln2b=bc128(ln2b),
             wr=wr_l, rb=bc128(rb_l), wgu=wgu_l, bgu=bgu_l, wdn=wdn_l, bdn=bdn_l)
    return m


_FUSED = {}


def kernel(x, mem, w_in_moba, w_in_fox, b_fgate, w_mem_kv, w_o, ln1_g, ln1_b, router_w, router_b,
           w_gate_up, b_gate_up, w_down, b_down, ln2_g, ln2_b):
    f32 = lambda a: np.ascontiguousarray(np.asarray(a, dtype=np.float32))
    x, mem, w_mem_kv = f32(x), f32(mem), f32(w_mem_kv)
    w_in_moba, w_in_fox, b_fgate, w_o = f32(w_in_moba), f32(w_in_fox), f32(b_fgate), f32(w_o)
    SW = N_SELF * HEAD_DIM
    shared = dict(ffn_consts())
    ca = attn_consts("moba", 0)
    for k in ("ident", "negm", "kaug", "qaug", "iq", "sel", "maskm"):
        shared[k] = ca[k]
    shared["tri"] = attn_consts("fox", 0)["tri"]
    shared["wr"] = f32(router_w)[:NL]
    shared["rb"] = np.stack([bc128(f32(router_b)[l]) for l in range(NL)])
    for nm, v in (("ln1g", ln1_g), ("ln1b", ln1_b), ("ln2g", ln2_g), ("ln2b", ln2_b)):
        shared[nm] = np.stack([bc128(f32(v)[l]) for l in range(NL)])
    shared["wgu"], shared["bgu"] = f32(w_gate_up)[:NL], f32(b_gate_up)[:NL]
    shared["wdn"], shared["bdn"] = f32(w_down)[:NL], f32(b_down)[:NL]
    perm = np.zeros(1024, np.int64)
    for hp in range(4):
        for r in range(2):
            for q in range(128):
                hl, dd = 2 * hp + q // 64, q % 64
                g = (6 * r + hl) * 64 + dd if hl < 6 else 768 + (2 * r + hl - 6) * 64 + dd
                perm[(hp * 2 + r) * 128 + q] = g
    shared["wo"] = np.ascontiguousarray(w_o[:NL][:, perm, :])
    per_half = []
    for hh in range(2):
        d = {}
        heads = slice(6 * hh * 64, (6 * hh + 6) * 64)
        wq, wk, wv = [], [], []
        for l in range(NL):
            w_in = w_in_moba[l // 2] if l % 2 == 0 else w_in_fox[l // 2]
            wq.append(np.concatenate([w_in[:, 0:SW][:, heads], w_in[:, 3 * SW:3 * SW + 256][:, hh * 128:(hh + 1) * 128]], axis=1))
            wk.append(w_in[:, SW:2 * SW][:, heads])
            wv.append(w_in[:, 2 * SW:3 * SW][:, heads])
        d["wq"], d["wk"], d["wv"] = np.ascontiguousarray(np.stack(wq)), np.ascontiguousarray(np.stack(wk)), np.ascontiguousarray(np.stack(wv))
        wfgp = np.zeros((2, 3, 1024, 16, 32), np.float32)
        bfg = np.zeros((2, 32, 3), np.float32)
        for j in range(2):
            wfg = w_in_fox[j][:, 2560:2572]
            for pp in range(3):
                for r in range(2):
                    h = 6 * hh + 2 * pp + r
                    for i in range(16):
                        wfgp[j, pp, :, i, r * 16 + i] = wfg[:, h]
                    bfg[j, r * 16:(r + 1) * 16, pp] = b_fgate[j, h]
        d["wfgp"] = wfgp.reshape(2, 3, 1024, 512)
        d["bfg"] = bfg
        d["cdec"] = attn_consts("moba", hh)["cdec"]
        d["wmk"] = np.ascontiguousarray(w_mem_kv[:, 0:256][:, hh * 128:(hh + 1) * 128])
        d["wmv"] = np.ascontiguousarray(w_mem_kv[:, 256:512][:, hh * 128:(hh + 1) * 128])
        kc = np.arange(8)[None, :, None]
        p = np.arange(128)[:, None, None]
        g = np.arange(8)[None, None, :]
        d["idxm"] = np.ascontiguousarray((((kc * 128 + p) * 2 + hh) * 8 + g).reshape(128, 64).astype(np.int32))
        per_half.append(d)
    maps = []
    for c in range(8):
        b, hh = c // 2, c % 2
        m = dict(shared)
        m.update(per_half[hh])
        m["x_own"] = np.ascontiguousarray(x[b, hh * NTOK:(hh + 1) * NTOK])
        m["memT"] = np.ascontiguousarray(mem[b].T)
        maps.append(m)
    if NL not in _FUSED:
        _FUSED[NL] = build_fused()
    res = run_bass_kernel_spmd(_FUSED[NL], maps, core_ids=list(range(8))).results
    out = np.empty((BATCH, SEQ, D_MODEL), np.float32)
    for c in range(8):
        b, hh = c // 2, c % 2
        out[b, hh * NTOK:(hh + 1) * NTOK] = np.asarray(res[c]["out"])
    return out
```

```python
import contextlib
import math
import numpy as np
import ml_dtypes
import concourse.bass as bass
import concourse.mybir as mybir
from concourse.bass_utils import run_bass_kernel_spmd

F32 = mybir.dt.float32
BF16 = mybir.dt.bfloat16
I32 = mybir.dt.int32
U32 = mybir.dt.uint32
ALU = mybir.AluOpType
AF = mybir.ActivationFunctionType
AX = mybir.AxisListType

D_MODEL = 1024
BATCH = 4
SEQ = 8192
DEPTH = 4
HEAD_DIM = 64
N_SELF = 12
N_MEMH = 4
N_MEM = 256
N_EXPERTS = 32
TOP_K = 4
D_FF = 1024
SWIGLU_LIMIT = 7.0
SWIGLU_ALPHA = 1.702
ALPHA = (2 * DEPTH) ** 0.25
LN_EPS = 1e-5
NEG = -30000.0
KAUG = 100
NQT = SEQ // 512
NKT = SEQ // 128


class Prog:
    def __init__(self, nc, stack):
        self.nc = nc
        self.stack = stack
        self.engs = {"pe": nc.tensor, "act": nc.scalar, "dve": nc.vector, "pool": nc.gpsimd, "sp": nc.sync}
        self.sem = {}
        self.cnt = {}
        self.seen = {k: {} for k in self.engs}
        for k in self.engs:
            self.sem[k] = stack.enter_context(nc.semaphore("s_" + k))
            self.cnt[k] = 0
        self.ndsem = 0

    def dsem(self, name=None):
        if name is not None and ("dn_" + name) in self.sem:
            return "dn_" + name
        self.ndsem += 1
        key = "dn_" + name if name is not None else "d%d" % self.ndsem
        self.sem[key] = self.stack.enter_context(self.nc.semaphore(key))
        self.cnt[key] = 0
        return key

    def wait(self, eng, tok):
        if tok is None:
            return
        key, val = tok
        if eng == "pe" and key == "pe":
            return
        if self.seen[eng].get(key, 0) >= val:
            return
        self.engs[eng].wait_ge(self.sem[key], val)
        self.seen[eng][key] = val

    def op(self, eng, fn, deps=(), inc=True):
        for d in deps:
            self.wait(eng, d)
        inst = fn(self.engs[eng])
        if inc:
            self.cnt[eng] += 1
            inst.then_inc(self.sem[eng], 1)
            return (eng, self.cnt[eng])
        return None

    def new_epoch(self):
        self.epoch = getattr(self, "epoch", 0) + 1
        for k in self.engs:
            self.sem[k] = self.stack.enter_context(self.nc.semaphore("s_%s_e%d" % (k, self.epoch)))
            self.cnt[k] = 0
            for e in self.engs:
                self.seen[e].pop(k, None)

    def cc(self, dkey, fn, deps=()):
        for d in deps:
            self.wait("pool", d)
        inst = fn(self.engs["pool"])
        self.cnt[dkey] += 1
        inst.then_inc(self.sem[dkey])
        return (dkey, self.cnt[dkey])

    def dma(self, q, dkey, fn, deps=()):
        for d in deps:
            self.wait(q, d)
        inst = fn(self.engs[q])
        self.cnt[dkey] += 16
        inst.then_inc(self.sem[dkey], 16)
        return (dkey, self.cnt[dkey])


def _alibi_slopes(n):
    def pow2(m):
        start = 2.0 ** (-(2.0 ** -(math.log2(m) - 3)))
        return [start * start ** i for i in range(m)]
    if math.log2(n).is_integer():
        s = pow2(n)
    else:
        c = 2 ** math.floor(math.log2(n))
        s = pow2(c) + pow2(2 * c)[0::2][: n - c]
    return np.array(s, dtype=np.float32)


def emit_attn(nc, P, B, Bbf, kind, E, L):
    fox = kind == "fox"
    j = L // 2
    sfx = "_a%d" % L
    hTp = E["hTp"]
    wq, wk, wv = E["wq"][L], E["wk"][L], E["wv"][L]
    memT, wmk, wmv = E["memT"], E["wmk"], E["wmv"]
    ident_d, negm_d, kaug_d, qaug_d, iq_d, sel_d = E["ident"], E["negm"], E["kaug"], E["qaug"], E["iq"], E["sel"]
    if fox:
        wfgp, bfg, tri_d = E["wfgp"][j], E["bfg"][j], E["tri"]
    else:
        cdec, maskm_d = E["cdec"], E["maskm"]
    OT, dscr = E["OT_own"], E["dscr"]

    with contextlib.ExitStack() as st:

        def sb(name, shape, dt):
            return st.enter_context(nc.sbuf_tensor(name + sfx, shape, dt))

        QT = [sb("QT%d" % r, [KAUG, SEQ], BF16) for r in range(2)]
        KT = [sb("KT%d" % r, [KAUG, SEQ], BF16) for r in range(2)]
        VA = sb("VA", [128, NKT, 2, 128], BF16)
        hTr = [sb("hTr%d" % s, [128, 8, 512], BF16) for s in range(2)]
        wq_s = sb("wq_s", [128, 8, 128], BF16)
        wk_s = sb("wk_s", [128, 8, 128], BF16)
        wv_s = sb("wv_s", [128, 8, 128], BF16)
        memT_s = sb("memT_s", [128, 8, N_MEM], BF16)
        wmk_s = sb("wmk_s", [128, 8, 128], BF16)
        wmv_s = sb("wmv_s", [128, 8, 128], BF16)
        PT = [sb("PT%d" % s, [128, 512], BF16) for s in range(4)]
        TMP = [sb("TMP%d" % s, [128, 512], F32) for s in range(2)]
        Dt = [sb("Dt%d" % r, [128, NQT * NKT], F32) for r in range(2)]
        negm = sb("negm_s", [128, 512], F32)
        ident = sb("ident_s", [128, 128], BF16)
        OS = [sb("OS%d" % s, [64, 512], BF16) for s in range(2)]
        RR = [sb("RR%d" % s, [64, 512], F32) for s in range(2)]
        iq = sb("iq_s", [32, 16], F32)
        sel = sb("sel_s", [32, 256], F32)
        Cc = sb("Cc", [32, 512], F32)
        Z1 = sb("Z1", [32, 512], F32)
        RQ = sb("RQ", [32, 2, 512], BF16)
        RK = sb("RK", [32, 2, 512], BF16)
        refs = sb("refs", [32, 80], F32)
        bq_s = sb("bq_s", [128, 16], F32)
        if fox:
            wfg_s = sb("wfg_s", [128, 8, 512], BF16)
            bfg_s = sb("bfg_s", [32, 3], F32)
            tri = sb("tri_s", [32, 32], F32)
            ones32 = sb("ones32", [32, 512], F32)
            offs = sb("offs", [32, 1], F32)
        else:
            maskm = sb("maskm_s", [128, 2048], F32)
            kbar = [sb("kbar%d" % r, [64, 32], BF16) for r in range(2)]
            kbf = sb("kbf", [64, 32], F32)
            gp = [sb("gp%d" % s, [128, 128], F32) for s in range(2)]
            m8 = [sb("m8_%d" % s, [128, 4, 8], F32) for s in range(2)]
            thr = [sb("thr%d" % s, [128, 4], F32) for s in range(2)]
            TB = [sb("TB%d" % s, [128, 4, 96], BF16) for s in range(2)]
        d_c = P.dsem("const")
        toks = []
        toks.append(P.dma("pool", d_c, lambda e: e.dma_start(out=ident[:], in_=ident_d)))
        toks.append(P.dma("sp", d_c, lambda e: e.dma_start(out=negm[:], in_=negm_d)))
        toks.append(P.dma("sp", d_c, lambda e: e.dma_start(out=iq[:], in_=iq_d)))
        toks.append(P.dma("sp", d_c, lambda e: e.dma_start(out=sel[:], in_=sel_d)))
        for r in range(2):
            toks.append(P.dma("pool", d_c, lambda e, r=r: e.dma_start(out=KT[r][64:100, :], in_=kaug_d)))
            toks.append(P.dma("pool", d_c, lambda e, r=r: e.dma_start(out=QT[r][64:100, :], in_=qaug_d)))
        toks.append(P.dma("pool", d_c, lambda e: e.dma_start(
            out=memT_s[:], in_=memT.rearrange("(kc p) t -> p kc t", p=128))))
        toks.append(P.dma("pool", d_c, lambda e: e.dma_start(
            out=wmk_s[:], in_=wmk.rearrange("(kc p) t -> p kc t", p=128))))
        toks.append(P.dma("pool", d_c, lambda e: e.dma_start(
            out=wmv_s[:], in_=wmv.rearrange("(kc p) t -> p kc t", p=128))))
        if fox:
            toks.append(P.dma("sp", d_c, lambda e: e.dma_start(out=bfg_s[:], in_=bfg)))
            toks.append(P.dma("sp", d_c, lambda e: e.dma_start(out=tri[:], in_=tri_d)))
        else:
            toks.append(P.dma("sp", d_c, lambda e: e.dma_start(out=maskm[:], in_=maskm_d)))
        const_tok = toks[-1]
        t_va = P.op("pool", lambda e: e.memset(VA[:], 1.0))
        if fox:
            t_ones = P.op("pool", lambda e: e.memset(ones32[:], 1.0))
        else:
            for s in range(2):
                t_tb = P.op("pool", lambda e, s=s: e.memset(TB[s][:], 0.0))

        d_h = [P.dsem("h%d" % s) for s in range(2)]
        d_w = P.dsem("w")
        d_o = [P.dsem("o%d" % s) for s in range(2)]
        d_dec = P.dsem("dec")
        d_dec2 = P.dsem("dec2")

        hT_free = [None, None]
        w_free = None
        bank_free = {i: None for i in range(7)}
        bank_free["bf"] = None
        PT_free = [None] * 4
        TMP_free = [None] * 2
        OS_free = [None] * 2
        RR_free = [None] * 2
        qk_last_reader = [None, None]
        unit = 0
        ctrs = {"qk": 0, "ex": 0, "tx": 0}
        TB_free = [None, None]
        out_toks = []
        dt_free = [None, None]

        for p in range(4):
            mem_pass = p == 3
            wdeps = [w_free]
            tw = P.dma("pool", d_w, lambda e, p=p: e.dma_start(
                out=wq_s[:], in_=wq[:, p * 128:(p + 1) * 128].rearrange("(kc p) t -> p kc t", p=128)), deps=wdeps)
            if not mem_pass:
                tw = P.dma("pool", d_w, lambda e, p=p: e.dma_start(
                    out=wk_s[:], in_=wk[:, p * 128:(p + 1) * 128].rearrange("(kc p) t -> p kc t", p=128)))
                tw = P.dma("pool", d_w, lambda e, p=p: e.dma_start(
                    out=wv_s[:], in_=wv[:, p * 128:(p + 1) * 128].rearrange("(kc p) t -> p kc t", p=128)))
                if fox:
                    tw = P.dma("pool", d_w, lambda e, p=p: e.dma_start(
                        out=wfg_s[:], in_=wfgp[p].rearrange("(kc p) t -> p kc t", p=128)))

            last_ev = {}
            for i in range(NQT):
                s = i % 2
                for c4 in range(4):
                    th = P.dma("pool", d_h[s], lambda e, i=i, s=s, c4=c4: e.dma_start(
                        out=hTr[s][:, 2 * c4:2 * c4 + 2, :],
                        in_=hTp[c4, i // 8, :, :, (i % 8) * 512:(i % 8 + 1) * 512].rearrange("kk p t -> p kk t")),
                        deps=[hT_free[s]])
                cols = slice(i * 512, (i + 1) * 512)
                bq = B[0 + s]
                for kc in range(8):
                    tq = P.op("pe", lambda e, kc=kc, bq=bq, s=s: e.matmul(
                        bq[:], wq_s[:, kc, :], hTr[s][:, kc, :], start=(kc == 0), stop=(kc == 7)),
                        deps=[th, tw, bank_free[0 + s]] + ([qk_last_reader[0], qk_last_reader[1]] if kc == 0 else []),
                        inc=(kc == 7))
                e0 = P.op("act", lambda e, bq=bq, cols=cols: e.mul(QT[0][0:64, cols], bq[0:64, :], 0.125), deps=[tq, const_tok])
                e1 = P.op("act", lambda e, bq=bq, cols=cols: e.mul(QT[1][0:64, cols], bq[64:128, :], 0.125), deps=[tq])
                bank_free[0 + s] = e1
                last_ev["q"] = e1
                if not mem_pass:
                    bk = B[2 + s]
                    for kc in range(8):
                        tk = P.op("pe", lambda e, kc=kc, bk=bk, s=s: e.matmul(
                            bk[:], wk_s[:, kc, :], hTr[s][:, kc, :], start=(kc == 0), stop=(kc == 7)),
                            deps=[bank_free[2 + s]], inc=(kc == 7))
                    e2 = P.op("dve", lambda e, bk=bk, cols=cols: e.tensor_copy(KT[0][0:64, cols], bk[0:64, :]), deps=[tk, const_tok])
                    e3 = P.op("dve", lambda e, bk=bk, cols=cols: e.tensor_copy(KT[1][0:64, cols], bk[64:128, :]), deps=[tk])
                    bank_free[2 + s] = e3
                    last_ev["k"] = e3
                    bv = B[4 + s]
                    for sub in range(4):
                        for kc in range(8):
                            tv = P.op("pe", lambda e, kc=kc, bv=bv, s=s, sub=sub: e.matmul(
                                bv[:, sub * 128:(sub + 1) * 128], hTr[s][:, kc, sub * 128:(sub + 1) * 128], wv_s[:, kc, :],
                                start=(kc == 0), stop=(kc == 7)),
                                deps=[bank_free[4 + s]], inc=(sub == 3 and kc == 7))
                    bv3 = bv[:].rearrange("p (a c) -> p a c", a=4)
                    e4 = P.op("dve", lambda e, bv3=bv3, i=i: e.tensor_copy(VA[:, 4 * i:4 * i + 4, 0, 0:64], bv3[:, :, 0:64]), deps=[tv, t_va])
                    e5 = P.op("act", lambda e, bv3=bv3, i=i: e.copy(VA[:, 4 * i:4 * i + 4, 1, 0:64], bv3[:, :, 64:128]), deps=[tv, t_va, e4])
                    bank_free[4 + s] = e5
                    last_ev["v0"] = e4
                    last_ev["v1"] = e5
                    if fox:
                        for kc in range(8):
                            tf = P.op("pe", lambda e, kc=kc, s=s, i=i: e.matmul(
                                B[6][0:32, :], wfg_s[:, kc, i * 32:(i + 1) * 32], hTr[s][:, kc, :],
                                start=(i == 0 and kc == 0), stop=(i == NQT - 1 and kc == 7)),
                                deps=[bank_free[6]] if (i == 0 and kc == 0) else [], inc=(kc == 7))
                        hT_free[s] = tf
                    else:
                        hT_free[s] = tv
                else:
                    hT_free[s] = tq
            w_free = hT_free[(NQT - 1) % 2]
            proj_done = [last_ev[k] for k in last_ev]

            if mem_pass:
                for kc in range(8):
                    tmk = P.op("pe", lambda e, kc=kc: e.matmul(
                        B[2][:, 0:N_MEM], wmk_s[:, kc, :], memT_s[:, kc, :], start=(kc == 0), stop=(kc == 7)),
                        deps=[bank_free[2], const_tok], inc=(kc == 7))
                e2 = P.op("dve", lambda e: e.tensor_copy(KT[0][0:64, 0:N_MEM], B[2][0:64, 0:N_MEM]), deps=[tmk])
                e3 = P.op("dve", lambda e: e.tensor_copy(KT[1][0:64, 0:N_MEM], B[2][64:128, 0:N_MEM]), deps=[tmk])
                bank_free[2] = e3
                for mt in range(2):
                    for kc in range(8):
                        tmv = P.op("pe", lambda e, kc=kc, mt=mt: e.matmul(
                            B[4][:, mt * 128:(mt + 1) * 128], memT_s[:, kc, mt * 128:(mt + 1) * 128], wmv_s[:, kc, :],
                            start=(kc == 0), stop=(kc == 7)),
                            deps=[bank_free[4]], inc=(mt == 1 and kc == 7))
                bv3 = B[4][:, 0:256].rearrange("p (a c) -> p a c", a=2)
                e4 = P.op("dve", lambda e: e.tensor_copy(VA[:, 0:2, 0, 0:64], bv3[:, :, 0:64]), deps=[tmv])
                e5 = P.op("dve", lambda e: e.tensor_copy(VA[:, 0:2, 1, 0:64], bv3[:, :, 64:128]), deps=[tmv])
                bank_free[4] = e5
                proj_done += [e3, e5]

            if not mem_pass:
                if fox:
                    a1 = P.op("act", lambda e, p=p: e.activation(
                        out=Z1[:], in_=B[6][0:32, :], func=AF.Sigmoid, bias=bfg_s[:, p:p + 1], scale=1.0),
                        deps=[tf, const_tok, dt_free[0], dt_free[1]])
                    bank_free[6] = a1
                    a2 = P.op("act", lambda e: e.activation(out=Z1[:], in_=Z1[:], func=AF.Ln), deps=[a1])
                    c1 = P.op("dve", lambda e: e.tensor_tensor_scan(
                        out=Cc[:], data0=ones32[:], data1=Z1[:], initial=0.0, op0=ALU.mult, op1=ALU.add),
                        deps=[a2, t_ones])
                    m1 = P.op("pe", lambda e: e.matmul(B[5][0:32, 0:1], tri[:], Cc[:, 511:512], start=True, stop=True),
                              deps=[c1, bank_free[5], const_tok])
                    c2 = P.op("dve", lambda e: e.tensor_copy(offs[:], B[5][0:32, 0:1]), deps=[m1])
                    c3 = P.op("dve", lambda e: e.tensor_scalar(
                        out=Cc[:], in0=Cc[:], scalar1=offs[:, 0:1], scalar2=None, op0=ALU.add), deps=[c2])
                    bank_free[5] = c2
                    tc_ready = c3
                else:
                    tcd = P.dma("sp", d_dec2, lambda e, p=p: e.dma_start(out=Cc[:], in_=cdec[p]),
                                deps=[dt_free[0], dt_free[1]])
                    tc_ready = tcd
                c4 = P.op("dve", lambda e: e.tensor_scalar(
                    out=Z1[:], in0=Cc[:], scalar1=Cc[:, 255:256], scalar2=None, op0=ALU.subtract), deps=[tc_ready])
                c5 = P.op("dve", lambda e: e.tensor_copy(RQ[:, 0, :], Z1[:]), deps=[c4])
                c6 = P.op("dve", lambda e: e.tensor_tensor(out=Z1[:], in0=Z1[:], in1=RQ[:, 0, :], op=ALU.subtract), deps=[c5])
                c7 = P.op("dve", lambda e: e.tensor_copy(RQ[:, 1, :], Z1[:]), deps=[c6])
                ck = c7
                for jj in range(4):
                    ck = P.op("dve", lambda e, jj=jj: e.tensor_scalar(
                        out=Z1[:, jj * 128:(jj + 1) * 128], in0=Cc[:, jj * 128:(jj + 1) * 128],
                        scalar1=Cc[:, jj * 128 + 63:jj * 128 + 64], scalar2=-1.0, op0=ALU.subtract, op1=ALU.mult), deps=[ck])
                c8 = P.op("dve", lambda e: e.tensor_copy(RK[:, 0, :], Z1[:]), deps=[ck])
                c9 = P.op("dve", lambda e: e.tensor_tensor(out=Z1[:], in0=Z1[:], in1=RK[:, 0, :], op=ALU.subtract), deps=[c8])
                c10 = P.op("dve", lambda e: e.tensor_copy(RK[:, 1, :], Z1[:]), deps=[c9])
                c11 = P.op("dve", lambda e: e.tensor_scalar(
                    out=refs[:, 0:16], in0=iq[:], scalar1=Cc[:, 255:256], scalar2=None, op0=ALU.mult), deps=[c10, const_tok])
                rk3 = refs[:, 16:80].rearrange("p (i j) -> p i j", j=4)
                for jj in range(4):
                    c11 = P.op("dve", lambda e, jj=jj: e.tensor_scalar(
                        out=rk3[:, :, jj], in0=iq[:], scalar1=Cc[:, jj * 128 + 63:jj * 128 + 64], scalar2=None,
                        op0=ALU.mult), deps=[c11])
                dts = []
                for r in range(2):
                    t1 = P.dma("sp", d_dec, lambda e, r=r: e.dma_start(
                        out=dscr[r, 0].rearrange("h (i t) -> i h t", i=16), in_=RQ[r * 16:(r + 1) * 16, :, :]), deps=[c11])
                    t2 = P.dma("sp", d_dec, lambda e, r=r: e.dma_start(
                        out=dscr[r, 1].rearrange("h (i t) -> i h t", i=16), in_=RK[r * 16:(r + 1) * 16, :, :]))
                    dts.append(t2)
                dts2 = []
                for r in range(2):
                    t3 = P.dma("sp", d_dec, lambda e, r=r: e.dma_start(out=QT[r][96:98, :], in_=dscr[r, 0]),
                               deps=[dts[-1], qk_last_reader[r]])
                    t4 = P.dma("sp", d_dec, lambda e, r=r: e.dma_start(out=KT[r][98:100, :], in_=dscr[r, 1]))
                    dts2.append(t4)
                dec_rows_tok = dts2[-1]
                for r in range(2):
                    m2 = P.op("pe", lambda e, r=r: e.matmul(
                        B[5][:, 0:80], sel[:, r * 128:(r + 1) * 128], refs[:], start=True, stop=True),
                        deps=[c11, bank_free[5], const_tok])
                    c12 = P.op("dve", lambda e: e.tensor_copy(bq_s[:], B[5][:, 0:16]), deps=[m2])
                    cl = c12
                    for i in range(NQT):
                        cl = P.op("dve", lambda e, r=r, i=i: e.tensor_scalar(
                            out=Dt[r][:, i * NKT:(i + 1) * NKT], in0=B[5][:, 16:80], scalar1=-1.0, scalar2=bq_s[:, i:i + 1],
                            op0=ALU.mult, op1=ALU.add), deps=[cl], inc=(i == NQT - 1))
                    bank_free[5] = cl
                    dt_ready = cl
                dt_ready_all = dt_ready
            else:
                dec_rows_tok = None
                dt_ready_all = None

            gate_done = []
            if (not mem_pass) and (not fox):
                for r in range(2):
                    k1 = P.op("dve", lambda e, r=r: e.tensor_reduce(
                        out=kbf[:], in_=KT[r][0:64, :].rearrange("p (n l) -> p n l", l=256), axis=AX.X, op=ALU.add),
                        deps=proj_done)
                    k2 = P.op("dve", lambda e, r=r: e.tensor_scalar(
                        out=kbar[r][:], in0=kbf[:], scalar1=1.0 / 256.0, scalar2=None, op0=ALU.mult), deps=[k1])
                    for i in range(NQT):
                        s = i % 2
                        for sub in range(4):
                            c0 = i * 512 + sub * 128
                            g1 = P.op("pe", lambda e, r=r, c0=c0, sub=sub: e.matmul(
                                B[5][:, sub * 32:(sub + 1) * 32], QT[r][0:64, c0:c0 + 128], kbar[r][:], start=True, stop=True),
                                deps=[k2, bank_free[5]] + proj_done, inc=(sub == 3))
                        g2 = P.op("dve", lambda e, s=s, i=i: e.tensor_tensor(
                            out=gp[s][:], in0=B[5][:, 0:128], in1=maskm[:, i * 128:(i + 1) * 128], op=ALU.add),
                            deps=[g1, const_tok])
                        bank_free[5] = g2
                        g3 = g2
                        for sub in range(4):
                            g3 = P.op("dve", lambda e, s=s, sub=sub: e.max(m8[s][:, sub, :], gp[s][:, sub * 32:(sub + 1) * 32]),
                                      deps=[g3])
                        g4 = P.op("dve", lambda e, s=s: e.tensor_scalar(
                            out=thr[s][:], in0=m8[s][:, :, 3], scalar1=-1e29, scalar2=None, op0=ALU.max), deps=[g3])
                        g5 = g4
                        for sub in range(4):
                            g5 = P.op("dve", lambda e, s=s, sub=sub: e.tensor_scalar(
                                out=TB[s][:, sub, 64:96], in0=gp[s][:, sub * 32:(sub + 1) * 32],
                                scalar1=thr[s][:, sub:sub + 1], scalar2=NEG, op0=ALU.is_lt, op1=ALU.mult),
                                deps=[g5, t_tb, TB_free[s]])
                        for sub in range(4):
                            g6 = P.op("pe", lambda e, s=s, sub=sub: e.transpose(
                                Bbf[0:96, (s * 4 + sub) * 128:(s * 4 + sub + 1) * 128], TB[s][:, sub, :], ident[:]),
                                deps=[g5, bank_free["bf"], const_tok], inc=(sub == 3))
                        g7 = P.op("act", lambda e, r=r, i=i, s=s: e.copy(QT[r][64:96, i * 512:(i + 1) * 512], Bbf[64:96, s * 512:(s + 1) * 512]),
                            deps=[g6])
                        TB_free[s] = g6
                        bank_free["bf"] = g7
                        gate_done = [g7]

            att_deps = proj_done + gate_done + [dec_rows_tok, dt_ready_all, const_tok]
            nk_rows = 64 if mem_pass else KAUG
            for r in range(2):
                hl = 2 * p + r
                for i in range(NQT):
                    u = unit
                    unit += 1
                    ob = B[3 + (u % 2)]
                    ntile = 2 if mem_pass else 4 * (i + 1)
                    qk_tok = [None] * ntile
                    ex_tok = [None] * ntile
                    pv_tok = None
                    sidx = [None] * ntile

                    def emit_qk(j):
                        nonlocal att_deps
                        sl = emit_qk.ctr % 3
                        emit_qk.ctr += 1
                        sidx[j] = sl
                        jj = j - 4 * i
                        c0 = 128 * jj if ((not mem_pass) and jj >= 0) else 0
                        t = P.op("pe", lambda e: e.matmul(
                            B[sl][:, c0:512], KT[r][0:nk_rows, j * 128:(j + 1) * 128],
                            QT[r][0:nk_rows, i * 512 + c0:(i + 1) * 512], start=True, stop=True),
                            deps=[bank_free[sl]] + att_deps)
                        att_deps = []
                        qk_tok[j] = t

                    def emit_exp(j):
                        sl = sidx[j]
                        pt = emit_exp.ctr % 4
                        emit_exp.ctr += 1
                        jj = j - 4 * i
                        diag = (not mem_pass) and jj >= 0
                        c0 = 128 * jj if diag else 0
                        bias = 0.0 if mem_pass else Dt[r][:, i * NKT + j:i * NKT + j + 1]
                        if diag:
                            ts = emit_exp.tctr % 2
                            emit_exp.tctr += 1
                            t1 = P.op("dve", lambda e: e.tensor_tensor(
                                out=TMP[ts][:, c0:512], in0=B[sl][:, c0:512], in1=negm[:, 0:512 - c0], op=ALU.add),
                                deps=[qk_tok[j], TMP_free[ts]])
                            bank_free[sl] = t1
                            t2 = P.op("act", lambda e: e.activation(
                                out=PT[pt][:, c0:512], in_=TMP[ts][:, c0:512], func=AF.Exp, bias=bias, scale=1.0),
                                deps=[t1, PT_free[pt]])
                            TMP_free[ts] = t2
                        else:
                            t2 = P.op("act", lambda e: e.activation(
                                out=PT[pt][:, :], in_=B[sl][:, :], func=AF.Exp, bias=bias, scale=1.0),
                                deps=[qk_tok[j], PT_free[pt]])
                            bank_free[sl] = t2
                        ex_tok[j] = (t2, pt, c0)

                    def emit_pv(j):
                        nonlocal pv_tok
                        t2, pt, c0 = ex_tok[j]
                        t = P.op("pe", lambda e: e.matmul(
                            ob[:, c0:512], VA[:, j, r, :], PT[pt][:, c0:512], start=(j == 0), stop=(j == ntile - 1)),
                            deps=[t2] + ([bank_free[3 + (u % 2)]] if j == 0 else []))
                        PT_free[pt] = t
                        pv_tok = t

                    emit_qk.ctr = ctrs["qk"]
                    emit_exp.ctr = ctrs["ex"]
                    emit_exp.tctr = ctrs["tx"]
                    emit_qk(0)
                    for j in range(ntile):
                        if j + 1 < ntile:
                            emit_qk(j + 1)
                        emit_exp(j)
                        emit_pv(j)
                    ctrs["qk"] = emit_qk.ctr
                    ctrs["ex"] = emit_exp.ctr
                    ctrs["tx"] = emit_exp.tctr
                    qk_last_reader[r] = pv_tok
                    os_ = u % 2
                    n1 = P.op("dve", lambda e: e.reciprocal(RR[os_][:], ob[64:128, :]), deps=[pv_tok, RR_free[os_]])
                    n2 = P.op("dve", lambda e: e.tensor_tensor(out=OS[os_][:], in0=ob[0:64, :], in1=RR[os_][:], op=ALU.mult),
                              deps=[n1, OS_free[os_]])
                    bank_free[3 + (u % 2)] = n2
                    RR_free[os_] = n2
                    to = P.dma("sp", d_o[os_], lambda e: e.dma_start(
                        out=OT[hl * 64:(hl + 1) * 64, i * 512:(i + 1) * 512], in_=OS[os_][:]), deps=[n2])
                    OS_free[os_] = to
                    out_toks.append(to)
                dt_free[r] = qk_last_reader[r]
        barrier(P)


_CONST = {}


def attn_consts(kind, hh):
    key = (kind, hh)
    if key in _CONST:
        return _CONST[key]
    c = {}
    c["ident"] = np.eye(128, dtype=np.float32)
    p = np.arange(128)[:, None]
    f = np.arange(512)[None, :]
    c["negm"] = np.where(f >= p, 0.0, NEG).astype(np.float32)
    t = np.arange(SEQ)
    kaug = np.zeros((36, SEQ), np.float32)
    kaug[t // 256, t] = 1.0
    kaug[32:34] = 1.0
    c["kaug"] = kaug
    qaug = np.zeros((36, SEQ), np.float32)
    qaug[34:36] = 1.0
    c["qaug"] = qaug
    k = np.arange(32)
    c["iq"] = (k[:, None] % 16 == np.arange(16)[None, :]).astype(np.float32)
    sel = np.zeros((32, 256), np.float32)
    for r in range(2):
        sel[r * 16:(r + 1) * 16, r * 128:(r + 1) * 128] = 1.0
    c["sel"] = sel
    if kind == "fox":
        c["tri"] = ((k[:, None] // 16 == k[None, :] // 16) & (k[:, None] < k[None, :])).astype(np.float32)
    else:
        slopes = _alibi_slopes(N_SELF)
        cdec = np.zeros((3, 32, 512), np.float32)
        for pp in range(3):
            for r in range(2):
                h = 6 * hh + 2 * pp + r
                for i in range(16):
                    cdec[pp, r * 16 + i] = -slopes[h] * (i * 512 + np.arange(512, dtype=np.float32))
        c["cdec"] = cdec
        mm = np.zeros((64, 32), np.float32)
        for s in range(64):
            qb = s // 2
            mm[s, :] = np.where(np.arange(32) < qb, 0.0, np.where(np.arange(32) == qb, 1e30, -1e30))
        c["maskm"] = np.ascontiguousarray(np.broadcast_to(mm.reshape(1, 2048), (128, 2048))).astype(np.float32)
    _CONST[key] = c
    return c


def attn_inputs(kind, hh, hT_b, memT_b, w_in, b_fg, w_mem_kv):
    SW = N_SELF * HEAD_DIM
    heads = slice(6 * hh * 64, (6 * hh + 6) * 64)
    m = dict(attn_consts(kind, hh))
    wq_self = w_in[:, 0:SW][:, heads]
    wq_mem = w_in[:, 3 * SW:3 * SW + 256][:, hh * 128:(hh + 1) * 128]
    m["wq"] = np.ascontiguousarray(np.concatenate([wq_self, wq_mem], axis=1))
    m["wk"] = np.ascontiguousarray(w_in[:, SW:2 * SW][:, heads])
    m["wv"] = np.ascontiguousarray(w_in[:, 2 * SW:3 * SW][:, heads])
    m["hT"] = hT_b
    m["memT"] = memT_b
    m["wmk"] = np.ascontiguousarray(w_mem_kv[:, 0:256][:, hh * 128:(hh + 1) * 128])
    m["wmv"] = np.ascontiguousarray(w_mem_kv[:, 256:512][:, hh * 128:(hh + 1) * 128])
    if kind == "fox":
        wfg = w_in[:, 2560:2572]
        wfgp = np.zeros((3, 1024, 16, 32), np.float32)
        bfg = np.zeros((32, 3), np.float32)
        for pp in range(3):
            for r in range(2):
                h = 6 * hh + 2 * pp + r
                for i in range(16):
                    wfgp[pp, :, i, r * 16 + i] = wfg[:, h]
                bfg[r * 16:(r + 1) * 16, pp] = b_fg[h]
        m["wfgp"] = wfgp.reshape(3, 1024, 512)
        m["bfg"] = bfg
    return m


NTOK = 4096
NTT = NTOK // 128
CAP = 640
NSLOT = N_EXPERTS * CAP
PART = 320


def barrier(P):
    keys = list(P.cnt.keys())
    for e in P.engs:
        for k in keys:
            if P.cnt[k] > 0:
                P.wait(e, (k, P.cnt[k]))


def emit_ffn(nc, P, B, Bbf, E, L):
    sfx = "_f%d" % L
    h_in = E["x_own"] if L == 0 else E["hres"]
    out = E["out"] if L == NL - 1 else E["hres"]
    emit_hT = L < NL - 1
    OTv = E["OT_pair"].rearrange("c r q (h g t) -> (c r q h g) t", h=2, g=8)
    idxm_d = E["idxm"]
    wo, ln1g, ln1b, ln2g, ln2b = E["wo"][L], E["ln1g"][L], E["ln1b"][L], E["ln2g"][L], E["ln2b"][L]
    wr, rb = E["wr"][L], E["rb"][L]
    wgu, bgu, wdn, bdn = E["wgu"][L], E["bgu"][L], E["wdn"][L], E["bdn"][L]
    ident_d, tstr_d, iota_d, ecap_d = E["ident"], E["tstr"], E["iotae"], E["ecap"]
    H1, Xs, Ys, hT_own = E["H1"], E["Xs"], E["Ys"], E["hT_own"]
    bc_reg, bc_reg2 = E["bc_reg"], E["bc_reg2"]

    with contextlib.ExitStack() as st:
        def sbp(name, shape, dt):
            return st.enter_context(nc.sbuf_tensor(name + sfx, shape, dt))

        gates = sbp("gates", [128, NTT, 4], F32)
        slots = sbp("slots", [128, NTT * 4], I32)
        identb = sbp("identb", [128, 128], BF16)
        identf = sbp("identf", [128, 128], F32)
        tstr = sbp("tstr_s", [128, 128], F32)
        onesf = sbp("onesf", [128, 128], F32)
        iotae = sbp("iotae_s", [128, 32], F32)
        ecap = sbp("ecap_s", [128, 32], F32)
        onesb = sbp("onesb", [1, 128], BF16)
        idxm = sbp("idxm_s", [128, 64], I32)

        d_c = P.dsem("const")
        P.dma("pool", d_c, lambda e: e.dma_start(out=identb[:], in_=ident_d))
        P.dma("sp", d_c, lambda e: e.dma_start(out=identf[:], in_=ident_d))
        P.dma("sp", d_c, lambda e: e.dma_start(out=tstr[:], in_=tstr_d))
        P.dma("sp", d_c, lambda e: e.dma_start(out=iotae[:], in_=iota_d))
        P.dma("sp", d_c, lambda e: e.dma_start(out=idxm[:], in_=idxm_d))
        const_tok = P.dma("sp", d_c, lambda e: e.dma_start(out=ecap[:], in_=ecap_d))
        t_ones = P.op("pool", lambda e: e.memset(onesf[:], 1.0))
        t_ones = P.op("pool", lambda e: e.memset(onesb[:], 1.0))

        with contextlib.ExitStack() as s1:
            def sb(name, shape, dt):
                return s1.enter_context(nc.sbuf_tensor(name + sfx, shape, dt))
            wo_s = sb("wo_s", [128, 8, 1024], BF16)
            g1 = sb("g1", [128, 1024], F32)
            b1 = sb("b1", [128, 1024], F32)
            wr_s = sb("wr_s", [128, 8, 32], BF16)
            rb_s = sb("rb_s", [128, 32], F32)
            mTr = [sb("mTr%d" % i, [128, 8, 512], BF16) for i in range(2)]
            htr = [sb("htr%d" % i, [128, 1024], F32) for i in range(2)]
            rt = [sb("rt%d" % i, [128, 1024], F32) for i in range(2)]
            h1r = [sb("h1r%d" % i, [128, 1024], F32) for i in range(2)]
            h1b = [sb("h1b%d" % i, [128, 1024], BF16) for i in range(3)]
            h1T = [sb("h1T%d" % i, [128, 8, 128], BF16) for i in range(2)]
            stats = [sb("stats%d" % i, [128, 2, 6], F32) for i in range(2)]
            mv = [sb("mv%d" % i, [128, 2], F32) for i in range(2)]
            rstd = [sb("rstd%d" % i, [128, 1], F32) for i in range(2)]
            Lg = [sb("Lg%d" % i, [128, 32], F32) for i in range(2)]
            m8 = [sb("m8_%d" % i, [128, 8], F32) for i in range(2)]
            i8 = [sb("i8_%d" % i, [128, 8], U32) for i in range(2)]
            i8f = [sb("i8f_%d" % i, [128, 8], F32) for i in range(2)]
            nm0 = [sb("nm0_%d" % i, [128, 1], F32) for i in range(2)]
            e4 = [sb("e4_%d" % i, [128, 4], F32) for i in range(2)]
            ssum = [sb("ssum%d" % i, [128, 1], F32) for i in range(2)]
            Mk = [sb("Mk%d" % i, [128, 32], F32) for i in range(2)]
            Srun = [sb("Srun%d" % i, [128, 32], F32) for i in range(2)]
            sbase = [sb("sbase%d" % i, [128, 32], F32) for i in range(2)]
            ovf = [sb("ovf%d" % i, [128, 32], F32) for i in range(2)]
            prod = [sb("prod%d" % i, [128, 4, 32], F32) for i in range(2)]
            slotf = [sb("slotf%d" % i, [128, 4], F32) for i in range(2)]
            zt = sb("zt", [128, 5, 1024], BF16)

            d_w1 = P.dsem("w1")
            P.dma("pool", d_w1, lambda e: e.dma_start(out=wo_s[:], in_=wo.rearrange("(kc p) t -> p kc t", p=128)))
            P.dma("pool", d_w1, lambda e: e.dma_start(out=wr_s[:], in_=wr.rearrange("(kc p) t -> p kc t", p=128)))
            P.dma("sp", d_w1, lambda e: e.dma_start(out=g1[:], in_=ln1g))
            P.dma("sp", d_w1, lambda e: e.dma_start(out=b1[:], in_=ln1b))
            w1_tok = P.dma("sp", d_w1, lambda e: e.dma_start(out=rb_s[:], in_=rb))
            d_z = P.dsem("z")
            z_tok = None
            if L == 0:
                tz = P.op("pool", lambda e: e.memset(zt[:], 0.0))
                for ex in range(N_EXPERTS):
                    z_tok = P.dma("sp", d_z, lambda e, ex=ex: e.dma_start(
                        out=Xs[ex * CAP:(ex + 1) * CAP, :].rearrange("(s p) d -> p s d", p=128), in_=zt[:]), deps=[tz])
            t_s0 = P.op("pool", lambda e: e.memset(Srun[0][:], 0.0))

            d_m = [P.dsem("m%d" % i) for i in range(2)]
            d_h = [P.dsem("h%d" % i) for i in range(2)]
            d_h1 = [P.dsem("h1_%d" % i) for i in range(2)]
            d_sc = [P.dsem("sc%d" % i) for i in range(3)]
            mT_free = [None, None]
            ht_free = [None, None]
            rt_free = [None, None]
            h1r_free = [[], []]
            h1b_free = [None] * 3
            h1T_free = [None, None]
            bfree = {i: None for i in range(7)}
            bfree["bf"] = None
            st1 = {}
            srun_tok = t_s0
            last_scatter = None
            m_tok = None

            def stage1(t):
                nonlocal m_tok
                s = t % 2
                if t % 4 == 0:
                    ms = (t // 4) % 2
                    for kc in range(8):
                        m_tok = P.dma("pool", d_m[ms], lambda e, kc=kc: e.indirect_dma_start(
                            out=mTr[ms][:, kc, :], out_offset=None, in_=OTv,
                            in_offset=bass.IndirectOffsetOnAxis(ap=idxm[:, kc * 8 + t // 4:kc * 8 + t // 4 + 1], axis=0),
                            bounds_check=bc_reg2, oob_is_err=False), deps=[mT_free[ms], const_tok])
                ms = (t // 4) % 2
                tl = t % 4
                th = P.dma("sp", d_h[s], lambda e: e.dma_start(out=htr[s][:], in_=h_in[t * 128:(t + 1) * 128, :]),
                           deps=[ht_free[s]])
                for half in range(2):
                    for kc in range(8):
                        tm = P.op("pe", lambda e: e.matmul(
                            B[half][:], mTr[ms][:, kc, tl * 128:(tl + 1) * 128], wo_s[:, kc, half * 512:(half + 1) * 512],
                            start=(kc == 0), stop=(kc == 7)), deps=[m_tok, w1_tok, bfree[half]], inc=(kc == 7))
                    r1 = P.op("dve", lambda e: e.scalar_tensor_tensor(
                        out=rt[s][:, half * 512:(half + 1) * 512], in0=htr[s][:, half * 512:(half + 1) * 512], scalar=ALPHA,
                        in1=B[half][:], op0=ALU.mult, op1=ALU.add), deps=[tm, th, rt_free[s]])
                    bfree[half] = r1
                    r2 = P.op("dve", lambda e: e.bn_stats(stats[s][:, half, :], rt[s][:, half * 512:(half + 1) * 512]), deps=[r1])
                mT_free[ms] = tm
                ht_free[s] = r1
                r3 = P.op("dve", lambda e: e.bn_aggr(mv[s][:], stats[s][:].rearrange("p a b -> p (a b)")), deps=[r2])
                r4 = P.op("dve", lambda e: e.tensor_scalar(
                    out=rstd[s][:], in0=mv[s][:, 1:2], scalar1=LN_EPS, scalar2=None, op0=ALU.add), deps=[r3])
                r4 = P.op("act", lambda e: e.sqrt(rstd[s][:], rstd[s][:]), deps=[r4])
                r4 = P.op("dve", lambda e: e.reciprocal(rstd[s][:], rstd[s][:]), deps=[r4])
                r5 = P.op("dve", lambda e: e.tensor_scalar(
                    out=rt[s][:], in0=rt[s][:], scalar1=mv[s][:, 0:1], scalar2=rstd[s][:, 0:1],
                    op0=ALU.subtract, op1=ALU.mult), deps=[r4])
                r6 = P.op("pool", lambda e: e.tensor_tensor(out=h1r[s][:], in0=rt[s][:], in1=g1[:], op=ALU.mult),
                          deps=[r5, w1_tok] + h1r_free[s])
                rt_free[s] = r6
                r7 = P.op("pool", lambda e: e.tensor_tensor(out=h1r[s][:], in0=h1r[s][:], in1=b1[:], op=ALU.add), deps=[r6])
                ts_ = P.dma("sp", d_h1[s], lambda e: e.dma_start(out=H1[t * 128:(t + 1) * 128, :], in_=h1r[s][:]), deps=[r7])
                bs = t % 3
                r8 = P.op("act", lambda e: e.copy(h1b[bs][:], h1r[s][:]), deps=[r7, h1b_free[bs]])
                h1r_free[s] = [ts_, r8]
                st1[t] = r8

            def stage2(t):
                nonlocal srun_tok, last_scatter
                s = t % 2
                bs = t % 3
                for kc in range(8):
                    t1 = P.op("pe", lambda e: e.transpose(Bbf[:, kc * 128:(kc + 1) * 128], h1b[bs][:, kc * 128:(kc + 1) * 128], identb[:]),
                              deps=[st1[t], bfree["bf"], const_tok], inc=(kc == 7))
                t2 = P.op("act", lambda e: e.copy(h1T[s][:], Bbf[:].rearrange("p (a c) -> p a c", a=8)), deps=[t1, h1T_free[s]])
                bfree["bf"] = t2
                for kc in range(8):
                    t3 = P.op("pe", lambda e: e.matmul(B[2][:, 0:32], h1T[s][:, kc, :], wr_s[:, kc, :], start=(kc == 0), stop=(kc == 7)),
                              deps=[t2, bfree[2]], inc=(kc == 7))
                h1T_free[s] = t3
                a1 = P.op("dve", lambda e: e.tensor_tensor(out=Lg[s][:], in0=B[2][:, 0:32], in1=rb_s[:], op=ALU.add), deps=[t3, w1_tok])
                bfree[2] = a1
                a2 = P.op("dve", lambda e: e.max(m8[s][:], Lg[s][:]), deps=[a1])
                a3 = P.op("dve", lambda e: e.max_index(i8[s][:], m8[s][:], Lg[s][:]), deps=[a2])
                a4 = P.op("dve", lambda e: e.tensor_copy(i8f[s][:], i8[s][:]), deps=[a3])
                a5 = P.op("dve", lambda e: e.tensor_scalar(out=nm0[s][:], in0=m8[s][:, 0:1], scalar1=-1.0, scalar2=None, op0=ALU.mult), deps=[a4])
                a6 = P.op("act", lambda e: e.activation(out=e4[s][:], in_=m8[s][:, 0:4], func=AF.Exp, bias=nm0[s][:, 0:1], scale=1.0),
                          deps=[a5])
                a7 = P.op("dve", lambda e: e.tensor_reduce(out=ssum[s][:], in_=e4[s][:], axis=AX.X, op=ALU.add), deps=[a6])
                a8 = P.op("dve", lambda e: e.reciprocal(ssum[s][:], ssum[s][:]), deps=[a7])
                a9 = P.op("dve", lambda e: e.tensor_scalar(out=gates[:, t, :], in0=e4[s][:], scalar1=ssum[s][:, 0:1], scalar2=None, op0=ALU.mult), deps=[a8])
                a10 = P.op("dve", lambda e: e.tensor_scalar(out=Mk[s][:], in0=Lg[s][:], scalar1=m8[s][:, 3:4], scalar2=None, op0=ALU.is_ge), deps=[a9])
                p1 = P.op("pe", lambda e: e.matmul(B[3][:, 0:32], tstr[:], Mk[s][:], start=True, stop=False), deps=[a10, bfree[3], const_tok], inc=False)
                p2 = P.op("pe", lambda e: e.matmul(B[3][:, 0:32], onesf[:], Srun[s][:], start=False, stop=True), deps=[srun_tok, t_ones])
                srun_tok = P.op("pool", lambda e: e.tensor_tensor(out=Srun[1 - s][:], in0=Srun[s][:], in1=Mk[s][:], op=ALU.add), deps=[a10, p2, srun_tok])
                q1 = P.op("dve", lambda e: e.tensor_tensor(out=sbase[s][:], in0=B[3][:, 0:32], in1=ecap[:], op=ALU.add), deps=[p2, const_tok])
                q2 = P.op("dve", lambda e: e.tensor_scalar(out=ovf[s][:], in0=B[3][:, 0:32], scalar1=CAP - 0.5, scalar2=1.0e6, op0=ALU.is_gt, op1=ALU.mult), deps=[q1])
                bfree[3] = q2
                q3 = P.op("dve", lambda e: e.tensor_tensor(out=sbase[s][:], in0=sbase[s][:], in1=ovf[s][:], op=ALU.add), deps=[q2])
                q4 = q3
                for k in range(4):
                    q4 = P.op("dve", lambda e: e.scalar_tensor_tensor(
                        out=prod[s][:, k, :], in0=iotae[:], scalar=i8f[s][:, k:k + 1], in1=sbase[s][:], op0=ALU.is_equal, op1=ALU.mult),
                        deps=[q4, const_tok])
                q5 = P.op("dve", lambda e: e.tensor_reduce(out=slotf[s][:], in_=prod[s][:], axis=AX.X, op=ALU.add), deps=[q4])
                q6 = P.op("dve", lambda e: e.tensor_copy(slots[:, t * 4:t * 4 + 4], slotf[s][:]), deps=[q5])
                for k in range(4):
                    last_scatter = P.dma("pool", d_sc[bs], lambda e: e.indirect_dma_start(
                        out=Xs, out_offset=bass.IndirectOffsetOnAxis(ap=slots[:, t * 4 + k:t * 4 + k + 1], axis=0),
                        in_=h1b[bs][:, :], in_offset=None, bounds_check=bc_reg, oob_is_err=False),
                        deps=[q6, z_tok])
                h1b_free[bs] = last_scatter

            for t in range(NTT + 1):
                if t < NTT:
                    stage1(t)
                if t >= 1:
                    stage2(t - 1)
            barrier(P)

        with contextlib.ExitStack() as s2:
            def sb(name, shape, dt):
                return s2.enter_context(nc.sbuf_tensor(name + sfx, shape, dt))
            xin = [sb("xin%d" % i, [128, 5, 1024], BF16) for i in range(2)]
            XeT = [sb("XeT%d" % i, [128, 8, CAP], BF16) for i in range(2)]
            NW = 5
            wring = [sb("wring%d" % i, [128, 8, 512], BF16) for i in range(NW)]
            AT = sb("AT", [128, 8, CAP], BF16)
            gs = [sb("gs%d" % i, [128, PART], F32) for i in range(2)]
            sg = [sb("sg%d" % i, [128, PART], F32) for i in range(2)]
            u1 = [sb("u1_%d" % i, [128, PART], F32) for i in range(2)]
            tt = [sb("tt%d" % i, [128, PART], F32) for i in range(2)]
            Yt = [sb("Yt%d" % i, [128, 512], F32) for i in range(3)]
            bgu_s = sb("bgu_s", [N_EXPERTS, 2048], F32)
            bguT = sb("bguT", [128, 16, N_EXPERTS], F32)
            bdn_r = [sb("bdn%d" % i, [1, 1024], BF16) for i in range(2)]

            d_b = P.dsem("bias")
            tb = P.dma("sp", d_b, lambda e: e.dma_start(out=bgu_s[:], in_=bgu))
            for c in range(16):
                x1 = P.op("pe", lambda e, c=c: e.transpose(B[6][:, 0:32], bgu_s[:, c * 128:(c + 1) * 128], identf[0:32, 0:32]),
                          deps=[tb, const_tok] + ([x2] if c > 0 else []))
                x2 = P.op("dve", lambda e, c=c: e.tensor_copy(bguT[:, c, :], B[6][:, 0:32]), deps=[x1])
            x3 = P.op("dve", lambda e: e.tensor_scalar(out=bguT[:, 8:16, :], in0=bguT[:, 8:16, :], scalar1=1.0, scalar2=None, op0=ALU.add), deps=[x2])
            bias_tok = x3

            d_x = [P.dsem("x%d" % i) for i in range(2)]
            d_wr = [P.dsem("wr%d" % i) for i in range(NW)]
            d_bd = [P.dsem("bd%d" % i) for i in range(2)]
            d_y = [P.dsem("y%d" % i) for i in range(3)]
            xin_free = [None, None]
            XeT_free = [None, None]
            wr_free = [None] * NW
            bd_free = [None, None]
            Yt_free = [None] * 3
            AT_free = None
            bfree = {i: None for i in range(7)}
            bfree["bf"] = None
            act_free = [None, None]
            wctr = 0
            yctr = 0
            actr = 0
            y_toks = []

            pieces = []
            for ex in range(N_EXPERTS):
                for q in range(4):
                    pieces.append((ex, "gu", q))
                for hf in range(2):
                    pieces.append((ex, "dn", hf))
            piece_tok = {}
            issued = 0

            def issue_piece():
                nonlocal issued
                if issued >= len(pieces):
                    return
                ex, kd, q = pieces[issued]
                sl = issued % NW
                if kd == "gu":
                    P.dma("pool", d_wr[sl], lambda e: e.dma_start(
                        out=wring[sl][:, :, 0:256], in_=wgu[ex][:, q * 256:(q + 1) * 256].rearrange("(kc p) c -> p kc c", p=128)),
                        deps=[wr_free[sl]])
                    tk = P.dma("pool", d_wr[sl], lambda e: e.dma_start(
                        out=wring[sl][:, :, 256:512], in_=wgu[ex][:, 1024 + q * 256:1024 + (q + 1) * 256].rearrange("(kc p) c -> p kc c", p=128)))
                else:
                    tk = P.dma("pool", d_wr[sl], lambda e: e.dma_start(
                        out=wring[sl][:, :, :], in_=wdn[ex][:, q * 512:(q + 1) * 512].rearrange("(kc p) c -> p kc c", p=128)),
                        deps=[wr_free[sl]])
                piece_tok[(ex, kd, q)] = (tk, sl)
                issued += 1

            for _ in range(NW - 1):
                issue_piece()

            x_tok = {}

            def load_x(ex):
                xs = ex % 2
                x_tok[ex] = P.dma("sp", d_x[xs], lambda e: e.dma_start(
                    out=xin[xs][:], in_=Xs[ex * CAP:(ex + 1) * CAP, :].rearrange("(s p) d -> p s d", p=128)),
                    deps=[xin_free[xs]])

            load_x(0)
            for ex in range(N_EXPERTS):
                xs = ex % 2
                if ex + 1 < N_EXPERTS:
                    load_x(ex + 1)
                tbd = P.dma("pool", d_bd[xs], lambda e: e.dma_start(out=bdn_r[xs][:], in_=bdn[ex:ex + 1, :]), deps=[bd_free[xs]])
                for sidx in range(5):
                    for kc in range(8):
                        t1 = P.op("pe", lambda e: e.transpose(Bbf[:, kc * 128:(kc + 1) * 128], xin[xs][:, sidx, kc * 128:(kc + 1) * 128], identb[:]),
                                  deps=[x_tok[ex], bfree["bf"]], inc=(kc == 7))
                    t2 = P.op("act", lambda e: e.copy(XeT[xs][:, :, sidx * 128:(sidx + 1) * 128], Bbf[:].rearrange("p (a c) -> p a c", a=8)),
                              deps=[t1, XeT_free[xs]])
                    bfree["bf"] = t2
                xin_free[xs] = t1
                xet_tok = t2
                last_pool = None
                for q in range(4):
                    issue_piece()
                    wt, sl = piece_tok[(ex, "gu", q)]
                    for cl in range(2):
                        c = 2 * q + cl
                        for part in range(2):
                            cs = slice(part * PART, (part + 1) * PART)
                            for kc in range(8):
                                tg = P.op("pe", lambda e: e.matmul(B[part][:, 0:PART], wring[sl][:, kc, cl * 128:(cl + 1) * 128], XeT[xs][:, kc, cs],
                                                                   start=(kc == 0), stop=(kc == 7)), deps=[wt, xet_tok, bfree[part]], inc=(kc == 7))
                            for kc in range(8):
                                tu = P.op("pe", lambda e: e.matmul(B[2 + part][:, 0:PART], wring[sl][:, kc, 256 + cl * 128:256 + (cl + 1) * 128], XeT[xs][:, kc, cs],
                                                                   start=(kc == 0), stop=(kc == 7)), deps=[bfree[2 + part]], inc=(kc == 7))
                            a = actr % 2
                            actr += 1
                            v1 = P.op("dve", lambda e: e.tensor_scalar(out=gs[a][:], in0=B[part][:, 0:PART], scalar1=bguT[:, c, ex:ex + 1], scalar2=SWIGLU_LIMIT,
                                                                       op0=ALU.add, op1=ALU.min), deps=[tg, bias_tok, act_free[a]])
                            bfree[part] = v1
                            v2 = P.op("act", lambda e: e.activation(out=sg[a][:], in_=gs[a][:], func=AF.Sigmoid, scale=SWIGLU_ALPHA), deps=[v1])
                            v3 = P.op("dve", lambda e: e.tensor_scalar(out=u1[a][:], in0=B[2 + part][:, 0:PART], scalar1=bguT[:, 8 + c, ex:ex + 1], scalar2=SWIGLU_LIMIT + 1.0,
                                                                       op0=ALU.add, op1=ALU.min), deps=[tu])
                            bfree[2 + part] = v3
                            v4 = P.op("dve", lambda e: e.scalar_tensor_tensor(out=tt[a][:], in0=u1[a][:], scalar=-SWIGLU_LIMIT + 1.0, in1=gs[a][:],
                                                                              op0=ALU.max, op1=ALU.mult), deps=[v3, v1])
                            v5 = P.op("dve", lambda e: e.tensor_tensor(out=AT[:, c, cs], in0=tt[a][:], in1=sg[a][:], op=ALU.mult), deps=[v2, v4, AT_free])
                            act_free[a] = v5
                            last_pool = v5
                    wr_free[sl] = tu
                XeT_free[xs] = tu
                for hf in range(2):
                    issue_piece()
                    wt, sl = piece_tok[(ex, "dn", hf)]
                    for sidx in range(5):
                        yb = 4 + (yctr % 2)
                        for kc in range(8):
                            td = P.op("pe", lambda e: e.matmul(B[yb][:], AT[:, kc, sidx * 128:(sidx + 1) * 128], wring[sl][:, kc, :],
                                                               start=(kc == 0), stop=False), deps=[wt, last_pool, bfree[yb]], inc=False)
                        td = P.op("pe", lambda e: e.matmul(B[yb][:], onesb[0:1, :], bdn_r[xs][0:1, hf * 512:(hf + 1) * 512], start=False, stop=True),
                                  deps=[tbd, t_ones])
                        ys = yctr % 3
                        yctr += 1
                        w1 = P.op("act", lambda e: e.copy(Yt[ys][:], B[yb][:]), deps=[td, Yt_free[ys]])
                        bfree[yb] = w1
                        ty = P.dma("sp", d_y[ys], lambda e: e.dma_start(
                            out=Ys[ex * CAP + sidx * 128:ex * CAP + (sidx + 1) * 128, hf * 512:(hf + 1) * 512], in_=Yt[ys][:]), deps=[w1])
                        Yt_free[ys] = ty
                        y_toks.append(ty)
                    wr_free[sl] = td
                AT_free = td
                bd_free[xs] = td
            barrier(P)

        with contextlib.ExitStack() as s3:
            def sb(name, shape, dt):
                return s3.enter_context(nc.sbuf_tensor(name + sfx, shape, dt))
            g2 = sb("g2", [128, 1024], F32)
            b2 = sb("b2", [128, 1024], F32)
            Yg = [sb("Yg%d" % i, [128, 4, 1024], F32) for i in range(2)]
            h1t = [sb("h1t%d" % i, [128, 1024], F32) for i in range(2)]
            acc = [sb("acc%d" % i, [128, 1024], F32) for i in range(2)]
            ot = [sb("ot%d" % i, [128, 1024], F32) for i in range(2)]
            stats = [sb("stats3_%d" % i, [128, 2, 6], F32) for i in range(2)]
            mv = [sb("mv3_%d" % i, [128, 2], F32) for i in range(2)]
            rstd = [sb("rstd3_%d" % i, [128, 1], F32) for i in range(2)]
            d_w3 = P.dsem("w3")
            P.dma("sp", d_w3, lambda e: e.dma_start(out=g2[:], in_=ln2g))
            w3_tok = P.dma("sp", d_w3, lambda e: e.dma_start(out=b2[:], in_=ln2b))
            d_g = [P.dsem("g%d" % i) for i in range(2)]
            d_h3 = [P.dsem("h3_%d" % i) for i in range(2)]
            d_out = [P.dsem("out%d" % i) for i in range(2)]
            for s in range(2):
                P.op("pool", lambda e, s=s: e.memset(Yg[s][:], 0.0))
            Yg_free = [None, None]
            h1t_free = [None, None]
            ot_free = [[], []]
            outs = []
            gt = {}
            hst = hT_state(nc, P, s3, sfx) if emit_hT else None

            def gather(t):
                s = t % 2
                for k in range(4):
                    gt[t] = P.dma("pool", d_g[s], lambda e: e.indirect_dma_start(
                        out=Yg[s][:, k, :], out_offset=None, in_=Ys,
                        in_offset=bass.IndirectOffsetOnAxis(ap=slots[:, t * 4 + k:t * 4 + k + 1], axis=0),
                        bounds_check=bc_reg, oob_is_err=False), deps=[Yg_free[s]])

            gather(0)
            for t in range(NTT):
                s = t % 2
                if t + 1 < NTT:
                    gather(t + 1)
                th = P.dma("sp", d_h3[s], lambda e: e.dma_start(out=h1t[s][:], in_=H1[t * 128:(t + 1) * 128, :]), deps=[h1t_free[s]])
                c1 = P.op("dve", lambda e: e.tensor_scalar(out=acc[s][:], in0=Yg[s][:, 0, :], scalar1=gates[:, t, 0:1], scalar2=None, op0=ALU.mult),
                          deps=[gt[t]])
                c2 = P.op("dve", lambda e: e.scalar_tensor_tensor(out=acc[s][:], in0=Yg[s][:, 1, :], scalar=gates[:, t, 1:2], in1=acc[s][:],
                                                                   op0=ALU.mult, op1=ALU.add), deps=[c1, gt[t]])
                c3 = P.op("dve", lambda e: e.scalar_tensor_tensor(out=acc[s][:], in0=Yg[s][:, 2, :], scalar=gates[:, t, 2:3], in1=acc[s][:],
                                                                  op0=ALU.mult, op1=ALU.add), deps=[c2])
                c4 = P.op("dve", lambda e: e.scalar_tensor_tensor(out=acc[s][:], in0=Yg[s][:, 3, :], scalar=gates[:, t, 3:4], in1=acc[s][:],
                                                                   op0=ALU.mult, op1=ALU.add), deps=[c3])
                Yg_free[s] = c4
                c5 = P.op("dve", lambda e: e.scalar_tensor_tensor(out=acc[s][:], in0=h1t[s][:], scalar=ALPHA, in1=acc[s][:],
                                                                  op0=ALU.mult, op1=ALU.add), deps=[c4, th])
                h1t_free[s] = c5
                for half in range(2):
                    c6 = P.op("dve", lambda e: e.bn_stats(stats[s][:, half, :], acc[s][:, half * 512:(half + 1) * 512]), deps=[c5])
                c7 = P.op("dve", lambda e: e.bn_aggr(mv[s][:], stats[s][:].rearrange("p a b -> p (a b)")), deps=[c6])
                c8 = P.op("dve", lambda e: e.tensor_scalar(out=rstd[s][:], in0=mv[s][:, 1:2], scalar1=LN_EPS, scalar2=None, op0=ALU.add), deps=[c7])
                c8 = P.op("act", lambda e: e.sqrt(rstd[s][:], rstd[s][:]), deps=[c8])
                c8 = P.op("dve", lambda e: e.reciprocal(rstd[s][:], rstd[s][:]), deps=[c8])
                c9 = P.op("dve", lambda e: e.tensor_scalar(out=acc[s][:], in0=acc[s][:], scalar1=mv[s][:, 0:1], scalar2=rstd[s][:, 0:1],
                                                           op0=ALU.subtract, op1=ALU.mult), deps=[c8])
                c10 = P.op("pool", lambda e: e.tensor_tensor(out=ot[s][:], in0=acc[s][:], in1=g2[:], op=ALU.mult), deps=[c9, w3_tok] + ot_free[s])
                c11 = P.op("pool", lambda e: e.tensor_tensor(out=ot[s][:], in0=ot[s][:], in1=b2[:], op=ALU.add), deps=[c10])
                to = P.dma("sp", d_out[s], lambda e: e.dma_start(out=out[t * 128:(t + 1) * 128, :], in_=ot[s][:]), deps=[c11])
                ot_free[s] = [to]
                outs.append(to)
                if emit_hT:
                    x3 = hT_tile(nc, P, Bbf, identb, hst, ot[s], c11, t, hT_own, [const_tok])
                    ot_free[s].append(hst["cast_tok"])
            barrier(P)


def hT_state(nc, P, stack, sfx):
    stt = {}
    stt["obf"] = [stack.enter_context(nc.sbuf_tensor("obf%d%s" % (i, sfx), [128, 1024], BF16)) for i in range(2)]
    stt["hTs"] = [stack.enter_context(nc.sbuf_tensor("hTs%d%s" % (i, sfx), [128, 8, 512], BF16)) for i in range(2)]
    stt["obf_free"] = [None, None]
    stt["hTs_free"] = [None, None]
    stt["bf_free"] = None
    stt["d"] = [P.dsem("hts0"), P.dsem("hts1")]
    stt["cast_tok"] = None
    return stt


def hT_tile(nc, P, Bbf, identb, stt, src, src_tok, t, hT_own, extra):
    s = t % 2
    g, tl = t // 4, t % 4
    hs = g % 2
    obf, hTs = stt["obf"], stt["hTs"]
    x1 = P.op("act", lambda e: e.copy(obf[s][:], src[:]), deps=[src_tok, stt["obf_free"][s]])
    stt["cast_tok"] = x1
    for kc in range(8):
        x2 = P.op("pe", lambda e, kc=kc: e.transpose(Bbf[:, kc * 128:(kc + 1) * 128], obf[s][:, kc * 128:(kc + 1) * 128], identb[:]),
                  deps=[x1, stt["bf_free"]] + extra, inc=(kc == 7))
    stt["obf_free"][s] = x2
    x3 = P.op("dve", lambda e: e.tensor_copy(hTs[hs][:, :, tl * 128:(tl + 1) * 128], Bbf[:].rearrange("p (a c) -> p a c", a=8)),
              deps=[x2] + ([stt["hTs_free"][hs]] if tl == 0 else []))
    stt["bf_free"] = x3
    if tl == 3:
        tw = P.dma("sp", stt["d"][hs], lambda e: e.dma_start(
            out=hT_own[:, g * 512:(g + 1) * 512].rearrange("(kc p) t -> p kc t", p=128), in_=hTs[hs][:]), deps=[x3])
        stt["hTs_free"][hs] = tw
    return x3


def emit_hT0(nc, P, Bbf, E):
    with contextlib.ExitStack() as st:
        identb = st.enter_context(nc.sbuf_tensor("identb_t0", [128, 128], BF16))
        xt = [st.enter_context(nc.sbuf_tensor("xt%d_t0" % i, [128, 1024], F32)) for i in range(2)]
        d_c = P.dsem("const")
        ct = P.dma("pool", d_c, lambda e: e.dma_start(out=identb[:], in_=E["ident"]))
        stt = hT_state(nc, P, st, "_t0")
        d_x = [P.dsem("h0"), P.dsem("h1")]
        xfree = [None, None]
        for t in range(NTT):
            s = t % 2
            tx = P.dma("sp", d_x[s], lambda e: e.dma_start(out=xt[s][:], in_=E["x_own"][t * 128:(t + 1) * 128, :]), deps=[xfree[s]])
            hT_tile(nc, P, Bbf, identb, stt, xt[s], tx, t, E["hT_own"], [ct])
            xfree[s] = stt["cast_tok"]
        barrier(P)


PAIRS = [[0, 1], [2, 3], [4, 5], [6, 7]]
NL = DEPTH


def allgather(nc, P, src, dst, kind):
    barrier(P)
    key = P.dsem("cc")
    for c in range(4):
        if kind == "hT":
            s_ap = src[c * 256:(c + 1) * 256, :]
            d_ap = dst[c].rearrange("r kk p t -> (r kk p) t")
        else:
            s_ap = src[c * 128:(c + 1) * 128, :]
            d_ap = dst[c].rearrange("r q t -> (r q) t")
        P.cc(key, lambda e: e.collective_compute("AllGather", ALU.bypass, replica_groups=PAIRS, ins=[s_ap.opt()], outs=[d_ap.opt()]))
    barrier(P)


def build_fused():
    nc = bass.Bass("TRN2", target_bir_lowering=False)
    E = {}

    def din(name, shape, dt=F32):
        E[name] = nc.dram_tensor(name, shape, dt, kind="ExternalInput").ap()

    def dint(name, shape, dt):
        E[name] = nc.dram_tensor(name, shape, dt, kind="Internal").ap()

    din("x_own", [NTOK, 1024])
    din("memT", [1024, N_MEM])
    din("wmk", [1024, 128])
    din("wmv", [1024, 128])
    din("wq", [NL, 1024, 512])
    din("wk", [NL, 1024, 384])
    din("wv", [NL, 1024, 384])
    din("wfgp", [2, 3, 1024, 512])
    din("bfg", [2, 32, 3])
    din("ident", [128, 128])
    din("negm", [128, 512])
    din("kaug", [36, SEQ])
    din("qaug", [36, SEQ])
    din("iq", [32, 16])
    din("sel", [32, 256])
    din("tri", [32, 32])
    din("cdec", [3, 32, 512])
    din("maskm", [128, 2048])
    din("idxm", [128, 64], I32)
    din("wo", [NL, 1024, 1024])
    for nm in ("ln1g", "ln1b", "ln2g", "ln2b"):
        din(nm, [NL, 128, 1024])
    din("wr", [NL, 1024, 32])
    din("rb", [NL, 128, 32])
    din("wgu", [NL, N_EXPERTS, 1024, 2048])
    din("bgu", [NL, N_EXPERTS, 2048])
    din("wdn", [NL, N_EXPERTS, 1024, 1024])
    din("bdn", [NL, N_EXPERTS, 1024])
    din("tstr", [128, 128])
    din("iotae", [128, 32])
    din("ecap", [128, 32])
    E["out"] = nc.dram_tensor("out", [NTOK, 1024], F32, kind="ExternalOutput").ap()
    dint("hT_own", [1024, NTOK], BF16)
    dint("hTp", [4, 2, 2, 128, NTOK], BF16)
    dint("OT_own", [512, SEQ], BF16)
    dint("OT_pair", [4, 2, 128, SEQ], BF16)
    dint("dscr", [2, 2, 2, SEQ], BF16)
    dint("H1", [NTOK, 1024], F32)
    dint("hres", [NTOK, 1024], F32)
    dint("Xs", [NSLOT, 1024], BF16)
    dint("Ys", [NSLOT, 1024], F32)

    with contextlib.ExitStack() as st:
        P = Prog(nc, st)
        B = [st.enter_context(nc.psum_tensor("B%d" % i, [128, 512], F32)) for i in range(7)]
        Bbf = st.enter_context(nc.psum_tensor("Bbf", [128, 1024], BF16))
        E["bc_reg"] = nc.gpsimd.to_reg(NSLOT - 1)
        E["bc_reg2"] = nc.gpsimd.to_reg(16383)
        emit_hT0(nc, P, Bbf, E)
        allgather(nc, P, E["hT_own"], E["hTp"], "hT")
        for L in range(NL):
            emit_attn(nc, P, B, Bbf, "moba" if L % 2 == 0 else "fox", E, L)
            allgather(nc, P, E["OT_own"], E["OT_pair"], "OT")
            emit_ffn(nc, P, B, Bbf, E, L)
            if L < NL - 1:
                allgather(nc, P, E["hT_own"], E["hTp"], "hT")
                P.new_epoch()
        barrier(P)
    return nc


def ffn_consts():
    if "ffn" in _CONST:
        return _CONST["ffn"]
    c = {}
    c["ident"] = np.eye(128, dtype=np.float32)
    k = np.arange(128)
    c["tstr"] = (k[:, None] < k[None, :]).astype(np.float32)
    c["iotae"] = np.ascontiguousarray(np.broadcast_to(np.arange(32, dtype=np.float32)[None, :], (128, 32)))
    c["ecap"] = np.ascontiguousarray(np.broadcast_to((np.arange(32, dtype=np.float32) * CAP)[None, :], (128, 32)))
    _CONST["ffn"] = c
    return c


def bc128(v):
    return np.ascontiguousarray(np.broadcast_to(np.asarray(v, np.float32).reshape(1, -1), (128, v.size)))


def ffn_inputs(h_tok, mT_tok, wo_l, ln1g, ln1b, ln2g, ln2b, wr_l, rb_l, wgu_l, bgu_l, wdn_l, bdn_l):
    m = dict(ffn_consts())
    m.update(h=h_tok, mT=mT_tok, wo=wo_l, ln1g=bc128(ln1g), ln1b=bc128(ln1b), ln2g=bc128(ln2g), ln2b=bc128(ln2b),
             wr=wr_l, rb=bc128(rb_l), wgu=wgu_l, bgu=bgu_l, wdn=wdn_l, bdn=bdn_l)
    return m


_FUSED = {}


def kernel(x, mem, w_in_moba, w_in_fox, b_fgate, w_mem_kv, w_o, ln1_g, ln1_b, router_w, router_b,
           w_gate_up, b_gate_up, w_down, b_down, ln2_g, ln2_b):
    f32 = lambda a: np.ascontiguousarray(np.asarray(a, dtype=np.float32))
    x, mem, w_mem_kv = f32(x), f32(mem), f32(w_mem_kv)
    w_in_moba, w_in_fox, b_fgate, w_o = f32(w_in_moba), f32(w_in_fox), f32(b_fgate), f32(w_o)
    SW = N_SELF * HEAD_DIM
    shared = dict(ffn_consts())
    ca = attn_consts("moba", 0)
    for k in ("ident", "negm", "kaug", "qaug", "iq", "sel", "maskm"):
        shared[k] = ca[k]
    shared["tri"] = attn_consts("fox", 0)["tri"]
    shared["wr"] = f32(router_w)[:NL]
    shared["rb"] = np.stack([bc128(f32(router_b)[l]) for l in range(NL)])
    for nm, v in (("ln1g", ln1_g), ("ln1b", ln1_b), ("ln2g", ln2_g), ("ln2b", ln2_b)):
        shared[nm] = np.stack([bc128(f32(v)[l]) for l in range(NL)])
    shared["wgu"], shared["bgu"] = f32(w_gate_up)[:NL], f32(b_gate_up)[:NL]
    shared["wdn"], shared["bdn"] = f32(w_down)[:NL], f32(b_down)[:NL]
    perm = np.zeros(1024, np.int64)
    for hp in range(4):
        for r in range(2):
            for q in range(128):
                hl, dd = 2 * hp + q // 64, q % 64
                g = (6 * r + hl) * 64 + dd if hl < 6 else 768 + (2 * r + hl - 6) * 64 + dd
                perm[(hp * 2 + r) * 128 + q] = g
    shared["wo"] = np.ascontiguousarray(w_o[:NL][:, perm, :])
    per_half = []
    for hh in range(2):
        d = {}
        heads = slice(6 * hh * 64, (6 * hh + 6) * 64)
        wq, wk, wv = [], [], []
        for l in range(NL):
            w_in = w_in_moba[l // 2] if l % 2 == 0 else w_in_fox[l // 2]
            wq.append(np.concatenate([w_in[:, 0:SW][:, heads], w_in[:, 3 * SW:3 * SW + 256][:, hh * 128:(hh + 1) * 128]], axis=1))
            wk.append(w_in[:, SW:2 * SW][:, heads])
            wv.append(w_in[:, 2 * SW:3 * SW][:, heads])
        d["wq"], d["wk"], d["wv"] = np.ascontiguousarray(np.stack(wq)), np.ascontiguousarray(np.stack(wk)), np.ascontiguousarray(np.stack(wv))
        wfgp = np.zeros((2, 3, 1024, 16, 32), np.float32)
        bfg = np.zeros((2, 32, 3), np.float32)
        for j in range(2):
            wfg = w_in_fox[j][:, 2560:2572]
            for pp in range(3):
                for r in range(2):
                    h = 6 * hh + 2 * pp + r
                    for i in range(16):
                        wfgp[j, pp, :, i, r * 16 + i] = wfg[:, h]
                    bfg[j, r * 16:(r + 1) * 16, pp] = b_fgate[j, h]
        d["wfgp"] = wfgp.reshape(2, 3, 1024, 512)
        d["bfg"] = bfg
        d["cdec"] = attn_consts("moba", hh)["cdec"]
        d["wmk"] = np.ascontiguousarray(w_mem_kv[:, 0:256][:, hh * 128:(hh + 1) * 128])
        d["wmv"] = np.ascontiguousarray(w_mem_kv[:, 256:512][:, hh * 128:(hh + 1) * 128])
        kc = np.arange(8)[None, :, None]
        p = np.arange(128)[:, None, None]
        g = np.arange(8)[None, None, :]
        d["idxm"] = np.ascontiguousarray((((kc * 128 + p) * 2 + hh) * 8 + g).reshape(128, 64).astype(np.int32))
        per_half.append(d)
    maps = []
    for c in range(8):
        b, hh = c // 2, c % 2
        m = dict(shared)
        m.update(per_half[hh])
        m["x_own"] = np.ascontiguousarray(x[b, hh * NTOK:(hh + 1) * NTOK])
        m["memT"] = np.ascontiguousarray(mem[b].T)
        maps.append(m)
    if NL not in _FUSED:
        _FUSED[NL] = build_fused()
    res = run_bass_kernel_spmd(_FUSED[NL], maps, core_ids=list(range(8))).results
    out = np.empty((BATCH, SEQ, D_MODEL), np.float32)
    for c in range(8):
        b, hh = c // 2, c % 2
        out[b, hh * NTOK:(hh + 1) * NTOK] = np.asarray(res[c]["out"])
    return out
```

```python
import contextlib
import math
import numpy as np
import ml_dtypes
import concourse.bass as bass
import concourse.mybir as mybir
from concourse.bass_utils import run_bass_kernel_spmd

F32 = mybir.dt.float32
BF16 = mybir.dt.bfloat16
I32 = mybir.dt.int32
U32 = mybir.dt.uint32
ALU = mybir.AluOpType
AF = mybir.ActivationFunctionType
AX = mybir.AxisListType

D_MODEL = 1024
BATCH = 4
SEQ = 8192
DEPTH = 4
HEAD_DIM = 64
N_SELF = 12
N_MEMH = 4
N_MEM = 256
N_EXPERTS = 32
TOP_K = 4
D_FF = 1024
SWIGLU_LIMIT = 7.0
SWIGLU_ALPHA = 1.702
ALPHA = (2 * DEPTH) ** 0.25
LN_EPS = 1e-5
NEG = -30000.0
KAUG = 100
NQT = SEQ // 512
NKT = SEQ // 128


class Prog:
    def __init__(self, nc, stack):
        self.nc = nc
        self.stack = stack
        self.engs = {"pe": nc.tensor, "act": nc.scalar, "dve": nc.vector, "pool": nc.gpsimd, "sp": nc.sync}
        self.sem = {}
        self.cnt = {}
        self.seen = {k: {} for k in self.engs}
        for k in self.engs:
            self.sem[k] = stack.enter_context(nc.semaphore("s_" + k))
            self.cnt[k] = 0
        self.ndsem = 0

    def dsem(self, name=None):
        if name is not None and ("dn_" + name) in self.sem:
            return "dn_" + name
        self.ndsem += 1
        key = "dn_" + name if name is not None else "d%d" % self.ndsem
        self.sem[key] = self.stack.enter_context(self.nc.semaphore(key))
        self.cnt[key] = 0
        return key

    def wait(self, eng, tok):
        if tok is None:
            return
        key, val = tok
        if eng == "pe" and key == "pe":
            return
        if self.seen[eng].get(key, 0) >= val:
            return
        self.engs[eng].wait_ge(self.sem[key], val)
        self.seen[eng][key] = val

    def op(self, eng, fn, deps=(), inc=True):
        for d in deps:
            self.wait(eng, d)
        inst = fn(self.engs[eng])
        if inc:
            self.cnt[eng] += 1
            inst.then_inc(self.sem[eng], 1)
            return (eng, self.cnt[eng])
        return None

    def new_epoch(self):
        self.epoch = getattr(self, "epoch", 0) + 1
        for k in self.engs:
            self.sem[k] = self.stack.enter_context(self.nc.semaphore("s_%s_e%d" % (k, self.epoch)))
            self.cnt[k] = 0
            for e in self.engs:
                self.seen[e].pop(k, None)

    def cc(self, dkey, fn, deps=()):
        for d in deps:
            self.wait("pool", d)
        inst = fn(self.engs["pool"])
        self.cnt[dkey] += 1
        inst.then_inc(self.sem[dkey])
        return (dkey, self.cnt[dkey])

    def dma(self, q, dkey, fn, deps=()):
        for d in deps:
            self.wait(q, d)
        inst = fn(self.engs[q])
        self.cnt[dkey] += 16
        inst.then_inc(self.sem[dkey], 16)
        return (dkey, self.cnt[dkey])


def _alibi_slopes(n):
    def pow2(m):
        start = 2.0 ** (-(2.0 ** -(math.log2(m) - 3)))
        return [start * start ** i for i in range(m)]
    if math.log2(n).is_integer():
        s = pow2(n)
    else:
        c = 2 ** math.floor(math.log2(n))
        s = pow2(c) + pow2(2 * c)[0::2][: n - c]
    return np.array(s, dtype=np.float32)


def emit_attn(nc, P, B, Bbf, kind, E, L):
    fox = kind == "fox"
    j = L // 2
    sfx = "_a%d" % L
    hTp = E["hTp"]
    wq, wk, wv = E["wq"][L], E["wk"][L], E["wv"][L]
    memT, wmk, wmv = E["memT"], E["wmk"], E["wmv"]
    ident_d, negm_d, kaug_d, qaug_d, iq_d, sel_d = E["ident"], E["negm"], E["kaug"], E["qaug"], E["iq"], E["sel"]
    if fox:
        wfgp, bfg, tri_d = E["wfgp"][j], E["bfg"][j], E["tri"]
    else:
        cdec, maskm_d = E["cdec"], E["maskm"]
    OT, dscr = E["OT_own"], E["dscr"]

    with contextlib.ExitStack() as st:

        def sb(name, shape, dt):
            return st.enter_context(nc.sbuf_tensor(name + sfx, shape, dt))

        QT = [sb("QT%d" % r, [KAUG, SEQ], BF16) for r in range(2)]
        KT = [sb("KT%d" % r, [KAUG, SEQ], BF16) for r in range(2)]
        VA = sb("VA", [128, NKT, 2, 128], BF16)
        hTr = [sb("hTr%d" % s, [128, 8, 512], BF16) for s in range(2)]
        wq_s = sb("wq_s", [128, 8, 128], BF16)
        wk_s = sb("wk_s", [128, 8, 128], BF16)
        wv_s = sb("wv_s", [128, 8, 128], BF16)
        memT_s = sb("memT_s", [128, 8, N_MEM], BF16)
        wmk_s = sb("wmk_s", [128, 8, 128], BF16)
        wmv_s = sb("wmv_s", [128, 8, 128], BF16)
        PT = [sb("PT%d" % s, [128, 512], BF16) for s in range(4)]
        TMP = [sb("TMP%d" % s, [128, 512], F32) for s in range(2)]
        Dt = [sb("Dt%d" % r, [128, NQT * NKT], F32) for r in range(2)]
        negm = sb("negm_s", [128, 512], F32)
        ident = sb("ident_s", [128, 128], BF16)
        OS = [sb("OS%d" % s, [64, 512], BF16) for s in range(2)]
        RR = [sb("RR%d" % s, [64, 512], F32) for s in range(2)]
        iq = sb("iq_s", [32, 16], F32)
        sel = sb("sel_s", [32, 256], F32)
        Cc = sb("Cc", [32, 512], F32)
        Z1 = sb("Z1", [32, 512], F32)
        RQ = sb("RQ", [32, 2, 512], BF16)
        RK = sb("RK", [32, 2, 512], BF16)
        refs = sb("refs", [32, 80], F32)
        bq_s = sb("bq_s", [128, 16], F32)
        if fox:
            wfg_s = sb("wfg_s", [128, 8, 512], BF16)
            bfg_s = sb("bfg_s", [32, 3], F32)
            tri = sb("tri_s", [32, 32], F32)
            ones32 = sb("ones32", [32, 512], F32)
            offs = sb("offs", [32, 1], F32)
        else:
            maskm = sb("maskm_s", [128, 2048], F32)
            kbar = [sb("kbar%d" % r, [64, 32], BF16) for r in range(2)]
            kbf = sb("kbf", [64, 32], F32)
            gp = [sb("gp%d" % s, [128, 128], F32) for s in range(2)]
            m8 = [sb("m8_%d" % s, [128, 4, 8], F32) for s in range(2)]
            thr = [sb("thr%d" % s, [128, 4], F32) for s in range(2)]
            TB = [sb("TB%d" % s, [128, 4, 96], BF16) for s in range(2)]
        d_c = P.dsem("const")
        toks = []
        toks.append(P.dma("pool", d_c, lambda e: e.dma_start(out=ident[:], in_=ident_d)))
        toks.append(P.dma("sp", d_c, lambda e: e.dma_start(out=negm[:], in_=negm_d)))
        toks.append(P.dma("sp", d_c, lambda e: e.dma_start(out=iq[:], in_=iq_d)))
        toks.append(P.dma("sp", d_c, lambda e: e.dma_start(out=sel[:], in_=sel_d)))
        for r in range(2):
            toks.append(P.dma("pool", d_c, lambda e, r=r: e.dma_start(out=KT[r][64:100, :], in_=kaug_d)))
            toks.append(P.dma("pool", d_c, lambda e, r=r: e.dma_start(out=QT[r][64:100, :], in_=qaug_d)))
        toks.append(P.dma("pool", d_c, lambda e: e.dma_start(
            out=memT_s[:], in_=memT.rearrange("(kc p) t -> p kc t", p=128))))
        toks.append(P.dma("pool", d_c, lambda e: e.dma_start(
            out=wmk_s[:], in_=wmk.rearrange("(kc p) t -> p kc t", p=128))))
        toks.append(P.dma("pool", d_c, lambda e: e.dma_start(
            out=wmv_s[:], in_=wmv.rearrange("(kc p) t -> p kc t", p=128))))
        if fox:
            toks.append(P.dma("sp", d_c, lambda e: e.dma_start(out=bfg_s[:], in_=bfg)))
            toks.append(P.dma("sp", d_c, lambda e: e.dma_start(out=tri[:], in_=tri_d)))
        else:
            toks.append(P.dma("sp", d_c, lambda e: e.dma_start(out=maskm[:], in_=maskm_d)))
        const_tok = toks[-1]
        t_va = P.op("pool", lambda e: e.memset(VA[:], 1.0))
        if fox:
            t_ones = P.op("pool", lambda e: e.memset(ones32[:], 1.0))
        else:
            for s in range(2):
                t_tb = P.op("pool", lambda e, s=s: e.memset(TB[s][:], 0.0))

        d_h = [P.dsem("h%d" % s) for s in range(2)]
        d_w = P.dsem("w")
        d_o = [P.dsem("o%d" % s) for s in range(2)]
        d_dec = P.dsem("dec")
        d_dec2 = P.dsem("dec2")

        hT_free = [None, None]
        w_free = None
        bank_free = {i: None for i in range(7)}
        bank_free["bf"] = None
        PT_free = [None] * 4
        TMP_free = [None] * 2
        OS_free = [None] * 2
        RR_free = [None] * 2
        qk_last_reader = [None, None]
        unit = 0
        ctrs = {"qk": 0, "ex": 0, "tx": 0}
        TB_free = [None, None]
        out_toks = []
        dt_free = [None, None]

        for p in range(4):
            mem_pass = p == 3
            wdeps = [w_free]
            tw = P.dma("pool", d_w, lambda e, p=p: e.dma_start(
                out=wq_s[:], in_=wq[:, p * 128:(p + 1) * 128].rearrange("(kc p) t -> p kc t", p=128)), deps=wdeps)
            if not mem_pass:
                tw = P.dma("pool", d_w, lambda e, p=p: e.dma_start(
                    out=wk_s[:], in_=wk[:, p * 128:(p + 1) * 128].rearrange("(kc p) t -> p kc t", p=128)))
                tw = P.dma("pool", d_w, lambda e, p=p: e.dma_start(
                    out=wv_s[:], in_=wv[:, p * 128:(p + 1) * 128].rearrange("(kc p) t -> p kc t", p=128)))
                if fox:
                    tw = P.dma("pool", d_w, lambda e, p=p: e.dma_start(
                        out=wfg_s[:], in_=wfgp[p].rearrange("(kc p) t -> p kc t", p=128)))

            last_ev = {}
            for i in range(NQT):
                s = i % 2
                for c4 in range(4):
                    th = P.dma("pool", d_h[s], lambda e, i=i, s=s, c4=c4: e.dma_start(
                        out=hTr[s][:, 2 * c4:2 * c4 + 2, :],
                        in_=hTp[c4, i // 8, :, :, (i % 8) * 512:(i % 8 + 1) * 512].rearrange("kk p t -> p kk t")),
                        deps=[hT_free[s]])
                cols = slice(i * 512, (i + 1) * 512)
                bq = B[0 + s]
                for kc in range(8):
                    tq = P.op("pe", lambda e, kc=kc, bq=bq, s=s: e.matmul(
                        bq[:], wq_s[:, kc, :], hTr[s][:, kc, :], start=(kc == 0), stop=(kc == 7)),
                        deps=[th, tw, bank_free[0 + s]] + ([qk_last_reader[0], qk_last_reader[1]] if kc == 0 else []),
                        inc=(kc == 7))
                e0 = P.op("dve", lambda e, bq=bq, cols=cols: e.tensor_scalar(out=QT[0][0:64, cols], in0=bq[0:64, :], scalar1=0.125, scalar2=None, op0=ALU.mult), deps=[tq, const_tok])
                e1 = P.op("dve", lambda e, bq=bq, cols=cols: e.tensor_scalar(out=QT[1][0:64, cols], in0=bq[64:128, :], scalar1=0.125, scalar2=None, op0=ALU.mult), deps=[tq])
                bank_free[0 + s] = e1
                last_ev["q"] = e1
                if not mem_pass:
                    bk = B[2 + s]
                    for kc in range(8):
                        tk = P.op("pe", lambda e, kc=kc, bk=bk, s=s: e.matmul(
                            bk[:], wk_s[:, kc, :], hTr[s][:, kc, :], start=(kc == 0), stop=(kc == 7)),
                            deps=[bank_free[2 + s]], inc=(kc == 7))
                    e2 = P.op("dve", lambda e, bk=bk, cols=cols: e.tensor_copy(KT[0][0:64, cols], bk[0:64, :]), deps=[tk, const_tok])
                    e3 = P.op("dve", lambda e, bk=bk, cols=cols: e.tensor_copy(KT[1][0:64, cols], bk[64:128, :]), deps=[tk])
                    bank_free[2 + s] = e3
                    last_ev["k"] = e3
                    bv = B[4 + s]
                    for sub in range(4):
                        for kc in range(8):
                            tv = P.op("pe", lambda e, kc=kc, bv=bv, s=s, sub=sub: e.matmul(
                                bv[:, sub * 128:(sub + 1) * 128], hTr[s][:, kc, sub * 128:(sub + 1) * 128], wv_s[:, kc, :],
                                start=(kc == 0), stop=(kc == 7)),
                                deps=[bank_free[4 + s]], inc=(sub == 3 and kc == 7))
                    bv3 = bv[:].rearrange("p (a c) -> p a c", a=4)
                    e4 = P.op("dve", lambda e, bv3=bv3, i=i: e.tensor_copy(VA[:, 4 * i:4 * i + 4, 0, 0:64], bv3[:, :, 0:64]), deps=[tv, t_va])
                    e5 = P.op("act", lambda e, bv3=bv3, i=i: e.copy(VA[:, 4 * i:4 * i + 4, 1, 0:64], bv3[:, :, 64:128]), deps=[tv, t_va, e4])
                    bank_free[4 + s] = e5
                    last_ev["v0"] = e4
                    last_ev["v1"] = e5
                    if fox:
                        for kc in range(8):
                            tf = P.op("pe", lambda e, kc=kc, s=s, i=i: e.matmul(
                                B[6][0:32, :], wfg_s[:, kc, i * 32:(i + 1) * 32], hTr[s][:, kc, :],
                                start=(i == 0 and kc == 0), stop=(i == NQT - 1 and kc == 7)),
                                deps=[bank_free[6]] if (i == 0 and kc == 0) else [], inc=(kc == 7))
                        hT_free[s] = tf
                    else:
                        hT_free[s] = tv
                else:
                    hT_free[s] = tq
            w_free = hT_free[(NQT - 1) % 2]
            proj_done = [last_ev[k] for k in last_ev]

            if mem_pass:
                for kc in range(8):
                    tmk = P.op("pe", lambda e, kc=kc: e.matmul(
                        B[2][:, 0:N_MEM], wmk_s[:, kc, :], memT_s[:, kc, :], start=(kc == 0), stop=(kc == 7)),
                        deps=[bank_free[2], const_tok], inc=(kc == 7))
                e2 = P.op("dve", lambda e: e.tensor_copy(KT[0][0:64, 0:N_MEM], B[2][0:64, 0:N_MEM]), deps=[tmk])
                e3 = P.op("dve", lambda e: e.tensor_copy(KT[1][0:64, 0:N_MEM], B[2][64:128, 0:N_MEM]), deps=[tmk])
                bank_free[2] = e3
                for mt in range(2):
                    for kc in range(8):
                        tmv = P.op("pe", lambda e, kc=kc, mt=mt: e.matmul(
                            B[4][:, mt * 128:(mt + 1) * 128], memT_s[:, kc, mt * 128:(mt + 1) * 128], wmv_s[:, kc, :],
                            start=(kc == 0), stop=(kc == 7)),
                            deps=[bank_free[4]], inc=(mt == 1 and kc == 7))
                bv3 = B[4][:, 0:256].rearrange("p (a c) -> p a c", a=2)
                e4 = P.op("dve", lambda e: e.tensor_copy(VA[:, 0:2, 0, 0:64], bv3[:, :, 0:64]), deps=[tmv])
                e5 = P.op("dve", lambda e: e.tensor_copy(VA[:, 0:2, 1, 0:64], bv3[:, :, 64:128]), deps=[tmv])
                bank_free[4] = e5
                proj_done += [e3, e5]

            if not mem_pass:
                if fox:
                    a1 = P.op("act", lambda e, p=p: e.activation(
                        out=Z1[:], in_=B[6][0:32, :], func=AF.Sigmoid, bias=bfg_s[:, p:p + 1], scale=1.0),
                        deps=[tf, const_tok, dt_free[0], dt_free[1]])
                    bank_free[6] = a1
                    a2 = P.op("act", lambda e: e.activation(out=Z1[:], in_=Z1[:], func=AF.Ln), deps=[a1])
                    c1 = P.op("dve", lambda e: e.tensor_tensor_scan(
                        out=Cc[:], data0=ones32[:], data1=Z1[:], initial=0.0, op0=ALU.mult, op1=ALU.add),
                        deps=[a2, t_ones])
                    m1 = P.op("pe", lambda e: e.matmul(B[5][0:32, 0:1], tri[:], Cc[:, 511:512], start=True, stop=True),
                              deps=[c1, bank_free[5], const_tok])
                    c2 = P.op("dve", lambda e: e.tensor_copy(offs[:], B[5][0:32, 0:1]), deps=[m1])
                    c3 = P.op("dve", lambda e: e.tensor_scalar(
                        out=Cc[:], in0=Cc[:], scalar1=offs[:, 0:1], scalar2=None, op0=ALU.add), deps=[c2])
                    bank_free[5] = c2
                    tc_ready = c3
                else:
                    tcd = P.dma("sp", d_dec2, lambda e, p=p: e.dma_start(out=Cc[:], in_=cdec[p]),
                                deps=[dt_free[0], dt_free[1]])
                    tc_ready = tcd
                c4 = P.op("dve", lambda e: e.tensor_scalar(
                    out=Z1[:], in0=Cc[:], scalar1=Cc[:, 255:256], scalar2=None, op0=ALU.subtract), deps=[tc_ready])
                c5 = P.op("dve", lambda e: e.tensor_copy(RQ[:, 0, :], Z1[:]), deps=[c4])
                c6 = P.op("dve", lambda e: e.tensor_tensor(out=Z1[:], in0=Z1[:], in1=RQ[:, 0, :], op=ALU.subtract), deps=[c5])
                c7 = P.op("dve", lambda e: e.tensor_copy(RQ[:, 1, :], Z1[:]), deps=[c6])
                ck = c7
                for jj in range(4):
                    ck = P.op("dve", lambda e, jj=jj: e.tensor_scalar(
                        out=Z1[:, jj * 128:(jj + 1) * 128], in0=Cc[:, jj * 128:(jj + 1) * 128],
                        scalar1=Cc[:, jj * 128 + 63:jj * 128 + 64], scalar2=-1.0, op0=ALU.subtract, op1=ALU.mult), deps=[ck])
                c8 = P.op("dve", lambda e: e.tensor_copy(RK[:, 0, :], Z1[:]), deps=[ck])
                c9 = P.op("dve", lambda e: e.tensor_tensor(out=Z1[:], in0=Z1[:], in1=RK[:, 0, :], op=ALU.subtract), deps=[c8])
                c10 = P.op("dve", lambda e: e.tensor_copy(RK[:, 1, :], Z1[:]), deps=[c9])
                c11 = P.op("dve", lambda e: e.tensor_scalar(
                    out=refs[:, 0:16], in0=iq[:], scalar1=Cc[:, 255:256], scalar2=None, op0=ALU.mult), deps=[c10, const_tok])
                rk3 = refs[:, 16:80].rearrange("p (i j) -> p i j", j=4)
                for jj in range(4):
                    c11 = P.op("dve", lambda e, jj=jj: e.tensor_scalar(
                        out=rk3[:, :, jj], in0=iq[:], scalar1=Cc[:, jj * 128 + 63:jj * 128 + 64], scalar2=None,
                        op0=ALU.mult), deps=[c11])
                dts = []
                for r in range(2):
                    t1 = P.dma("sp", d_dec, lambda e, r=r: e.dma_start(
                        out=dscr[r, 0].rearrange("h (i t) -> i h t", i=16), in_=RQ[r * 16:(r + 1) * 16, :, :]), deps=[c11])
                    t2 = P.dma("sp", d_dec, lambda e, r=r: e.dma_start(
                        out=dscr[r, 1].rearrange("h (i t) -> i h t", i=16), in_=RK[r * 16:(r + 1) * 16, :, :]))
                    dts.append(t2)
                dts2 = []
                for r in range(2):
                    t3 = P.dma("sp", d_dec, lambda e, r=r: e.dma_start(out=QT[r][96:98, :], in_=dscr[r, 0]),
                               deps=[dts[-1], qk_last_reader[r]])
                    t4 = P.dma("sp", d_dec, lambda e, r=r: e.dma_start(out=KT[r][98:100, :], in_=dscr[r, 1]))
                    dts2.append(t4)
                dec_rows_tok = dts2[-1]
                for r in range(2):
                    m2 = P.op("pe", lambda e, r=r: e.matmul(
                        B[5][:, 0:80], sel[:, r * 128:(r + 1) * 128], refs[:], start=True, stop=True),
                        deps=[c11, bank_free[5], const_tok])
                    c12 = P.op("dve", lambda e: e.tensor_copy(bq_s[:], B[5][:, 0:16]), deps=[m2])
                    cl = c12
                    for i in range(NQT):
                        cl = P.op("dve", lambda e, r=r, i=i: e.tensor_scalar(
                            out=Dt[r][:, i * NKT:(i + 1) * NKT], in0=B[5][:, 16:80], scalar1=-1.0, scalar2=bq_s[:, i:i + 1],
                            op0=ALU.mult, op1=ALU.add), deps=[cl], inc=(i == NQT - 1))
                    bank_free[5] = cl
                    dt_ready = cl
                dt_ready_all = dt_ready
            else:
                dec_rows_tok = None
                dt_ready_all = None

            gate_done = []
            if (not mem_pass) and (not fox):
                for r in range(2):
                    k1 = P.op("dve", lambda e, r=r: e.tensor_reduce(
                        out=kbf[:], in_=KT[r][0:64, :].rearrange("p (n l) -> p n l", l=256), axis=AX.X, op=ALU.add),
                        deps=proj_done)
                    k2 = P.op("dve", lambda e, r=r: e.tensor_scalar(
                        out=kbar[r][:], in0=kbf[:], scalar1=1.0 / 256.0, scalar2=None, op0=ALU.mult), deps=[k1])
                    for i in range(NQT):
                        s = i % 2
                        for sub in range(4):
                            c0 = i * 512 + sub * 128
                            g1 = P.op("pe", lambda e, r=r, c0=c0, sub=sub: e.matmul(
                                B[5][:, sub * 32:(sub + 1) * 32], QT[r][0:64, c0:c0 + 128], kbar[r][:], start=True, stop=True),
                                deps=[k2, bank_free[5]] + proj_done, inc=(sub == 3))
                        g2 = P.op("dve", lambda e, s=s, i=i: e.tensor_tensor(
                            out=gp[s][:], in0=B[5][:, 0:128], in1=maskm[:, i * 128:(i + 1) * 128], op=ALU.add),
                            deps=[g1, const_tok])
                        bank_free[5] = g2
                        g3 = g2
                        for sub in range(4):
                            g3 = P.op("dve", lambda e, s=s, sub=sub: e.max(m8[s][:, sub, :], gp[s][:, sub * 32:(sub + 1) * 32]),
                                      deps=[g3])
                        g4 = P.op("dve", lambda e, s=s: e.tensor_scalar(
                            out=thr[s][:], in0=m8[s][:, :, 3], scalar1=-1e29, scalar2=None, op0=ALU.max), deps=[g3])
                        g5 = g4
                        for sub in range(4):
                            g5 = P.op("dve", lambda e, s=s, sub=sub: e.tensor_scalar(
                                out=TB[s][:, sub, 64:96], in0=gp[s][:, sub * 32:(sub + 1) * 32],
                                scalar1=thr[s][:, sub:sub + 1], scalar2=NEG, op0=ALU.is_lt, op1=ALU.mult),
                                deps=[g5, t_tb, TB_free[s]])
                        for sub in range(4):
                            g6 = P.op("pe", lambda e, s=s, sub=sub: e.transpose(
                                Bbf[0:96, (s * 4 + sub) * 128:(s * 4 + sub + 1) * 128], TB[s][:, sub, :], ident[:]),
                                deps=[g5, bank_free["bf"], const_tok], inc=(sub == 3))
                        g7 = P.op("act", lambda e, r=r, i=i, s=s: e.copy(QT[r][64:96, i * 512:(i + 1) * 512], Bbf[64:96, s * 512:(s + 1) * 512]),
                            deps=[g6])
                        TB_free[s] = g6
                        bank_free["bf"] = g7
                        gate_done = [g7]

            att_deps = proj_done + gate_done + [dec_rows_tok, dt_ready_all, const_tok]
            nk_rows = 64 if mem_pass else KAUG
            for r in range(2):
                hl = 2 * p + r
                for i in range(NQT):
                    u = unit
                    unit += 1
                    ob = B[3 + (u % 2)]
                    ntile = 2 if mem_pass else 4 * (i + 1)
                    qk_tok = [None] * ntile
                    ex_tok = [None] * ntile
                    pv_tok = None
                    sidx = [None] * ntile

                    def emit_qk(j):
                        nonlocal att_deps
                        sl = emit_qk.ctr % 3
                        emit_qk.ctr += 1
                        sidx[j] = sl
                        jj = j - 4 * i
                        c0 = 128 * jj if ((not mem_pass) and jj >= 0) else 0
                        t = P.op("pe", lambda e: e.matmul(
                            B[sl][:, c0:512], KT[r][0:nk_rows, j * 128:(j + 1) * 128],
                            QT[r][0:nk_rows, i * 512 + c0:(i + 1) * 512], start=True, stop=True),
                            deps=[bank_free[sl]] + att_deps)
                        att_deps = []
                        qk_tok[j] = t

                    def emit_exp(j):
                        sl = sidx[j]
                        pt = emit_exp.ctr % 4
                        emit_exp.ctr += 1
                        jj = j - 4 * i
                        diag = (not mem_pass) and jj >= 0
                        c0 = 128 * jj if diag else 0
                        bias = 0.0 if mem_pass else Dt[r][:, i * NKT + j:i * NKT + j + 1]
                        if diag:
                            ts = emit_exp.tctr % 2
                            emit_exp.tctr += 1
                            t1 = P.op("dve", lambda e: e.tensor_tensor(
                                out=TMP[ts][:, c0:512], in0=B[sl][:, c0:512], in1=negm[:, 0:512 - c0], op=ALU.add),
                                deps=[qk_tok[j], TMP_free[ts]])
                            bank_free[sl] = t1
                            t2 = P.op("act", lambda e: e.activation(
                                out=PT[pt][:, c0:512], in_=TMP[ts][:, c0:512], func=AF.Exp, bias=bias, scale=1.0),
                                deps=[t1, PT_free[pt]])
                            TMP_free[ts] = t2
                        else:
                            t2 = P.op("act", lambda e: e.activation(
                                out=PT[pt][:, :], in_=B[sl][:, :], func=AF.Exp, bias=bias, scale=1.0),
                                deps=[qk_tok[j], PT_free[pt]])
                            bank_free[sl] = t2
                        ex_tok[j] = (t2, pt, c0)

                    def emit_pv(j):
                        nonlocal pv_tok
                        t2, pt, c0 = ex_tok[j]
                        t = P.op("pe", lambda e: e.matmul(
                            ob[:, c0:512], VA[:, j, r, :], PT[pt][:, c0:512], start=(j == 0), stop=(j == ntile - 1)),
                            deps=[t2] + ([bank_free[3 + (u % 2)]] if j == 0 else []))
                        PT_free[pt] = t
                        pv_tok = t

                    emit_qk.ctr = ctrs["qk"]
                    emit_exp.ctr = ctrs["ex"]
                    emit_exp.tctr = ctrs["tx"]
                    emit_qk(0)
                    for j in range(ntile):
                        if j + 1 < ntile:
                            emit_qk(j + 1)
                        emit_exp(j)
                        emit_pv(j)
                    ctrs["qk"] = emit_qk.ctr
                    ctrs["ex"] = emit_exp.ctr
                    ctrs["tx"] = emit_exp.tctr
                    qk_last_reader[r] = pv_tok
                    os_ = u % 2
                    n1 = P.op("dve", lambda e: e.reciprocal(RR[os_][:], ob[64:128, :]), deps=[pv_tok, RR_free[os_]])
                    n2 = P.op("dve", lambda e: e.tensor_tensor(out=OS[os_][:], in0=ob[0:64, :], in1=RR[os_][:], op=ALU.mult),
                              deps=[n1, OS_free[os_]])
                    bank_free[3 + (u % 2)] = n2
                    RR_free[os_] = n2
                    to = P.dma("sp", d_o[os_], lambda e: e.dma_start(
                        out=OT[hl * 64:(hl + 1) * 64, i * 512:(i + 1) * 512], in_=OS[os_][:]), deps=[n2])
                    OS_free[os_] = to
                    out_toks.append(to)
                dt_free[r] = qk_last_reader[r]
        barrier(P)


_CONST = {}


def attn_consts(kind, hh):
    key = (kind, hh)
    if key in _CONST:
        return _CONST[key]
    c = {}
    c["ident"] = np.eye(128, dtype=np.float32)
    p = np.arange(128)[:, None]
    f = np.arange(512)[None, :]
    c["negm"] = np.where(f >= p, 0.0, NEG).astype(np.float32)
    t = np.arange(SEQ)
    kaug = np.zeros((36, SEQ), np.float32)
    kaug[t // 256, t] = 1.0
    kaug[32:34] = 1.0
    c["kaug"] = kaug
    qaug = np.zeros((36, SEQ), np.float32)
    qaug[34:36] = 1.0
    c["qaug"] = qaug
    k = np.arange(32)
    c["iq"] = (k[:, None] % 16 == np.arange(16)[None, :]).astype(np.float32)
    sel = np.zeros((32, 256), np.float32)
    for r in range(2):
        sel[r * 16:(r + 1) * 16, r * 128:(r + 1) * 128] = 1.0
    c["sel"] = sel
    if kind == "fox":
        c["tri"] = ((k[:, None] // 16 == k[None, :] // 16) & (k[:, None] < k[None, :])).astype(np.float32)
    else:
        slopes = _alibi_slopes(N_SELF)
        cdec = np.zeros((3, 32, 512), np.float32)
        for pp in range(3):
            for r in range(2):
                h = 6 * hh + 2 * pp + r
                for i in range(16):
                    cdec[pp, r * 16 + i] = -slopes[h] * (i * 512 + np.arange(512, dtype=np.float32))
        c["cdec"] = cdec
        mm = np.zeros((64, 32), np.float32)
        for s in range(64):
            qb = s // 2
            mm[s, :] = np.where(np.arange(32) < qb, 0.0, np.where(np.arange(32) == qb, 1e30, -1e30))
        c["maskm"] = np.ascontiguousarray(np.broadcast_to(mm.reshape(1, 2048), (128, 2048))).astype(np.float32)
    _CONST[key] = c
    return c


def attn_inputs(kind, hh, hT_b, memT_b, w_in, b_fg, w_mem_kv):
    SW = N_SELF * HEAD_DIM
    heads = slice(6 * hh * 64, (6 * hh + 6) * 64)
    m = dict(attn_consts(kind, hh))
    wq_self = w_in[:, 0:SW][:, heads]
    wq_mem = w_in[:, 3 * SW:3 * SW + 256][:, hh * 128:(hh + 1) * 128]
    m["wq"] = np.ascontiguousarray(np.concatenate([wq_self, wq_mem], axis=1))
    m["wk"] = np.ascontiguousarray(w_in[:, SW:2 * SW][:, heads])
    m["wv"] = np.ascontiguousarray(w_in[:, 2 * SW:3 * SW][:, heads])
    m["hT"] = hT_b
    m["memT"] = memT_b
    m["wmk"] = np.ascontiguousarray(w_mem_kv[:, 0:256][:, hh * 128:(hh + 1) * 128])
    m["wmv"] = np.ascontiguousarray(w_mem_kv[:, 256:512][:, hh * 128:(hh + 1) * 128])
    if kind == "fox":
        wfg = w_in[:, 2560:2572]
        wfgp = np.zeros((3, 1024, 16, 32), np.float32)
        bfg = np.zeros((32, 3), np.float32)
        for pp in range(3):
            for r in range(2):
                h = 6 * hh + 2 * pp + r
                for i in range(16):
                    wfgp[pp, :, i, r * 16 + i] = wfg[:, h]
                bfg[r * 16:(r + 1) * 16, pp] = b_fg[h]
        m["wfgp"] = wfgp.reshape(3, 1024, 512)
        m["bfg"] = bfg
    return m


NTOK = 4096
NTT = NTOK // 128
CAP = 640
NSLOT = N_EXPERTS * CAP
PART = 320


def barrier(P):
    keys = list(P.cnt.keys())
    for e in P.engs:
        for k in keys:
            if P.cnt[k] > 0:
                P.wait(e, (k, P.cnt[k]))


def emit_ffn(nc, P, B, Bbf, E, L):
    sfx = "_f%d" % L
    h_in = E["x_own"] if L == 0 else E["hres"]
    out = E["out"] if L == NL - 1 else E["hres"]
    emit_hT = L < NL - 1
    OTv = E["OT_pair"].rearrange("c r q (h g t) -> (c r q h g) t", h=2, g=8)
    idxm_d = E["idxm"]
    wo, ln1g, ln1b, ln2g, ln2b = E["wo"][L], E["ln1g"][L], E["ln1b"][L], E["ln2g"][L], E["ln2b"][L]
    wr, rb = E["wr"][L], E["rb"][L]
    wgu, bgu, wdn, bdn = E["wgu"][L], E["bgu"][L], E["wdn"][L], E["bdn"][L]
    ident_d, tstr_d, iota_d, ecap_d = E["ident"], E["tstr"], E["iotae"], E["ecap"]
    H1, Xs, Ys, hT_own = E["H1"], E["Xs"], E["Ys"], E["hT_own"]
    bc_reg, bc_reg2 = E["bc_reg"], E["bc_reg2"]

    with contextlib.ExitStack() as st:
        def sbp(name, shape, dt):
            return st.enter_context(nc.sbuf_tensor(name + sfx, shape, dt))

        gates = sbp("gates", [128, NTT, 4], F32)
        slots = sbp("slots", [128, NTT * 4], I32)
        identb = sbp("identb", [128, 128], BF16)
        identf = sbp("identf", [128, 128], F32)
        tstr = sbp("tstr_s", [128, 128], F32)
        onesf = sbp("onesf", [128, 128], F32)
        iotae = sbp("iotae_s", [128, 32], F32)
        ecap = sbp("ecap_s", [128, 32], F32)
        onesb = sbp("onesb", [1, 128], BF16)
        idxm = sbp("idxm_s", [128, 64], I32)

        d_c = P.dsem("const")
        P.dma("pool", d_c, lambda e: e.dma_start(out=identb[:], in_=ident_d))
        P.dma("sp", d_c, lambda e: e.dma_start(out=identf[:], in_=ident_d))
        P.dma("sp", d_c, lambda e: e.dma_start(out=tstr[:], in_=tstr_d))
        P.dma("sp", d_c, lambda e: e.dma_start(out=iotae[:], in_=iota_d))
        P.dma("sp", d_c, lambda e: e.dma_start(out=idxm[:], in_=idxm_d))
        const_tok = P.dma("sp", d_c, lambda e: e.dma_start(out=ecap[:], in_=ecap_d))
        t_ones = P.op("pool", lambda e: e.memset(onesf[:], 1.0))
        t_ones = P.op("pool", lambda e: e.memset(onesb[:], 1.0))

        with contextlib.ExitStack() as s1:
            def sb(name, shape, dt):
                return s1.enter_context(nc.sbuf_tensor(name + sfx, shape, dt))
            wo_s = sb("wo_s", [128, 8, 1024], BF16)
            g1 = sb("g1", [128, 1024], F32)
            b1 = sb("b1", [128, 1024], F32)
            wr_s = sb("wr_s", [128, 8, 32], BF16)
            rb_s = sb("rb_s", [128, 32], F32)
            mTr = [sb("mTr%d" % i, [128, 8, 512], BF16) for i in range(2)]
            htr = [sb("htr%d" % i, [128, 1024], F32) for i in range(2)]
            rt = [sb("rt%d" % i, [128, 1024], F32) for i in range(2)]
            h1r = [sb("h1r%d" % i, [128, 1024], F32) for i in range(2)]
            h1b = [sb("h1b%d" % i, [128, 1024], BF16) for i in range(3)]
            h1T = [sb("h1T%d" % i, [128, 8, 128], BF16) for i in range(2)]
            stats = [sb("stats%d" % i, [128, 2, 6], F32) for i in range(2)]
            mv = [sb("mv%d" % i, [128, 2], F32) for i in range(2)]
            rstd = [sb("rstd%d" % i, [128, 1], F32) for i in range(2)]
            Lg = [sb("Lg%d" % i, [128, 32], F32) for i in range(2)]
            m8 = [sb("m8_%d" % i, [128, 8], F32) for i in range(2)]
            i8 = [sb("i8_%d" % i, [128, 8], U32) for i in range(2)]
            i8f = [sb("i8f_%d" % i, [128, 8], F32) for i in range(2)]
            nm0 = [sb("nm0_%d" % i, [128, 1], F32) for i in range(2)]
            e4 = [sb("e4_%d" % i, [128, 4], F32) for i in range(2)]
            ssum = [sb("ssum%d" % i, [128, 1], F32) for i in range(2)]
            Mk = [sb("Mk%d" % i, [128, 32], F32) for i in range(2)]
            Srun = [sb("Srun%d" % i, [128, 32], F32) for i in range(2)]
            sbase = [sb("sbase%d" % i, [128, 32], F32) for i in range(2)]
            ovf = [sb("ovf%d" % i, [128, 32], F32) for i in range(2)]
            prod = [sb("prod%d" % i, [128, 4, 32], F32) for i in range(2)]
            slotf = [sb("slotf%d" % i, [128, 4], F32) for i in range(2)]
            zt = sb("zt", [128, 5, 1024], BF16)

            d_w1 = P.dsem("w1")
            P.dma("pool", d_w1, lambda e: e.dma_start(out=wo_s[:], in_=wo.rearrange("(kc p) t -> p kc t", p=128)))
            P.dma("pool", d_w1, lambda e: e.dma_start(out=wr_s[:], in_=wr.rearrange("(kc p) t -> p kc t", p=128)))
            P.dma("sp", d_w1, lambda e: e.dma_start(out=g1[:], in_=ln1g))
            P.dma("sp", d_w1, lambda e: e.dma_start(out=b1[:], in_=ln1b))
            w1_tok = P.dma("sp", d_w1, lambda e: e.dma_start(out=rb_s[:], in_=rb))
            d_z = P.dsem("z")
            z_tok = None
            if L == 0:
                tz = P.op("pool", lambda e: e.memset(zt[:], 0.0))
                for ex in range(N_EXPERTS):
                    z_tok = P.dma("sp", d_z, lambda e, ex=ex: e.dma_start(
                        out=Xs[ex * CAP:(ex + 1) * CAP, :].rearrange("(s p) d -> p s d", p=128), in_=zt[:]), deps=[tz])
            t_s0 = P.op("pool", lambda e: e.memset(Srun[0][:], 0.0))

            d_m = [P.dsem("m%d" % i) for i in range(2)]
            d_h = [P.dsem("h%d" % i) for i in range(2)]
            d_h1 = [P.dsem("h1_%d" % i) for i in range(2)]
            d_sc = [P.dsem("sc%d" % i) for i in range(3)]
            mT_free = [None, None]
            ht_free = [None, None]
            rt_free = [None, None]
            h1r_free = [[], []]
            h1b_free = [None] * 3
            h1T_free = [None, None]
            bfree = {i: None for i in range(7)}
            bfree["bf"] = None
            st1 = {}
            srun_tok = t_s0
            last_scatter = None
            m_tok = None

            def stage1(t):
                nonlocal m_tok
                s = t % 2
                if t % 4 == 0:
                    ms = (t // 4) % 2
                    for kc in range(8):
                        m_tok = P.dma("pool", d_m[ms], lambda e, kc=kc: e.indirect_dma_start(
                            out=mTr[ms][:, kc, :], out_offset=None, in_=OTv,
                            in_offset=bass.IndirectOffsetOnAxis(ap=idxm[:, kc * 8 + t // 4:kc * 8 + t // 4 + 1], axis=0),
                            bounds_check=bc_reg2, oob_is_err=False), deps=[mT_free[ms], const_tok])
                ms = (t // 4) % 2
                tl = t % 4
                th = P.dma("sp", d_h[s], lambda e: e.dma_start(out=htr[s][:], in_=h_in[t * 128:(t + 1) * 128, :]),
                           deps=[ht_free[s]])
                for half in range(2):
                    for kc in range(8):
                        tm = P.op("pe", lambda e: e.matmul(
                            B[half][:], mTr[ms][:, kc, tl * 128:(tl + 1) * 128], wo_s[:, kc, half * 512:(half + 1) * 512],
                            start=(kc == 0), stop=(kc == 7)), deps=[m_tok, w1_tok, bfree[half]], inc=(kc == 7))
                    r1 = P.op("dve", lambda e: e.scalar_tensor_tensor(
                        out=rt[s][:, half * 512:(half + 1) * 512], in0=htr[s][:, half * 512:(half + 1) * 512], scalar=ALPHA,
                        in1=B[half][:], op0=ALU.mult, op1=ALU.add), deps=[tm, th, rt_free[s]])
                    bfree[half] = r1
                    r2 = P.op("dve", lambda e: e.bn_stats(stats[s][:, half, :], rt[s][:, half * 512:(half + 1) * 512]), deps=[r1])
                mT_free[ms] = tm
                ht_free[s] = r1
                r3 = P.op("dve", lambda e: e.bn_aggr(mv[s][:], stats[s][:].rearrange("p a b -> p (a b)")), deps=[r2])
                r4 = P.op("dve", lambda e: e.tensor_scalar(
                    out=rstd[s][:], in0=mv[s][:, 1:2], scalar1=LN_EPS, scalar2=None, op0=ALU.add), deps=[r3])
                r4 = P.op("act", lambda e: e.sqrt(rstd[s][:], rstd[s][:]), deps=[r4])
                r4 = P.op("dve", lambda e: e.reciprocal(rstd[s][:], rstd[s][:]), deps=[r4])
                r5 = P.op("dve", lambda e: e.tensor_scalar(
                    out=rt[s][:], in0=rt[s][:], scalar1=mv[s][:, 0:1], scalar2=rstd[s][:, 0:1],
                    op0=ALU.subtract, op1=ALU.mult), deps=[r4])
                r6 = P.op("dve", lambda e: e.tensor_tensor(out=h1r[s][:], in0=rt[s][:], in1=g1[:], op=ALU.mult),
                          deps=[r5, w1_tok] + h1r_free[s])
                rt_free[s] = r6
                r7 = P.op("dve", lambda e: e.tensor_tensor(out=h1r[s][:], in0=h1r[s][:], in1=b1[:], op=ALU.add), deps=[r6])
                ts_ = P.dma("sp", d_h1[s], lambda e: e.dma_start(out=H1[t * 128:(t + 1) * 128, :], in_=h1r[s][:]), deps=[r7])
                bs = t % 3
                r8 = P.op("act", lambda e: e.copy(h1b[bs][:], h1r[s][:]), deps=[r7, h1b_free[bs]])
                h1r_free[s] = [ts_, r8]
                st1[t] = r8

            def stage2(t):
                nonlocal srun_tok, last_scatter
                s = t % 2
                bs = t % 3
                for kc in range(8):
                    t1 = P.op("pe", lambda e: e.transpose(Bbf[:, kc * 128:(kc + 1) * 128], h1b[bs][:, kc * 128:(kc + 1) * 128], identb[:]),
                              deps=[st1[t], bfree["bf"], const_tok], inc=(kc == 7))
                t2 = P.op("act", lambda e: e.copy(h1T[s][:], Bbf[:].rearrange("p (a c) -> p a c", a=8)), deps=[t1, h1T_free[s]])
                bfree["bf"] = t2
                for kc in range(8):
                    t3 = P.op("pe", lambda e: e.matmul(B[2][:, 0:32], h1T[s][:, kc, :], wr_s[:, kc, :], start=(kc == 0), stop=(kc == 7)),
                              deps=[t2, bfree[2]], inc=(kc == 7))
                h1T_free[s] = t3
                a1 = P.op("dve", lambda e: e.tensor_tensor(out=Lg[s][:], in0=B[2][:, 0:32], in1=rb_s[:], op=ALU.add), deps=[t3, w1_tok])
                bfree[2] = a1
                a2 = P.op("dve", lambda e: e.max(m8[s][:], Lg[s][:]), deps=[a1])
                a3 = P.op("dve", lambda e: e.max_index(i8[s][:], m8[s][:], Lg[s][:]), deps=[a2])
                a4 = P.op("dve", lambda e: e.tensor_copy(i8f[s][:], i8[s][:]), deps=[a3])
                a5 = P.op("dve", lambda e: e.tensor_scalar(out=nm0[s][:], in0=m8[s][:, 0:1], scalar1=-1.0, scalar2=None, op0=ALU.mult), deps=[a4])
                a6 = P.op("act", lambda e: e.activation(out=e4[s][:], in_=m8[s][:, 0:4], func=AF.Exp, bias=nm0[s][:, 0:1], scale=1.0),
                          deps=[a5])
                a7 = P.op("dve", lambda e: e.tensor_reduce(out=ssum[s][:], in_=e4[s][:], axis=AX.X, op=ALU.add), deps=[a6])
                a8 = P.op("dve", lambda e: e.reciprocal(ssum[s][:], ssum[s][:]), deps=[a7])
                a9 = P.op("dve", lambda e: e.tensor_scalar(out=gates[:, t, :], in0=e4[s][:], scalar1=ssum[s][:, 0:1], scalar2=None, op0=ALU.mult), deps=[a8])
                a10 = P.op("dve", lambda e: e.tensor_scalar(out=Mk[s][:], in0=Lg[s][:], scalar1=m8[s][:, 3:4], scalar2=None, op0=ALU.is_ge), deps=[a9])
                p1 = P.op("pe", lambda e: e.matmul(B[3][:, 0:32], tstr[:], Mk[s][:], start=True, stop=False), deps=[a10, bfree[3], const_tok], inc=False)
                p2 = P.op("pe", lambda e: e.matmul(B[3][:, 0:32], onesf[:], Srun[s][:], start=False, stop=True), deps=[srun_tok, t_ones])
                srun_tok = P.op("pool", lambda e: e.tensor_tensor(out=Srun[1 - s][:], in0=Srun[s][:], in1=Mk[s][:], op=ALU.add), deps=[a10, p2, srun_tok])
                q1 = P.op("dve", lambda e: e.tensor_tensor(out=sbase[s][:], in0=B[3][:, 0:32], in1=ecap[:], op=ALU.add), deps=[p2, const_tok])
                q2 = P.op("dve", lambda e: e.tensor_scalar(out=ovf[s][:], in0=B[3][:, 0:32], scalar1=CAP - 0.5, scalar2=1.0e6, op0=ALU.is_gt, op1=ALU.mult), deps=[q1])
                bfree[3] = q2
                q3 = P.op("dve", lambda e: e.tensor_tensor(out=sbase[s][:], in0=sbase[s][:], in1=ovf[s][:], op=ALU.add), deps=[q2])
                q4 = q3
                for k in range(4):
                    q4 = P.op("dve", lambda e: e.scalar_tensor_tensor(
                        out=prod[s][:, k, :], in0=iotae[:], scalar=i8f[s][:, k:k + 1], in1=sbase[s][:], op0=ALU.is_equal, op1=ALU.mult),
                        deps=[q4, const_tok])
                q5 = P.op("dve", lambda e: e.tensor_reduce(out=slotf[s][:], in_=prod[s][:], axis=AX.X, op=ALU.add), deps=[q4])
                q6 = P.op("dve", lambda e: e.tensor_copy(slots[:, t * 4:t * 4 + 4], slotf[s][:]), deps=[q5])
                for k in range(4):
                    last_scatter = P.dma("pool", d_sc[bs], lambda e: e.indirect_dma_start(
                        out=Xs, out_offset=bass.IndirectOffsetOnAxis(ap=slots[:, t * 4 + k:t * 4 + k + 1], axis=0),
                        in_=h1b[bs][:, :], in_offset=None, bounds_check=bc_reg, oob_is_err=False),
                        deps=[q6, z_tok])
                h1b_free[bs] = last_scatter

            for t in range(NTT + 1):
                if t < NTT:
                    stage1(t)
                if t >= 1:
                    stage2(t - 1)
            barrier(P)

        with contextlib.ExitStack() as s2:
            def sb(name, shape, dt):
                return s2.enter_context(nc.sbuf_tensor(name + sfx, shape, dt))
            xin = [sb("xin%d" % i, [128, 5, 1024], BF16) for i in range(2)]
            XeT = [sb("XeT%d" % i, [128, 8, CAP], BF16) for i in range(2)]
            NW = 5
            wring = [sb("wring%d" % i, [128, 8, 512], BF16) for i in range(NW)]
            AT = sb("AT", [128, 8, CAP], BF16)
            gs = [sb("gs%d" % i, [128, PART], F32) for i in range(2)]
            sg = [sb("sg%d" % i, [128, PART], F32) for i in range(2)]
            u1 = [sb("u1_%d" % i, [128, PART], F32) for i in range(2)]
            tt = [sb("tt%d" % i, [128, PART], F32) for i in range(2)]
            Yt = [sb("Yt%d" % i, [128, 512], F32) for i in range(3)]
            bgu_s = sb("bgu_s", [N_EXPERTS, 2048], F32)
            bguT = sb("bguT", [128, 16, N_EXPERTS], F32)
            bdn_r = [sb("bdn%d" % i, [1, 1024], BF16) for i in range(2)]

            d_b = P.dsem("bias")
            tb = P.dma("sp", d_b, lambda e: e.dma_start(out=bgu_s[:], in_=bgu))
            for c in range(16):
                x1 = P.op("pe", lambda e, c=c: e.transpose(B[6][:, 0:32], bgu_s[:, c * 128:(c + 1) * 128], identf[0:32, 0:32]),
                          deps=[tb, const_tok] + ([x2] if c > 0 else []))
                x2 = P.op("dve", lambda e, c=c: e.tensor_copy(bguT[:, c, :], B[6][:, 0:32]), deps=[x1])
            x3 = P.op("dve", lambda e: e.tensor_scalar(out=bguT[:, 8:16, :], in0=bguT[:, 8:16, :], scalar1=1.0, scalar2=None, op0=ALU.add), deps=[x2])
            bias_tok = x3

            d_x = [P.dsem("x%d" % i) for i in range(2)]
            d_wr = [P.dsem("wr%d" % i) for i in range(NW)]
            d_bd = [P.dsem("bd%d" % i) for i in range(2)]
            d_y = [P.dsem("y%d" % i) for i in range(3)]
            xin_free = [None, None]
            XeT_free = [None, None]
            wr_free = [None] * NW
            bd_free = [None, None]
            Yt_free = [None] * 3
            AT_free = None
            bfree = {i: None for i in range(7)}
            bfree["bf"] = None
            act_free = [None, None]
            wctr = 0
            yctr = 0
            actr = 0
            y_toks = []

            pieces = []
            for ex in range(N_EXPERTS):
                for q in range(4):
                    pieces.append((ex, "gu", q))
                for hf in range(2):
                    pieces.append((ex, "dn", hf))
            piece_tok = {}
            issued = 0

            def issue_piece():
                nonlocal issued
                if issued >= len(pieces):
                    return
                ex, kd, q = pieces[issued]
                sl = issued % NW
                if kd == "gu":
                    P.dma("pool", d_wr[sl], lambda e: e.dma_start(
                        out=wring[sl][:, :, 0:256], in_=wgu[ex][:, q * 256:(q + 1) * 256].rearrange("(kc p) c -> p kc c", p=128)),
                        deps=[wr_free[sl]])
                    tk = P.dma("pool", d_wr[sl], lambda e: e.dma_start(
                        out=wring[sl][:, :, 256:512], in_=wgu[ex][:, 1024 + q * 256:1024 + (q + 1) * 256].rearrange("(kc p) c -> p kc c", p=128)))
                else:
                    tk = P.dma("pool", d_wr[sl], lambda e: e.dma_start(
                        out=wring[sl][:, :, :], in_=wdn[ex][:, q * 512:(q + 1) * 512].rearrange("(kc p) c -> p kc c", p=128)),
                        deps=[wr_free[sl]])
                piece_tok[(ex, kd, q)] = (tk, sl)
                issued += 1

            for _ in range(NW - 1):
                issue_piece()

            x_tok = {}

            def load_x(ex):
                xs = ex % 2
                x_tok[ex] = P.dma("sp", d_x[xs], lambda e: e.dma_start(
                    out=xin[xs][:], in_=Xs[ex * CAP:(ex + 1) * CAP, :].rearrange("(s p) d -> p s d", p=128)),
                    deps=[xin_free[xs]])

            load_x(0)
            for ex in range(N_EXPERTS):
                xs = ex % 2
                if ex + 1 < N_EXPERTS:
                    load_x(ex + 1)
                tbd = P.dma("pool", d_bd[xs], lambda e: e.dma_start(out=bdn_r[xs][:], in_=bdn[ex:ex + 1, :]), deps=[bd_free[xs]])
                for sidx in range(5):
                    for kc in range(8):
                        t1 = P.op("pe", lambda e: e.transpose(Bbf[:, kc * 128:(kc + 1) * 128], xin[xs][:, sidx, kc * 128:(kc + 1) * 128], identb[:]),
                                  deps=[x_tok[ex], bfree["bf"]], inc=(kc == 7))
                    t2 = P.op("act", lambda e: e.copy(XeT[xs][:, :, sidx * 128:(sidx + 1) * 128], Bbf[:].rearrange("p (a c) -> p a c", a=8)),
                              deps=[t1, XeT_free[xs]])
                    bfree["bf"] = t2
                xin_free[xs] = t1
                xet_tok = t2
                last_pool = None
                for q in range(4):
                    issue_piece()
                    wt, sl = piece_tok[(ex, "gu", q)]
                    for cl in range(2):
                        c = 2 * q + cl
                        for part in range(2):
                            cs = slice(part * PART, (part + 1) * PART)
                            for kc in range(8):
                                tg = P.op("pe", lambda e: e.matmul(B[part][:, 0:PART], wring[sl][:, kc, cl * 128:(cl + 1) * 128], XeT[xs][:, kc, cs],
                                                                   start=(kc == 0), stop=(kc == 7)), deps=[wt, xet_tok, bfree[part]], inc=(kc == 7))
                            for kc in range(8):
                                tu = P.op("pe", lambda e: e.matmul(B[2 + part][:, 0:PART], wring[sl][:, kc, 256 + cl * 128:256 + (cl + 1) * 128], XeT[xs][:, kc, cs],
                                                                   start=(kc == 0), stop=(kc == 7)), deps=[bfree[2 + part]], inc=(kc == 7))
                            a = actr % 2
                            actr += 1
                            v1 = P.op("dve", lambda e: e.tensor_scalar(out=gs[a][:], in0=B[part][:, 0:PART], scalar1=bguT[:, c, ex:ex + 1], scalar2=SWIGLU_LIMIT,
                                                                       op0=ALU.add, op1=ALU.min), deps=[tg, bias_tok, act_free[a]])
                            bfree[part] = v1
                            v2 = P.op("act", lambda e: e.activation(out=sg[a][:], in_=gs[a][:], func=AF.Sigmoid, scale=SWIGLU_ALPHA), deps=[v1])
                            v3 = P.op("dve", lambda e: e.tensor_scalar(out=u1[a][:], in0=B[2 + part][:, 0:PART], scalar1=bguT[:, 8 + c, ex:ex + 1], scalar2=SWIGLU_LIMIT + 1.0,
                                                                       op0=ALU.add, op1=ALU.min), deps=[tu])
                            bfree[2 + part] = v3
                            v4 = P.op("dve", lambda e: e.scalar_tensor_tensor(out=tt[a][:], in0=u1[a][:], scalar=-SWIGLU_LIMIT + 1.0, in1=gs[a][:],
                                                                              op0=ALU.max, op1=ALU.mult), deps=[v3, v1])
                            v5 = P.op("dve", lambda e: e.tensor_tensor(out=AT[:, c, cs], in0=tt[a][:], in1=sg[a][:], op=ALU.mult), deps=[v2, v4, AT_free])
                            act_free[a] = v5
                            last_pool = v5
                    wr_free[sl] = tu
                XeT_free[xs] = tu
                for hf in range(2):
                    issue_piece()
                    wt, sl = piece_tok[(ex, "dn", hf)]
                    for sidx in range(5):
                        yb = 4 + (yctr % 2)
                        for kc in range(8):
                            td = P.op("pe", lambda e: e.matmul(B[yb][:], AT[:, kc, sidx * 128:(sidx + 1) * 128], wring[sl][:, kc, :],
                                                               start=(kc == 0), stop=False), deps=[wt, last_pool, bfree[yb]], inc=False)
                        td = P.op("pe", lambda e: e.matmul(B[yb][:], onesb[0:1, :], bdn_r[xs][0:1, hf * 512:(hf + 1) * 512], start=False, stop=True),
                                  deps=[tbd, t_ones])
                        ys = yctr % 3
                        yctr += 1
                        w1 = P.op("act", lambda e: e.copy(Yt[ys][:], B[yb][:]), deps=[td, Yt_free[ys]])
                        bfree[yb] = w1
                        ty = P.dma("sp", d_y[ys], lambda e: e.dma_start(
                            out=Ys[ex * CAP + sidx * 128:ex * CAP + (sidx + 1) * 128, hf * 512:(hf + 1) * 512], in_=Yt[ys][:]), deps=[w1])
                        Yt_free[ys] = ty
                        y_toks.append(ty)
                    wr_free[sl] = td
                AT_free = td
                bd_free[xs] = td
            barrier(P)

        with contextlib.ExitStack() as s3:
            def sb(name, shape, dt):
                return s3.enter_context(nc.sbuf_tensor(name + sfx, shape, dt))
            g2 = sb("g2", [128, 1024], F32)
            b2 = sb("b2", [128, 1024], F32)
            Yg = [sb("Yg%d" % i, [128, 4, 1024], F32) for i in range(2)]
            h1t = [sb("h1t%d" % i, [128, 1024], F32) for i in range(2)]
            acc = [sb("acc%d" % i, [128, 1024], F32) for i in range(2)]
            ot = [sb("ot%d" % i, [128, 1024], F32) for i in range(2)]
            stats = [sb("stats3_%d" % i, [128, 2, 6], F32) for i in range(2)]
            mv = [sb("mv3_%d" % i, [128, 2], F32) for i in range(2)]
            rstd = [sb("rstd3_%d" % i, [128, 1], F32) for i in range(2)]
            d_w3 = P.dsem("w3")
            P.dma("sp", d_w3, lambda e: e.dma_start(out=g2[:], in_=ln2g))
            w3_tok = P.dma("sp", d_w3, lambda e: e.dma_start(out=b2[:], in_=ln2b))
            d_g = [P.dsem("g%d" % i) for i in range(2)]
            d_h3 = [P.dsem("h3_%d" % i) for i in range(2)]
            d_out = [P.dsem("out%d" % i) for i in range(2)]
            for s in range(2):
                P.op("pool", lambda e, s=s: e.memset(Yg[s][:], 0.0))
            Yg_free = [None, None]
            h1t_free = [None, None]
            ot_free = [[], []]
            outs = []
            gt = {}
            hst = hT_state(nc, P, s3, sfx) if emit_hT else None

            def gather(t):
                s = t % 2
                for k in range(4):
                    gt[t] = P.dma("pool", d_g[s], lambda e: e.indirect_dma_start(
                        out=Yg[s][:, k, :], out_offset=None, in_=Ys,
                        in_offset=bass.IndirectOffsetOnAxis(ap=slots[:, t * 4 + k:t * 4 + k + 1], axis=0),
                        bounds_check=bc_reg, oob_is_err=False), deps=[Yg_free[s]])

            gather(0)
            for t in range(NTT):
                s = t % 2
                if t + 1 < NTT:
                    gather(t + 1)
                th = P.dma("sp", d_h3[s], lambda e: e.dma_start(out=h1t[s][:], in_=H1[t * 128:(t + 1) * 128, :]), deps=[h1t_free[s]])
                c1 = P.op("dve", lambda e: e.tensor_scalar(out=acc[s][:], in0=Yg[s][:, 0, :], scalar1=gates[:, t, 0:1], scalar2=None, op0=ALU.mult),
                          deps=[gt[t]])
                c2 = P.op("dve", lambda e: e.scalar_tensor_tensor(out=acc[s][:], in0=Yg[s][:, 1, :], scalar=gates[:, t, 1:2], in1=acc[s][:],
                                                                   op0=ALU.mult, op1=ALU.add), deps=[c1, gt[t]])
                c3 = P.op("dve", lambda e: e.scalar_tensor_tensor(out=acc[s][:], in0=Yg[s][:, 2, :], scalar=gates[:, t, 2:3], in1=acc[s][:],
                                                                  op0=ALU.mult, op1=ALU.add), deps=[c2])
                c4 = P.op("dve", lambda e: e.scalar_tensor_tensor(out=acc[s][:], in0=Yg[s][:, 3, :], scalar=gates[:, t, 3:4], in1=acc[s][:],
                                                                   op0=ALU.mult, op1=ALU.add), deps=[c3])
                Yg_free[s] = c4
                c5 = P.op("dve", lambda e: e.scalar_tensor_tensor(out=acc[s][:], in0=h1t[s][:], scalar=ALPHA, in1=acc[s][:],
                                                                  op0=ALU.mult, op1=ALU.add), deps=[c4, th])
                h1t_free[s] = c5
                for half in range(2):
                    c6 = P.op("dve", lambda e: e.bn_stats(stats[s][:, half, :], acc[s][:, half * 512:(half + 1) * 512]), deps=[c5])
                c7 = P.op("dve", lambda e: e.bn_aggr(mv[s][:], stats[s][:].rearrange("p a b -> p (a b)")), deps=[c6])
                c8 = P.op("dve", lambda e: e.tensor_scalar(out=rstd[s][:], in0=mv[s][:, 1:2], scalar1=LN_EPS, scalar2=None, op0=ALU.add), deps=[c7])
                c8 = P.op("act", lambda e: e.sqrt(rstd[s][:], rstd[s][:]), deps=[c8])
                c8 = P.op("dve", lambda e: e.reciprocal(rstd[s][:], rstd[s][:]), deps=[c8])
                c9 = P.op("dve", lambda e: e.tensor_scalar(out=acc[s][:], in0=acc[s][:], scalar1=mv[s][:, 0:1], scalar2=rstd[s][:, 0:1],
                                                           op0=ALU.subtract, op1=ALU.mult), deps=[c8])
                c10 = P.op("dve", lambda e: e.tensor_tensor(out=ot[s][:], in0=acc[s][:], in1=g2[:], op=ALU.mult), deps=[c9, w3_tok] + ot_free[s])
                c11 = P.op("dve", lambda e: e.tensor_tensor(out=ot[s][:], in0=ot[s][:], in1=b2[:], op=ALU.add), deps=[c10])
                to = P.dma("sp", d_out[s], lambda e: e.dma_start(out=out[t * 128:(t + 1) * 128, :], in_=ot[s][:]), deps=[c11])
                ot_free[s] = [to]
                outs.append(to)
                if emit_hT:
                    x3 = hT_tile(nc, P, Bbf, identb, hst, ot[s], c11, t, hT_own, [const_tok])
                    ot_free[s].append(hst["cast_tok"])
            barrier(P)


def hT_state(nc, P, stack, sfx):
    stt = {}
    stt["obf"] = [stack.enter_context(nc.sbuf_tensor("obf%d%s" % (i, sfx), [128, 1024], BF16)) for i in range(2)]
    stt["hTs"] = [stack.enter_context(nc.sbuf_tensor("hTs%d%s" % (i, sfx), [128, 8, 512], BF16)) for i in range(2)]
    stt["obf_free"] = [None, None]
    stt["hTs_free"] = [None, None]
    stt["bf_free"] = None
    stt["d"] = [P.dsem("hts0"), P.dsem("hts1")]
    stt["cast_tok"] = None
    return stt


def hT_tile(nc, P, Bbf, identb, stt, src, src_tok, t, hT_own, extra):
    s = t % 2
    g, tl = t // 4, t % 4
    hs = g % 2
    obf, hTs = stt["obf"], stt["hTs"]
    x1 = P.op("act", lambda e: e.copy(obf[s][:], src[:]), deps=[src_tok, stt["obf_free"][s]])
    stt["cast_tok"] = x1
    for kc in range(8):
        x2 = P.op("pe", lambda e, kc=kc: e.transpose(Bbf[:, kc * 128:(kc + 1) * 128], obf[s][:, kc * 128:(kc + 1) * 128], identb[:]),
                  deps=[x1, stt["bf_free"]] + extra, inc=(kc == 7))
    stt["obf_free"][s] = x2
    x3 = P.op("dve", lambda e: e.tensor_copy(hTs[hs][:, :, tl * 128:(tl + 1) * 128], Bbf[:].rearrange("p (a c) -> p a c", a=8)),
              deps=[x2] + ([stt["hTs_free"][hs]] if tl == 0 else []))
    stt["bf_free"] = x3
    if tl == 3:
        tw = P.dma("sp", stt["d"][hs], lambda e: e.dma_start(
            out=hT_own[:, g * 512:(g + 1) * 512].rearrange("(kc p) t -> p kc t", p=128), in_=hTs[hs][:]), deps=[x3])
        stt["hTs_free"][hs] = tw
    return x3


def emit_hT0(nc, P, Bbf, E):
    with contextlib.ExitStack() as st:
        identb = st.enter_context(nc.sbuf_tensor("identb_t0", [128, 128], BF16))
        xt = [st.enter_context(nc.sbuf_tensor("xt%d_t0" % i, [128, 1024], F32)) for i in range(2)]
        d_c = P.dsem("const")
        ct = P.dma("pool", d_c, lambda e: e.dma_start(out=identb[:], in_=E["ident"]))
        stt = hT_state(nc, P, st, "_t0")
        d_x = [P.dsem("h0"), P.dsem("h1")]
        xfree = [None, None]
        for t in range(NTT):
            s = t % 2
            tx = P.dma("sp", d_x[s], lambda e: e.dma_start(out=xt[s][:], in_=E["x_own"][t * 128:(t + 1) * 128, :]), deps=[xfree[s]])
            hT_tile(nc, P, Bbf, identb, stt, xt[s], tx, t, E["hT_own"], [ct])
            xfree[s] = stt["cast_tok"]
        barrier(P)


PAIRS = [[0, 1], [2, 3], [4, 5], [6, 7]]
NL = DEPTH


def allgather(nc, P, src, dst, kind):
    barrier(P)
    key = P.dsem("cc")
    for c in range(4):
        if kind == "hT":
            s_ap = src[c * 256:(c + 1) * 256, :]
            d_ap = dst[c].rearrange("r kk p t -> (r kk p) t")
        else:
            s_ap = src[c * 128:(c + 1) * 128, :]
            d_ap = dst[c].rearrange("r q t -> (r q) t")
        P.cc(key, lambda e: e.collective_compute("AllGather", ALU.bypass, replica_groups=PAIRS, ins=[s_ap.opt()], outs=[d_ap.opt()]))
    barrier(P)


def build_fused():
    nc = bass.Bass("TRN2", target_bir_lowering=False)
    E = {}

    def din(name, shape, dt=F32):
        E[name] = nc.dram_tensor(name, shape, dt, kind="ExternalInput").ap()

    def dint(name, shape, dt):
        E[name] = nc.dram_tensor(name, shape, dt, kind="Internal").ap()

    din("x_own", [NTOK, 1024])
    din("memT", [1024, N_MEM])
    din("wmk", [1024, 128])
    din("wmv", [1024, 128])
    din("wq", [NL, 1024, 512])
    din("wk", [NL, 1024, 384])
    din("wv", [NL, 1024, 384])
    din("wfgp", [2, 3, 1024, 512])
    din("bfg", [2, 32, 3])
    din("ident", [128, 128])
    din("negm", [128, 512])
    din("kaug", [36, SEQ])
    din("qaug", [36, SEQ])
    din("iq", [32, 16])
    din("sel", [32, 256])
    din("tri", [32, 32])
    din("cdec", [3, 32, 512])
    din("maskm", [128, 2048])
    din("idxm", [128, 64], I32)
    din("wo", [NL, 1024, 1024])
    for nm in ("ln1g", "ln1b", "ln2g", "ln2b"):
        din(nm, [NL, 128, 1024])
    din("wr", [NL, 1024, 32])
    din("rb", [NL, 128, 32])
    din("wgu", [NL, N_EXPERTS, 1024, 2048])
    din("bgu", [NL, N_EXPERTS, 2048])
    din("wdn", [NL, N_EXPERTS, 1024, 1024])
    din("bdn", [NL, N_EXPERTS, 1024])
    din("tstr", [128, 128])
    din("iotae", [128, 32])
    din("ecap", [128, 32])
    E["out"] = nc.dram_tensor("out", [NTOK, 1024], F32, kind="ExternalOutput").ap()
    dint("hT_own", [1024, NTOK], BF16)
    dint("hTp", [4, 2, 2, 128, NTOK], BF16)
    dint("OT_own", [512, SEQ], BF16)
    dint("OT_pair", [4, 2, 128, SEQ], BF16)
    dint("dscr", [2, 2, 2, SEQ], BF16)
    dint("H1", [NTOK, 1024], F32)
    dint("hres", [NTOK, 1024], F32)
    dint("Xs", [NSLOT, 1024], BF16)
    dint("Ys", [NSLOT, 1024], F32)

    with contextlib.ExitStack() as st:
        P = Prog(nc, st)
        B = [st.enter_context(nc.psum_tensor("B%d" % i, [128, 512], F32)) for i in range(7)]
        Bbf = st.enter_context(nc.psum_tensor("Bbf", [128, 1024], BF16))
        E["bc_reg"] = nc.gpsimd.to_reg(NSLOT - 1)
        E["bc_reg2"] = nc.gpsimd.to_reg(16383)
        emit_hT0(nc, P, Bbf, E)
        allgather(nc, P, E["hT_own"], E["hTp"], "hT")
        for L in range(NL):
            emit_attn(nc, P, B, Bbf, "moba" if L % 2 == 0 else "fox", E, L)
            allgather(nc, P, E["OT_own"], E["OT_pair"], "OT")
            emit_ffn(nc, P, B, Bbf, E, L)
            if L < NL - 1:
                allgather(nc, P, E["hT_own"], E["hTp"], "hT")
                P.new_epoch()
        barrier(P)
    return nc


def ffn_consts():
    if "ffn" in _CONST:
        return _CONST["ffn"]
    c = {}
    c["ident"] = np.eye(128, dtype=np.float32)
    k = np.arange(128)
    c["tstr"] = (k[:, None] < k[None, :]).astype(np.float32)
    c["iotae"] = np.ascontiguousarray(np.broadcast_to(np.arange(32, dtype=np.float32)[None, :], (128, 32)))
    c["ecap"] = np.ascontiguousarray(np.broadcast_to((np.arange(32, dtype=np.float32) * CAP)[None, :], (128, 32)))
    _CONST["ffn"] = c
    return c


def bc128(v):
    return np.ascontiguousarray(np.broadcast_to(np.asarray(v, np.float32).reshape(1, -1), (128, v.size)))


def ffn_inputs(h_tok, mT_tok, wo_l, ln1g, ln1b, ln2g, ln2b, wr_l, rb_l, wgu_l, bgu_l, wdn_l, bdn_l):
    m = dict(ffn_consts())
    m.update(h=h_tok, mT=mT_tok, wo=wo_l, ln1g=bc128(ln1g), ln1b=bc128(ln1b), ln2g=bc128(ln2g), ln2b=bc128(ln2b),
             wr=wr_l, rb=bc128(rb_l), wgu=wgu_l, bgu=bgu_l, wdn=wdn_l, bdn=bdn_l)
    return m


_FUSED = {}


def kernel(x, mem, w_in_moba, w_in_fox, b_fgate, w_mem_kv, w_o, ln1_g, ln1_b, router_w, router_b,
           w_gate_up, b_gate_up, w_down, b_down, ln2_g, ln2_b):
    f32 = lambda a: np.ascontiguousarray(np.asarray(a, dtype=np.float32))
    x, mem, w_mem_kv = f32(x), f32(mem), f32(w_mem_kv)
    w_in_moba, w_in_fox, b_fgate, w_o = f32(w_in_moba), f32(w_in_fox), f32(b_fgate), f32(w_o)
    SW = N_SELF * HEAD_DIM
    shared = dict(ffn_consts())
    ca = attn_consts("moba", 0)
    for k in ("ident", "negm", "kaug", "qaug", "iq", "sel", "maskm"):
        shared[k] = ca[k]
    shared["tri"] = attn_consts("fox", 0)["tri"]
    shared["wr"] = f32(router_w)[:NL]
    shared["rb"] = np.stack([bc128(f32(router_b)[l]) for l in range(NL)])
    for nm, v in (("ln1g", ln1_g), ("ln1b", ln1_b), ("ln2g", ln2_g), ("ln2b", ln2_b)):
        shared[nm] = np.stack([bc128(f32(v)[l]) for l in range(NL)])
    shared["wgu"], shared["bgu"] = f32(w_gate_up)[:NL], f32(b_gate_up)[:NL]
    shared["wdn"], shared["bdn"] = f32(w_down)[:NL], f32(b_down)[:NL]
    perm = np.zeros(1024, np.int64)
    for hp in range(4):
        for r in range(2):
            for q in range(128):
                hl, dd = 2 * hp + q // 64, q % 64
                g = (6 * r + hl) * 64 + dd if hl < 6 else 768 + (2 * r + hl - 6) * 64 + dd
                perm[(hp * 2 + r) * 128 + q] = g
    shared["wo"] = np.ascontiguousarray(w_o[:NL][:, perm, :])
    per_half = []
    for hh in range(2):
        d = {}
        heads = slice(6 * hh * 64, (6 * hh + 6) * 64)
        wq, wk, wv = [], [], []
        for l in range(NL):
            w_in = w_in_moba[l // 2] if l % 2 == 0 else w_in_fox[l // 2]
            wq.append(np.concatenate([w_in[:, 0:SW][:, heads], w_in[:, 3 * SW:3 * SW + 256][:, hh * 128:(hh + 1) * 128]], axis=1))
            wk.append(w_in[:, SW:2 * SW][:, heads])
            wv.append(w_in[:, 2 * SW:3 * SW][:, heads])
        d["wq"], d["wk"], d["wv"] = np.ascontiguousarray(np.stack(wq)), np.ascontiguousarray(np.stack(wk)), np.ascontiguousarray(np.stack(wv))
        wfgp = np.zeros((2, 3, 1024, 16, 32), np.float32)
        bfg = np.zeros((2, 32, 3), np.float32)
        for j in range(2):
            wfg = w_in_fox[j][:, 2560:2572]
            for pp in range(3):
                for r in range(2):
                    h = 6 * hh + 2 * pp + r
                    for i in range(16):
                        wfgp[j, pp, :, i, r * 16 + i] = wfg[:, h]
                    bfg[j, r * 16:(r + 1) * 16, pp] = b_fgate[j, h]
        d["wfgp"] = wfgp.reshape(2, 3, 1024, 512)
        d["bfg"] = bfg
        d["cdec"] = attn_consts("moba", hh)["cdec"]
        d["wmk"] = np.ascontiguousarray(w_mem_kv[:, 0:256][:, hh * 128:(hh + 1) * 128])
        d["wmv"] = np.ascontiguousarray(w_mem_kv[:, 256:512][:, hh * 128:(hh + 1) * 128])
        kc = np.arange(8)[None, :, None]
        p = np.arange(128)[:, None, None]
        g = np.arange(8)[None, None, :]
        d["idxm"] = np.ascontiguousarray((((kc * 128 + p) * 2 + hh) * 8 + g).reshape(128, 64).astype(np.int32))
        per_half.append(d)
    maps = []
    for c in range(8):
        b, hh = c // 2, c % 2
        m = dict(shared)
        m.update(per_half[hh])
        m["x_own"] = np.ascontiguousarray(x[b, hh * NTOK:(hh + 1) * NTOK])
        m["memT"] = np.ascontiguousarray(mem[b].T)
        maps.append(m)
    if NL not in _FUSED:
        _FUSED[NL] = build_fused()
    res = run_bass_kernel_spmd(_FUSED[NL], maps, core_ids=list(range(8))).results
    out = np.empty((BATCH, SEQ, D_MODEL), np.float32)
    for c in range(8):
        b, hh = c // 2, c % 2
        out[b, hh * NTOK:(hh + 1) * NTOK] = np.asarray(res[c]["out"])
    return out
```
